# Optimizing a Trainium2 kernel written in Bass

```python
import jax, jax.numpy as jnp
from jax import lax
import numpy as np

D_MODEL = 1024
BATCH = 8
SEQ = 4096
DEPTH = 4

N_SELF = DEPTH // 2
N_CROSS = DEPTH - N_SELF

D_FF = 2816

RET_HEADS = 8
RET_QK_DIM = D_MODEL // RET_HEADS
RET_V_DIM = 2 * D_MODEL // RET_HEADS
RET_CHUNK = 128
RET_PROJ = 2 * RET_HEADS * RET_QK_DIM + 2 * RET_HEADS * RET_V_DIM

MLA_HEADS = 8
MLA_NOPE = 128
MLA_ROPE = 64
MLA_V = 128
Q_LORA = 384
KV_LORA = 256
ATTN_BLOCK = 128

ROPE_BASE = 10000.0
EPS = 1e-6

kernel_name = "yoco_retnet_mla_macaron"


def rmsnorm(x, g):
    xf = x.astype(jnp.float32)
    y = xf * lax.rsqrt(jnp.mean(xf * xf, axis=-1, keepdims=True) + EPS)
    return (y * g.astype(jnp.float32)).astype(x.dtype)


def swiglu(x, w_gate, w_up, w_down):
    return (jax.nn.silu(x @ w_gate) * (x @ w_up)) @ w_down


def rope(x, positions):
    d = x.shape[-1]
    inv_freq = ROPE_BASE ** (-jnp.arange(0, d, 2, dtype=jnp.float32) / d)
    ang = positions.astype(jnp.float32)[..., None] * inv_freq
    cos = jnp.cos(ang)[:, :, None, :].astype(x.dtype)
    sin = jnp.sin(ang)[:, :, None, :].astype(x.dtype)
    x1, x2 = jnp.split(x, 2, axis=-1)
    return jnp.concatenate([x1 * cos - x2 * sin, x2 * cos + x1 * sin], axis=-1)


def retention(x, positions, w_in, gn_g, w_o):
    B, S, _ = x.shape
    H, dk, dv, C = RET_HEADS, RET_QK_DIM, RET_V_DIM, RET_CHUNK
    N = S // C
    proj = x @ w_in
    q, k, v, g = jnp.split(proj, [H * dk, 2 * H * dk, 2 * H * dk + H * dv], axis=-1)
    q = rope(q.reshape(B, S, H, dk), positions)
    k = rope(k.reshape(B, S, H, dk), positions) * (dk ** -0.5)
    v = v.reshape(B, S, H, dv)

    lg = jnp.log1p(-jnp.exp2(-5.0 - jnp.arange(H, dtype=jnp.float32)))
    idx = jnp.arange(C, dtype=jnp.float32)
    diff = idx[:, None] - idx[None, :]
    d_intra = jnp.where(diff >= 0, jnp.exp(lg[:, None, None] * jnp.maximum(diff, 0.0)), 0.0).astype(x.dtype)
    q_decay = jnp.exp(lg[:, None] * (idx[None, :] + 1.0)).astype(x.dtype)
    k_decay = jnp.exp(lg[:, None] * (C - 1.0 - idx[None, :])).astype(x.dtype)
    chunk_decay = jnp.exp(lg * C).astype(x.dtype)

    to_chunks = lambda t: t.reshape(B, N, C, H, t.shape[-1]).transpose(1, 0, 3, 2, 4)
    qc, kc, vc = to_chunks(q), to_chunks(k), to_chunks(v)

    scores = jnp.einsum('nbhid,nbhjd->nbhij', qc, kc) * d_intra[None, None]
    inner = jnp.einsum('nbhij,nbhjv->nbhiv', scores, vc)

    def step(state, inp):
        q_t, k_t, v_t = inp
        cross = jnp.einsum('bhcd,bhdv->bhcv', q_t * q_decay[None, :, :, None], state)
        state = state * chunk_decay[None, :, None, None] + jnp.einsum(
            'bhcd,bhcv->bhdv', k_t * k_decay[None, :, :, None], v_t)
        return state, cross

    state0 = jnp.zeros((B, H, dk, dv), dtype=qc.dtype)
    _, cross = lax.scan(step, state0, (qc, kc, vc))
    o = (inner + cross).transpose(1, 0, 3, 2, 4).reshape(B, S, H, dv)

    of = o.astype(jnp.float32)
    mu = jnp.mean(of, axis=-1, keepdims=True)
    var = jnp.mean(jnp.square(of - mu), axis=-1, keepdims=True)
    o = ((of - mu) * lax.rsqrt(var + EPS) * gn_g.astype(jnp.float32)).astype(x.dtype)
    o = o.reshape(B, S, H * dv)
    return (jax.nn.silu(g) * o) @ w_o


def mla_shared_kv(h, positions, kv_norm_g, kv_w_down, kv_latent_norm_g, kv_w_up,
                  k_nope_norm_g, k_rope_norm_g):
    B, S, _ = h.shape
    hn = rmsnorm(h, kv_norm_g)
    c_kv, k_pe = jnp.split(hn @ kv_w_down, [KV_LORA], axis=-1)
    c_kv = rmsnorm(c_kv, kv_latent_norm_g)
    kv = (c_kv @ kv_w_up).reshape(B, S, MLA_HEADS, MLA_NOPE + MLA_V)
    k_nope, v = jnp.split(kv, [MLA_NOPE], axis=-1)
    k_nope = rmsnorm(k_nope, k_nope_norm_g)
    k_pe = rope(rmsnorm(k_pe, k_rope_norm_g)[:, :, None, :], positions)
    k = jnp.concatenate([k_nope, jnp.broadcast_to(k_pe, (B, S, MLA_HEADS, MLA_ROPE))], axis=-1)
    return k, v


def mla_attend(x, positions, k, v, w_dq, q_lora_norm_g, w_uq, q_nope_norm_g, q_rope_norm_g, w_o):
    B, S, _ = x.shape
    H = MLA_HEADS
    cq = rmsnorm(x @ w_dq, q_lora_norm_g)
    q = (cq @ w_uq).reshape(B, S, H, MLA_NOPE + MLA_ROPE)
    q_nope, q_pe = jnp.split(q, [MLA_NOPE], axis=-1)
    q = jnp.concatenate([rmsnorm(q_nope, q_nope_norm_g),
                         rope(rmsnorm(q_pe, q_rope_norm_g), positions)], axis=-1)
    scale = (MLA_NOPE + MLA_ROPE) ** -0.5
    nb = S // ATTN_BLOCK
    q_blocks = q.reshape(B, nb, ATTN_BLOCK, H, MLA_NOPE + MLA_ROPE).transpose(1, 0, 2, 3, 4)
    key_pos = jnp.arange(S)

    def block(args):
        qb, blk = args
        s = jnp.einsum('bqhd,bkhd->bhqk', qb, k).astype(jnp.float32) * scale
        q_pos = blk * ATTN_BLOCK + jnp.arange(ATTN_BLOCK)
        mask = key_pos[None, :] <= q_pos[:, None]
        p = jax.nn.softmax(jnp.where(mask[None, None], s, -jnp.inf), axis=-1)
        return jnp.einsum('bhqk,bkhd->bqhd', p.astype(v.dtype), v)

    o = lax.map(block, (q_blocks, jnp.arange(nb)))
    o = o.transpose(1, 0, 2, 3, 4).reshape(B, S, H * MLA_V)
    return o @ w_o


def setup_inputs(seed: int = 0) -> dict:
    key = jax.random.key(seed)
    ks = iter(jax.random.split(key, 32))
    f32 = jnp.float32

    def w(shape, fan_in):
        return jax.random.normal(next(ks), shape, f32) * (fan_in ** -0.5)

    def gain(shape):
        return 1.0 + 0.02 * jax.random.normal(next(ks), shape, f32)

    x = jax.random.normal(next(ks), (BATCH, SEQ, D_MODEL), f32)
    positions = jnp.broadcast_to(jnp.arange(SEQ, dtype=jnp.int32)[None, :], (BATCH, SEQ))
    return {
        "x": x,
        "positions": positions,
        "norm_g": gain((DEPTH, 3, D_MODEL)),
        "ffn_w_gate": w((DEPTH, 2, D_MODEL, D_FF), D_MODEL),
        "ffn_w_up": w((DEPTH, 2, D_MODEL, D_FF), D_MODEL),
        "ffn_w_down": w((DEPTH, 2, D_FF, D_MODEL), D_FF),
        "ret_w_in": w((N_SELF, D_MODEL, RET_PROJ), D_MODEL),
        "ret_gn_g": gain((N_SELF, RET_HEADS, RET_V_DIM)),
        "ret_w_o": w((N_SELF, RET_HEADS * RET_V_DIM, D_MODEL), RET_HEADS * RET_V_DIM),
        "kv_norm_g": gain((D_MODEL,)),
        "kv_w_down": w((D_MODEL, KV_LORA + MLA_ROPE), D_MODEL),
        "kv_latent_norm_g": gain((KV_LORA,)),
        "kv_w_up": w((KV_LORA, MLA_HEADS * (MLA_NOPE + MLA_V)), KV_LORA),
        "k_nope_norm_g": gain((MLA_NOPE,)),
        "k_rope_norm_g": gain((MLA_ROPE,)),
        "mla_w_dq": w((N_CROSS, D_MODEL, Q_LORA), D_MODEL),
        "mla_q_lora_norm_g": gain((N_CROSS, Q_LORA)),
        "mla_w_uq": w((N_CROSS, Q_LORA, MLA_HEADS * (MLA_NOPE + MLA_ROPE)), Q_LORA),
        "mla_q_nope_norm_g": gain((N_CROSS, MLA_NOPE)),
        "mla_q_rope_norm_g": gain((N_CROSS, MLA_ROPE)),
        "mla_w_o": w((N_CROSS, MLA_HEADS * MLA_V, D_MODEL), MLA_HEADS * MLA_V),
    }


def reference(x, positions, norm_g, ffn_w_gate, ffn_w_up, ffn_w_down,
              ret_w_in, ret_gn_g, ret_w_o,
              kv_norm_g, kv_w_down, kv_latent_norm_g, kv_w_up, k_nope_norm_g, k_rope_norm_g,
              mla_w_dq, mla_q_lora_norm_g, mla_w_uq, mla_q_nope_norm_g, mla_q_rope_norm_g, mla_w_o):
    k_shared = None
    v_shared = None
    for layer in range(DEPTH):
        x = x + 0.5 * swiglu(rmsnorm(x, norm_g[layer, 0]), ffn_w_gate[layer, 0],
                             ffn_w_up[layer, 0], ffn_w_down[layer, 0])
        h = rmsnorm(x, norm_g[layer, 1])
        if layer < N_SELF:
            x = x + retention(h, positions, ret_w_in[layer], ret_gn_g[layer], ret_w_o[layer])
        else:
            j = layer - N_SELF
            x = x + mla_attend(h, positions, k_shared, v_shared, mla_w_dq[j], mla_q_lora_norm_g[j],
                               mla_w_uq[j], mla_q_nope_norm_g[j], mla_q_rope_norm_g[j], mla_w_o[j])
        x = x + 0.5 * swiglu(rmsnorm(x, norm_g[layer, 2]), ffn_w_gate[layer, 1],
                             ffn_w_up[layer, 1], ffn_w_down[layer, 1])
        if layer == N_SELF - 1:
            k_shared, v_shared = mla_shared_kv(x, positions, kv_norm_g, kv_w_down, kv_latent_norm_g,
                                               kv_w_up, k_nope_norm_g, k_rope_norm_g)
    return x
```

```python
import contextlib
import math
import numpy as np
import concourse.bass as bass
import concourse.mybir as mybir
from concourse.bass_utils import run_bass_kernel_spmd

F32 = mybir.dt.float32
BF16 = mybir.dt.bfloat16
I32 = mybir.dt.int32
AF = mybir.ActivationFunctionType
ALU = mybir.AluOpType
AX = mybir.AxisListType

D = 1024
DFF = 2816
NCH = DFF // 128
DEPTH = 4
N_SELF = 2
H = 8
RDK = 128
RDV = 256
RPROJ = 6144
NOPE = 128
ROPE = 64
QL = 384
KVL = 256
EPS = 1e-6
ROPE_BASE = 10000.0
SEQ = 4096
WSLOTS = 66

C_ID, C_MASK, C_KD, C_EPSR, C_INVR, C_INVM, C_SGR, C_SGM, C_NC = 0, 128, 256, 264, 272, 273, 274, 275, 276
G_NORM = 0
G_KVN = 96
G_LAT = 104
G_KN = 106
G_KR = 107
G_QL = 108
G_GN = 118
G_NC = 150


class Prog:
    NDMA = 8

    def __init__(self, nc, stack):
        self.nc = nc
        self.eng = {"pe": nc.tensor, "act": nc.scalar, "dve": nc.vector, "pool": nc.gpsimd, "sp": nc.sync}
        self.sem, self.cnt = {}, {}
        self.seen = {e: {} for e in self.eng}
        self.lastw, self.readers, self.dq = {}, {}, {}
        self.ninst = 0
        for e in self.eng:
            self.sem[e] = stack.enter_context(nc.semaphore(f"s_{e}"))
            self.cnt[e] = 0
        for q in ("sp", "pool"):
            sems = [stack.enter_context(nc.semaphore(f"d_{q}{i}")) for i in range(self.NDMA)]
            self.dq[q] = {"sems": sems, "cnt": [0] * self.NDMA, "i": 0}

    def _deps(self, e, reads, writes):
        toks = []
        for k in reads:
            w = self.lastw.get(k)
            if w is not None:
                toks.append(w)
        for k in writes:
            w = self.lastw.get(k)
            if w is not None:
                toks.append(w)
            toks.extend(self.readers.get(k, {}).values())
        best = {}
        for (sem, val, src) in toks:
            if src == "pe" and e == "pe":
                continue
            key = id(sem)
            if key not in best or best[key][1] < val:
                best[key] = (sem, val)
        out = []
        seen = self.seen[e]
        for key, (sem, val) in best.items():
            if seen.get(key, 0) >= val:
                continue
            seen[key] = val
            out.append((sem, val))
        return out

    def _commit(self, tok, reads, writes):
        sid = id(tok[0])
        for k in reads:
            self.readers.setdefault(k, {})[sid] = tok
        for k in writes:
            self.lastw[k] = tok
            self.readers[k] = {}

    def op(self, e, fn, reads=(), writes=()):
        eng = self.eng[e]
        for (sem, val) in self._deps(e, reads, writes):
            eng.wait_ge(sem, val)
            self.ninst += 1
        ins = fn(eng)
        self.cnt[e] += 1
        ins.then_inc(self.sem[e], 1)
        tok = (self.sem[e], self.cnt[e], e)
        self._commit(tok, reads, writes)
        self.ninst += 1
        return tok

    def dma(self, q, out, in_, reads=(), writes=()):
        eng = self.eng[q]
        d = self.dq[q]
        i = d["i"]
        d["i"] = (i + 1) % self.NDMA
        sem = d["sems"][i]
        waits = self._deps(q, reads, writes)
        prev = d["cnt"][i]
        if prev > 0 and self.seen[q].get(id(sem), 0) < prev:
            self.seen[q][id(sem)] = prev
            waits = [w for w in waits if w[0] is not sem] + [(sem, prev)]
        for (s, v) in waits:
            eng.wait_ge(s, v)
            self.ninst += 1
        eng.dma_start(out=out, in_=in_).then_inc(sem, 16)
        d["cnt"][i] = prev + 16
        tok = (sem, prev + 16, "dma")
        self._commit(tok, reads, writes)
        self.ninst += 1
        return tok

    def barrier(self, engines=("pe", "act", "dve", "pool", "sp"), dma_queues=("sp",)):
        targets = [(self.sem[e], self.cnt[e]) for e in engines if self.cnt[e] > 0]
        for q in dma_queues:
            d = self.dq[q]
            targets += [(s, c) for s, c in zip(d["sems"], d["cnt"]) if c > 0]
        for e in engines:
            for (s, v) in targets:
                if s is self.sem[e]:
                    continue
                if self.seen[e].get(id(s), 0) >= v:
                    continue
                self.seen[e][id(s)] = v
                self.eng[e].wait_ge(s, v)
                self.ninst += 1

    def finish(self):
        for q in ("sp", "pool"):
            d = self.dq[q]
            for s, c in zip(d["sems"], d["cnt"]):
                if c > 0:
                    self.nc.sync.wait_ge(s, c)


def K(name, idxs=None):
    if idxs is None:
        return [name]
    return [(name, i) for i in idxs]


def pstride(t):
    return t[:].ap[0][0]


_UNIQ = [0]


def uniq(n):
    _UNIQ[0] += 1
    return f"{n}_{_UNIQ[0]}"


def build(NT=SEQ, nstages=None, first_layer_only=False):
    nc = bass.Bass("TRN2", target_bir_lowering=False)
    NB = NT // 128

    def din(name, shape, dt=F32):
        return nc.dram_tensor(name, list(shape), dt, kind="ExternalInput").ap()

    x_in = din("x", [NT, D])
    pos_in = din("pos", [1, NT], I32)
    consts_in = din("consts", [128, C_NC])
    gains_in = din("gains", [128, G_NC])
    w_gate = din("ffn_w_gate", [DEPTH, 2, D, DFF])
    w_up = din("ffn_w_up", [DEPTH, 2, D, DFF])
    w_down = din("ffn_w_down", [DEPTH, 2, DFF, D])
    ret_w_in = din("ret_w_in", [N_SELF, D, RPROJ])
    ret_w_o = din("ret_w_o", [N_SELF, H * RDV, D])
    kv_w_down = din("kv_w_down", [D, KVL + ROPE])
    kv_w_up = din("kv_w_up", [KVL, H * (NOPE + 128)])
    mla_w_dq = din("mla_w_dq", [2, D, QL])
    mla_w_uq = din("mla_w_uq", [2, QL, H * (NOPE + ROPE)])
    mla_w_o = din("mla_w_o", [2, H * 128, D])
    out_d = nc.dram_tensor("out", [NT, D], F32, kind="ExternalOutput").ap()

    def dscr(name, shape, dt):
        return nc.dram_tensor(name, list(shape), dt, kind="Internal").ap()

    xs = dscr("xs", [8, 128, NT], F32)
    tabR = dscr("tabR", [2, 128, NT], F32)
    tabM = dscr("tabM", [2, 64, NT], F32)
    knT_d = dscr("knT", [H, 128, NT], BF16)
    kpeT_d = dscr("kpeT", [64, NT], BF16)
    vs_d = dscr("vs", [H, 128, NB, 128], BF16)

    with contextlib.ExitStack() as top:
        P = Prog(nc, top)
        Wt = top.enter_context(nc.sbuf_tensor("W", [128, WSLOTS, 1024], BF16))
        consts = top.enter_context(nc.sbuf_tensor("consts_sb", [128, C_NC], F32))
        gains = top.enter_context(nc.sbuf_tensor("gains_sb", [128, G_NC], F32))
        identb = top.enter_context(nc.sbuf_tensor("identb", [128, 128], BF16))
        onesb = top.enter_context(nc.sbuf_tensor("onesb", [128, 128], BF16))
        maskb = top.enter_context(nc.sbuf_tensor("maskb", [128, 128], BF16))
        PS = top.enter_context(nc.psum_tensor("PS", [128, 8, 512], F32))
        ident = consts[:, C_ID:C_ID + 128]

        def bank(i):
            return PS[:, i, :]

        def bankb(i):
            return PS[:, i, :].bitcast(BF16)

        def BK(i):
            return ("PS", i)

        P.dma("sp", consts[:], consts_in[:, :], writes=K("consts"))
        P.dma("sp", gains[:], gains_in[:, :], writes=K("gains"))
        P.op("dve", lambda e: e.tensor_copy(out=identb[:], in_=consts[:, C_ID:C_ID + 128]), reads=K("consts"), writes=K("identb"))
        P.op("dve", lambda e: e.tensor_copy(out=maskb[:], in_=consts[:, C_MASK:C_MASK + 128]), reads=K("consts"), writes=K("maskb"))
        P.op("pool", lambda e: e.memset(onesb[:], 1.0), writes=K("onesb"))

        def wview(slot0, kch, ncols):
            tot = kch * ncols
            assert tot % 1024 == 0 or True
            nsl = (tot + 1023) // 1024
            assert slot0 + nsl <= WSLOTS
            ps_ = pstride(Wt)
            ap = bass.AP(Wt, slot0 * 1024, [[ps_, 128], [ncols, kch], [1, ncols]])
            return ap, list(range(slot0, slot0 + nsl))

        def wload(src2d, slot0, kch, ncols):
            view, slots = wview(slot0, kch, ncols)
            for k in range(kch):
                s_lo = slot0 + (k * ncols) // 1024
                s_hi = slot0 + ((k + 1) * ncols - 1) // 1024
                P.dma("pool", view[:, k, :], src2d[k * 128:(k + 1) * 128, :],
                      writes=K("W", range(s_lo, s_hi + 1)))
            return view, K("W", slots)

        def mm_group(outap, pairs, reads, writes, first=True, last=True):
            def fn(e):
                ins = None
                n = len(pairs)
                for i, (l, r) in enumerate(pairs):
                    ins = e.matmul(outap, l, r, start=(first and i == 0), stop=(last and i == n - 1))
                return ins
            return P.op("pe", fn, reads=reads, writes=writes)

        def norm_T(srcs, src_keys, npart, gcols, outs, out_keys, sq, sq_keys, rs, rs_key, ssb, Tn, out_rope_f32=False):
            n = len(srcs)
            for i in range(n):
                P.op("act", lambda e, i=i: e.activation(out=sq[i], in_=srcs[i], func=AF.Square),
                     reads=src_keys[i], writes=sq_keys[i])
            mm_group(bank(ssb)[0:npart, 0:Tn], [(onesb[0:npart, 0:npart], sq[i]) for i in range(n)],
                     reads=K("onesb") + [k for ks in sq_keys for k in ks], writes=[BK(ssb)])
            P.op("act", lambda e: e.activation(out=rs, in_=bank(ssb)[0:npart, 0:Tn], func=AF.Sqrt,
                                               scale=1.0 / (n * npart), bias=EPS),
                 reads=[BK(ssb)], writes=rs_key)
            P.op("dve", lambda e: e.reciprocal(out=rs, in_=rs), reads=rs_key, writes=rs_key)
            for i in range(n):
                P.op("dve", lambda e, i=i: e.scalar_tensor_tensor(
                    out=outs[i], in0=srcs[i], scalar=gains[0:npart, gcols[i]:gcols[i] + 1], in1=rs,
                    op0=ALU.mult, op1=ALU.mult),
                    reads=src_keys[i] + rs_key + K("gains"), writes=out_keys[i])

        def rope_T(src, src_key, npart, cosT, sinT, tab_key, t1, t2, tkeys, out, out_key, eng2="pool"):
            hf = npart // 2
            P.op("dve", lambda e: e.tensor_tensor(out=t1, in0=src, in1=cosT, op=ALU.mult),
                 reads=src_key + tab_key, writes=[tkeys[0]])
            P.op(eng2, lambda e: e.tensor_tensor(out=t2[0:hf], in0=src[hf:npart], in1=sinT[hf:npart], op=ALU.mult),
                 reads=src_key + tab_key, writes=[tkeys[1]])
            P.op(eng2, lambda e: e.tensor_tensor(out=t2[hf:npart], in0=src[0:hf], in1=sinT[0:hf], op=ALU.mult),
                 reads=src_key + tab_key, writes=[tkeys[2]])
            P.op("dve", lambda e: e.tensor_tensor(out=out, in0=t1, in1=t2[0:npart], op=ALU.add),
                 reads=list(tkeys), writes=out_key)

        def prologue():
            CH = min(1024, NT)
            with contextlib.ExitStack() as st:
                sbt = lambda n, s, d=F32: st.enter_context(nc.sbuf_tensor(uniq(n), s, d))
                pi = sbt("pi", [128, CH], I32)
                pf = sbt("pf", [128, CH])
                y = sbt("py", [128, CH])
                ki = sbt("pki", [128, CH], I32)
                kf = sbt("pkf", [128, CH])
                tb = sbt("ptb", [128, 2, CH])
                c1 = 6.28125
                c2 = 2.0 * math.pi - c1
                for c0 in range(0, NT, CH):
                    pos_b = bass.AP(pos_in.tensor, c0, [[0, 128], [1, CH]])
                    P.dma("sp", pi[:], pos_b, writes=K("pi"))
                    P.op("dve", lambda e: e.tensor_copy(out=pf[:], in_=pi[:]), reads=K("pi"), writes=K("pf"))
                    for (npart, invc, sgc, dst) in ((128, C_INVR, C_SGR, tabR), (64, C_INVM, C_SGM, tabM)):
                        for which in (0, 1):
                            shift = math.pi / 2 if which == 0 else 0.0
                            P.op("dve", lambda e: e.tensor_scalar(out=y[0:npart], in0=pf[0:npart], scalar1=consts[0:npart, invc:invc + 1],
                                                                  scalar2=shift, op0=ALU.mult, op1=ALU.add),
                                 reads=K("pf") + K("consts"), writes=K("py"))
                            P.op("dve", lambda e: e.tensor_scalar(out=ki[0:npart], in0=y[0:npart], scalar1=1.0 / (2 * math.pi), scalar2=None, op0=ALU.mult),
                                 reads=K("py"), writes=K("pki"))
                            P.op("dve", lambda e: e.tensor_copy(out=kf[0:npart], in_=ki[0:npart]), reads=K("pki"), writes=K("pkf"))
                            P.op("dve", lambda e: e.scalar_tensor_tensor(out=y[0:npart], in0=kf[0:npart], scalar=-c1, in1=y[0:npart], op0=ALU.mult, op1=ALU.add),
                                 reads=K("pkf") + K("py"), writes=K("py"))
                            P.op("dve", lambda e: e.scalar_tensor_tensor(out=y[0:npart], in0=kf[0:npart], scalar=-c2, in1=y[0:npart], op0=ALU.mult, op1=ALU.add),
                                 reads=K("pkf") + K("py"), writes=K("py"))
                            P.op("dve", lambda e: e.tensor_scalar(out=y[0:npart], in0=y[0:npart], scalar1=-math.pi, scalar2=math.pi, op0=ALU.max, op1=ALU.min),
                                 reads=K("py"), writes=K("py"))
                            if which == 0:
                                P.op("act", lambda e: e.activation(out=tb[0:npart, 0, :], in_=y[0:npart], func=AF.Sin),
                                     reads=K("py"), writes=K("ptb", [0]))
                            else:
                                P.op("act", lambda e: e.activation(out=tb[0:npart, 1, :], in_=y[0:npart], func=AF.Sin,
                                                                   scale=consts[0:npart, sgc:sgc + 1]),
                                     reads=K("py") + K("consts"), writes=K("ptb", [1]))
                        P.dma("sp", dst[:, :, c0:c0 + CH].rearrange("w p t -> p w t"), tb[0:npart, :, :], reads=K("ptb", [0, 1]))
                P.barrier()

        def load_xT(xT, t, T, first, stg):
            if not first:
                P.dma("sp", xT[:], xs[:, :, t * T:(t + 1) * T].rearrange("c p t -> p c t"), writes=K("xT", range(8)))
                return
            for s in range(T // 128):
                sg = stg[s % 2]
                P.dma("sp", sg[:], x_in[t * T + s * 128: t * T + (s + 1) * 128, :], writes=K("stg", [s % 2]))
                for hb in range(2):
                    b = 6 + hb
                    def fn(e, hb=hb, sg=sg, b=b):
                        ins = None
                        for c in range(4):
                            ins = e.transpose(out=bank(b)[:, c * 128:(c + 1) * 128], in_=sg[:, (hb * 4 + c) * 128:(hb * 4 + c + 1) * 128], identity=ident)
                        return ins
                    P.op("pe", fn, reads=K("stg", [s % 2]) + K("consts"), writes=[BK(b)])
                    P.op("act" if hb == 0 else "dve",
                         (lambda e, hb=hb, b=b, s=s: e.activation(out=xT[:, hb * 4:(hb + 1) * 4, s * 128:(s + 1) * 128],
                                                                  in_=bank(b).rearrange("p (c j) -> p c j", c=4), func=AF.Copy)) if hb == 0 else
                         (lambda e, hb=hb, b=b, s=s: e.tensor_copy(out=xT[:, hb * 4:(hb + 1) * 4, s * 128:(s + 1) * 128],
                                                                   in_=bank(b).rearrange("p (c j) -> p c j", c=4))),
                         reads=[BK(b)], writes=K("xT", range(hb * 4, hb * 4 + 4)))

        def store_xT(xT, t, T, last, stg):
            if not last:
                P.dma("sp", xs[:, :, t * T:(t + 1) * T].rearrange("c p t -> p c t"), xT[:], reads=K("xT", range(8)))
                return
            for s in range(T // 128):
                sg = stg[s % 2]
                for hb in range(2):
                    b = 6 + hb
                    def fn(e, hb=hb, b=b, s=s):
                        ins = None
                        for c in range(4):
                            ins = e.transpose(out=bank(b)[:, c * 128:(c + 1) * 128], in_=xT[:, hb * 4 + c, s * 128:(s + 1) * 128], identity=ident)
                        return ins
                    P.op("pe", fn, reads=K("xT", range(hb * 4, hb * 4 + 4)) + K("consts"), writes=[BK(b)])
                    if hb == 0:
                        P.op("act", lambda e, b=b, sg=sg: e.activation(out=sg[:, 0:512], in_=bank(b), func=AF.Copy),
                             reads=[BK(b)], writes=[("stg", s % 2, 0)] + K("stg", [s % 2]))
                    else:
                        P.op("dve", lambda e, b=b, sg=sg: e.tensor_copy(out=sg[:, 512:1024], in_=bank(b)),
                             reads=[BK(b)], writes=[("stg", s % 2, 1)])
                P.dma("sp", out_d[t * T + s * 128: t * T + (s + 1) * 128, :], sg[:],
                      reads=K("stg", [s % 2]) + [("stg", s % 2, 0), ("stg", s % 2, 1)])

        def store_chunk_prefetch(xT, f, t, T, NTI):
            P.dma("sp", xs[f, :, t * T:(t + 1) * T], xT[:, f, :], reads=K("xT", [f]))
            if t + 1 < NTI:
                P.dma("sp", xT[:, f, :], xs[f, :, (t + 1) * T:(t + 2) * T], writes=K("xT", [f]))

        def ffn_stage(layer, which, first, last):
            T = 512
            NTI = NT // T
            gbase = G_NORM + (layer * 3 + (0 if which == 0 else 2)) * 8
            wg, _ = wview(0, 8, DFF)
            wu, _ = wview(22, 8, DFF)
            wd, _ = wview(44, NCH, D)
            CG = [(0, 6), (6, 12), (12, 17), (17, 22)]
            cgrp = {c: gi for gi, (a, b) in enumerate(CG) for c in range(a, b)}
            for gi, (a, b) in enumerate(CG):
                for (wv, src, nm) in ((wg, w_gate, "Wg"), (wu, w_up, "Wu")):
                    for k in range(8):
                        P.dma("pool", wv[:, k, a * 128:b * 128], src[layer, which][k * 128:(k + 1) * 128, a * 128:b * 128],
                              writes=[(nm, gi, k)])
            for c in range(NCH):
                P.dma("pool", wd[:, c, :], w_down[layer, which][c * 128:(c + 1) * 128, :], writes=[("Wd", c)])
            kd = [("Wd", c) for c in range(NCH)]
            with contextlib.ExitStack() as st:
                sbt = lambda n, s, d=F32: st.enter_context(nc.sbuf_tensor(uniq(n), s, d))
                xT = sbt("xT", [128, 8, T])
                hT = sbt("hT", [128, 8, T], BF16)
                aT = sbt("aT", [128, NCH, T], BF16)
                sg = [sbt(f"sg{i}", [128, T]) for i in range(2)]
                rs = sbt("rs", [128, T])
                stg = [sbt(f"stg{i}", [128, D]) for i in range(2)] if (first or last) else None
                for t in range(NTI):
                    if first or t == 0:
                        load_xT(xT, t, T, first, stg)
                    norm_T([xT[:, c, :] for c in range(8)], [K("xT", [c]) for c in range(8)], 128,
                           [gbase + c for c in range(8)],
                           [hT[:, c, :] for c in range(8)], [K("hT", [c]) for c in range(8)],
                           [aT[:, c, :] for c in range(8)], [K("aT", [c]) for c in range(8)],
                           rs[:], K("rs"), 6, T)
                    for c in range(NCH):
                        bg, bu = c % 2, 2 + c % 2
                        cs = slice(c * 128, (c + 1) * 128)
                        mm_group(bank(bg), [(wg[:, k, cs], hT[:, k, :]) for k in range(8)],
                                 reads=[("Wg", cgrp[c], k) for k in range(8)] + K("hT", range(8)), writes=[BK(bg)])
                        mm_group(bank(bu), [(wu[:, k, cs], hT[:, k, :]) for k in range(8)],
                                 reads=[("Wu", cgrp[c], k) for k in range(8)] + K("hT", range(8)), writes=[BK(bu)])
                        P.op("act", lambda e, c=c, bg=bg: e.activation(out=sg[c % 2][:], in_=bank(bg), func=AF.Silu),
                             reads=[BK(bg)], writes=K("sg", [c % 2]))
                        P.op("dve", lambda e, c=c, bu=bu: e.tensor_tensor(out=aT[:, c, :], in0=bank(bu), in1=sg[c % 2][:], op=ALU.mult),
                             reads=[BK(bu)] + K("sg", [c % 2]), writes=K("aT", [c]))
                    for f in range(8):
                        bo = 4 + f % 2
                        fs = slice(f * 128, (f + 1) * 128)
                        mm_group(bank(bo), [(wd[:, c, fs], aT[:, c, :]) for c in range(NCH)],
                                 reads=kd + K("aT", range(NCH)), writes=[BK(bo)])
                        P.op("dve", lambda e, f=f, bo=bo: e.scalar_tensor_tensor(out=xT[:, f, :], in0=bank(bo), scalar=0.5, in1=xT[:, f, :],
                                                                                 op0=ALU.mult, op1=ALU.add),
                             reads=[BK(bo)] + K("xT", [f]), writes=K("xT", [f]))
                        if not last:
                            if first:
                                P.dma("sp", xs[f, :, t * T:(t + 1) * T], xT[:, f, :], reads=K("xT", [f]))
                            else:
                                store_chunk_prefetch(xT, f, t, T, NTI)
                    if last:
                        store_xT(xT, t, T, True, stg)
                        if t + 1 < NTI and not first:
                            load_xT(xT, t + 1, T, False, None)
                P.barrier()

        def ret_stage(layer):
            T = 256
            NTI = NT // T
            gbase = G_NORM + (layer * 3 + 1) * 8
            gam = [1.0 - 2.0 ** (-5.0 - h) for h in range(H)]
            gC = [g ** 128 for g in gam]
            wi, ki_ = wload(ret_w_in[layer], 0, 8, RPROJ)
            wo, ko_ = wload(ret_w_o[layer], 48, 16, D)
            for cc in range(16):
                P.op("dve", lambda e, cc=cc: e.tensor_scalar(out=wo[:, cc, :], in0=wo[:, cc, :],
                                                             scalar1=gains[:, G_GN + layer * 16 + cc:G_GN + layer * 16 + cc + 1],
                                                             scalar2=None, op0=ALU.mult),
                     reads=K("W", [48 + cc]) + K("gains"), writes=K("W", [48 + cc]))
            with contextlib.ExitStack() as st:
                sbt = lambda n, s, d=F32: st.enter_context(nc.sbuf_tensor(uniq(n), s, d))
                xT = sbt("xT", [128, 8, T])
                hT = sbt("hT", [128, 8, T], BF16)
                yT = sbt("yT", [128, 16, T], BF16)
                rs = sbt("rs", [128, T])
                qT = sbt("qT", [128, 8, T], BF16)
                kT = sbt("kT", [128, 8, T], BF16)
                qf = sbt("qf", [128, T])
                t1 = sbt("t1", [128, T])
                t2 = sbt("t2", [128, T])
                tab = sbt("tab", [128, 2, T])
                vd = sbt("vd", [128, H * RDV], BF16)
                sil = sbt("sil", [128, 512])
                gs = sbt("gs", [128, H * RDV], BF16)
                ktok = sbt("ktok", [128, 8, 128], BF16)
                Ssb = sbt("Ssb", [128, 4, 128], BF16)
                state = sbt("state", [128, H, RDV])
                G = sbt("G", [128, H, RDV], BF16)
                sq = sbt("sq", [128, 1024])
                yn = sbt("yn", [128, 1024])
                y = sbt("y", [128, H * RDV], BF16)
                stt = sbt("stt", [128, 8, 4])
                P.op("pool", lambda e: e.memset(state[:], 0.0), writes=K("state", range(8)))
                P.op("pool", lambda e: e.memset(G[:], 0.0), writes=K("G", range(8)))
                ps_c = pstride(consts)
                for t in range(NTI):
                    if t == 0:
                        load_xT(xT, t, T, False, None)
                    P.dma("sp", tab[:], tabR[:, :, t * T:(t + 1) * T].rearrange("w p t -> p w t"), writes=K("tab"))
                    norm_T([xT[:, c, :] for c in range(8)], [K("xT", [c]) for c in range(8)], 128,
                           [gbase + c for c in range(8)],
                           [hT[:, c, :] for c in range(8)], [K("hT", [c]) for c in range(8)],
                           [yT[:, c, :] for c in range(8)], [K("yT", [c]) for c in range(8)],
                           rs[:], K("rs"), 4, T)
                    i = 0
                    for h in range(H):
                        for (dst, dkey, col0) in ((qT, "qT", 0), (kT, "kT", 1024)):
                            b = i % 2
                            i += 1
                            cs = slice(col0 + h * 128, col0 + (h + 1) * 128)
                            mm_group(bank(b)[:, 0:T], [(wi[:, k, cs], hT[:, k, :]) for k in range(8)],
                                     reads=ki_ + K("hT", range(8)), writes=[BK(b)])
                            P.op("act", lambda e, b=b: e.activation(out=qf[:], in_=bank(b)[:, 0:T], func=AF.Copy),
                                 reads=[BK(b)], writes=K("qf"))
                            rope_T(qf[:], K("qf"), 128, tab[:, 0, :], tab[:, 1, :], K("tab"), t1[:], t2[:],
                                   [("t1",), ("t2", 0), ("t2", 1)], dst[:, h, :], K(dkey, [h]))
                    for n in range(T // 128):
                        tk = slice(n * 128, (n + 1) * 128)
                        for grp in range(8):
                            b = 2 + grp % 2
                            col = 2048 + grp * 512
                            mm_group(bank(b), [(hT[:, k, tk], wi[:, k, col:col + 512]) for k in range(8)],
                                     reads=ki_ + K("hT", range(8)), writes=[BK(b)])
                            if grp < 4:
                                kdb = bass.AP(consts, C_KD + grp * 2, [[ps_c, 128], [1, 2], [0, RDV]])
                                P.op("dve", lambda e, b=b, grp=grp, kdb=kdb: e.tensor_tensor(
                                    out=vd[:, grp * 512:(grp + 1) * 512].rearrange("p (a v) -> p a v", a=2),
                                    in0=bank(b).rearrange("p (a v) -> p a v", a=2), in1=kdb, op=ALU.mult),
                                    reads=[BK(b)] + K("consts"), writes=K("vd", [grp]))
                            else:
                                g4 = grp - 4
                                P.op("act", lambda e, b=b, g4=g4: e.activation(out=gs[:, g4 * 512:(g4 + 1) * 512], in_=bank(b), func=AF.Silu),
                                     reads=[BK(b)], writes=K("gs", [g4]))
                        def fnk(e, tk=tk):
                            ins = None
                            for h in range(H):
                                ins = e.transpose(out=bankb(4)[:, h * 128:(h + 1) * 128], in_=kT[:, h, tk], identity=identb[:])
                            return ins
                        P.op("pe", fnk, reads=K("kT", range(8)) + K("identb"), writes=[BK(4)])
                        P.op("act", lambda e: e.activation(out=ktok[:].rearrange("p h d -> p (h d)"), in_=bankb(4), func=AF.Copy),
                             reads=[BK(4)], writes=K("ktok"))
                        for hg in range(2):
                            hs = list(range(hg * 4, hg * 4 + 4))
                            def fns(e, hs=hs, tk=tk):
                                ins = None
                                for j, h in enumerate(hs):
                                    ins = e.matmul(bank(5)[:, j * 128:(j + 1) * 128], kT[:, h, tk], qT[:, h, tk], start=True, stop=True)
                                return ins
                            P.op("pe", fns, reads=K("kT", hs) + K("qT", hs), writes=[BK(5)])
                            mkb = bass.AP(maskb, 0, [[pstride(maskb), 128], [0, 4], [1, 128]])
                            P.op("dve", lambda e, mkb=mkb: e.tensor_tensor(out=Ssb[:], in0=bank(5).rearrange("p (a i) -> p a i", a=4), in1=mkb, op=ALU.mult),
                                 reads=[BK(5)] + K("maskb"), writes=K("Ssb"))
                            def fno(e, hs=hs, tk=tk):
                                ins = None
                                for j, h in enumerate(hs):
                                    o = PS[:, 6 + j // 2, (j % 2) * 256:(j % 2 + 1) * 256]
                                    e.matmul(o, Ssb[:, j, :], vd[:, h * 256:(h + 1) * 256], start=True, stop=False)
                                    ins = e.matmul(o, qT[:, h, tk], G[:, h, :], start=False, stop=True)
                                return ins
                            P.op("pe", fno, reads=K("Ssb") + K("vd", [hg * 2, hg * 2 + 1]) + K("qT", hs) + K("G", hs), writes=[BK(6), BK(7)])
                            def fnu(e, hs=hs):
                                ins = None
                                for j, h in enumerate(hs):
                                    o = PS[:, 2 + j // 2, (j % 2) * 256:(j % 2 + 1) * 256]
                                    ins = e.matmul(o, ktok[:, h, :], vd[:, h * 256:(h + 1) * 256], start=True, stop=True)
                                return ins
                            P.op("pe", fnu, reads=K("ktok") + K("vd", [hg * 2, hg * 2 + 1]), writes=[BK(2), BK(3)])
                            for j, h in enumerate(hs):
                                o = PS[:, 2 + j // 2, (j % 2) * 256:(j % 2 + 1) * 256]
                                P.op("dve", lambda e, h=h, o=o: e.scalar_tensor_tensor(out=state[:, h, :], in0=state[:, h, :], scalar=gC[h], in1=o,
                                                                                       op0=ALU.mult, op1=ALU.add),
                                     reads=[BK(2 + j // 2)] + K("state", [h]), writes=K("state", [h]))
                                P.op("act", lambda e, h=h: e.activation(out=G[:, h, :], in_=state[:, h, :], func=AF.Copy, scale=gC[h]),
                                     reads=K("state", [h]), writes=K("G", [h]))
                            o4 = PS[:, 6:8, :].rearrange("p b (a v) -> p (b a) v", a=2)
                            oflat = PS[:, 6:8, :].rearrange("p b c -> p (b c)")
                            P.op("act", lambda e: e.activation(out=sq[:], in_=oflat, func=AF.Square), reads=[BK(6), BK(7)], writes=K("sq"))
                            P.op("dve", lambda e: e.tensor_reduce(out=stt[:, 0, :], in_=o4, axis=AX.X, op=ALU.add), reads=[BK(6), BK(7)], writes=K("stt", [0]))
                            P.op("dve", lambda e: e.tensor_reduce(out=stt[:, 1, :], in_=sq[:].rearrange("p (a v) -> p a v", a=4), axis=AX.X, op=ALU.add),
                                 reads=K("sq"), writes=K("stt", [1]))
                            P.op("dve", lambda e: e.tensor_scalar(out=stt[:, 2, :], in0=stt[:, 0, :], scalar1=1.0 / RDV, scalar2=None, op0=ALU.mult),
                                 reads=K("stt", [0]), writes=K("stt", [2]))
                            P.op("dve", lambda e: e.tensor_tensor(out=stt[:, 3, :], in0=stt[:, 2, :], in1=stt[:, 2, :], op=ALU.mult),
                                 reads=K("stt", [2]), writes=K("stt", [3]))
                            P.op("dve", lambda e: e.scalar_tensor_tensor(out=stt[:, 3, :], in0=stt[:, 1, :], scalar=1.0 / RDV, in1=stt[:, 3, :],
                                                                         op0=ALU.mult, op1=ALU.subtract),
                                 reads=K("stt", [1, 3]), writes=K("stt", [3]))
                            P.op("dve", lambda e, hg=hg: e.tensor_tensor(out=stt[:, 3, :], in0=stt[:, 3, :], in1=consts[:, C_EPSR + hg * 4:C_EPSR + hg * 4 + 4], op=ALU.add),
                                 reads=K("stt", [3]) + K("consts"), writes=K("stt", [3]))
                            P.op("act", lambda e: e.activation(out=stt[:, 4, :], in_=stt[:, 3, :], func=AF.Sqrt), reads=K("stt", [3]), writes=K("stt", [4]))
                            P.op("dve", lambda e: e.reciprocal(out=stt[:, 4, :], in_=stt[:, 4, :]), reads=K("stt", [4]), writes=K("stt", [4]))
                            P.op("dve", lambda e: e.scalar_tensor_tensor(out=stt[:, 5, :], in0=stt[:, 2, :], scalar=-1.0, in1=stt[:, 4, :],
                                                                         op0=ALU.mult, op1=ALU.mult),
                                 reads=K("stt", [2, 4]), writes=K("stt", [5]))
                            for j, h in enumerate(hs):
                                o = PS[:, 6 + j // 2, (j % 2) * 256:(j % 2 + 1) * 256]
                                P.op("act", lambda e, j=j, o=o: e.activation(out=yn[:, j * 256:(j + 1) * 256], in_=o, func=AF.Identity,
                                                                             scale=stt[:, 4, j:j + 1], bias=stt[:, 5, j:j + 1]),
                                     reads=[BK(6 + j // 2)] + K("stt", [4, 5]), writes=[("yn", j)])
                            P.op("pool", lambda e, hg=hg: e.tensor_tensor(out=y[:, hg * 1024:(hg + 1) * 1024], in0=yn[:], in1=gs[:, hg * 1024:(hg + 1) * 1024], op=ALU.mult),
                                 reads=[("yn", j) for j in range(4)] + K("gs", [hg * 2, hg * 2 + 1]), writes=K("y", [hg]))
                        for hb in range(2):
                            def fny(e, hb=hb):
                                ins = None
                                for c in range(8):
                                    cc = hb * 8 + c
                                    ins = e.transpose(out=bankb(hb)[:, c * 128:(c + 1) * 128], in_=y[:, cc * 128:(cc + 1) * 128], identity=identb[:])
                                return ins
                            P.op("pe", fny, reads=K("y", [hb]) + K("identb"), writes=[BK(hb)])
                            P.op("act" if hb == 0 else "dve",
                                 (lambda e, hb=hb, tk=tk: e.activation(out=yT[:, hb * 8:(hb + 1) * 8, tk], in_=bankb(hb).rearrange("p (c j) -> p c j", c=8), func=AF.Copy)) if hb == 0 else
                                 (lambda e, hb=hb, tk=tk: e.tensor_copy(out=yT[:, hb * 8:(hb + 1) * 8, tk], in_=bankb(hb).rearrange("p (c j) -> p c j", c=8))),
                                 reads=[BK(hb)], writes=K("yT", range(hb * 8, hb * 8 + 8)))
                    for f in range(8):
                        bo = 2 + f % 2
                        fs = slice(f * 128, (f + 1) * 128)
                        mm_group(bank(bo)[:, 0:T], [(wo[:, c, fs], yT[:, c, :]) for c in range(16)],
                                 reads=ko_ + K("yT", range(16)), writes=[BK(bo)])
                        P.op("dve", lambda e, f=f, bo=bo: e.tensor_tensor(out=xT[:, f, :], in0=bank(bo)[:, 0:T], in1=xT[:, f, :], op=ALU.add),
                             reads=[BK(bo)] + K("xT", [f]), writes=K("xT", [f]))
                        store_chunk_prefetch(xT, f, t, T, NTI)
                P.barrier()

        def kv_stage():
            T = 512
            NTI = NT // T
            wdn, kdn = wload(kv_w_down, 0, 8, KVL + ROPE)
            wup, kup = wload(kv_w_up, 4, 2, 2048)
            with contextlib.ExitStack() as st:
                sbt = lambda n, s, d=F32: st.enter_context(nc.sbuf_tensor(uniq(n), s, d))
                xT = sbt("xT", [128, 8, T])
                hT = sbt("hT", [128, 8, T], BF16)
                sqb = sbt("sqb", [128, 8, T], BF16)
                rs = sbt("rs", [128, T])
                cnT = sbt("cnT", [128, 2, T], BF16)
                kpf = sbt("kpf", [64, T])
                t1 = sbt("t1", [128, T])
                t2 = sbt("t2", [128, T])
                tab = sbt("tab", [64, 2, T])
                kpo = sbt("kpo", [64, T], BF16)
                kno = sbt("kno", [128, H, T], BF16)
                vsb = sbt("vsb", [128, 4, 1024], BF16)
                for t in range(NTI):
                    load_xT(xT, t, T, False, None)
                    P.dma("sp", tab[:], tabM[:, :, t * T:(t + 1) * T].rearrange("w p t -> p w t"), writes=K("tab"))
                    norm_T([xT[:, c, :] for c in range(8)], [K("xT", [c]) for c in range(8)], 128,
                           [G_KVN + c for c in range(8)],
                           [hT[:, c, :] for c in range(8)], [K("hT", [c]) for c in range(8)],
                           [sqb[:, c, :] for c in range(8)], [K("sqb", [c]) for c in range(8)],
                           rs[:], K("rs"), 7, T)
                    for m in range(2):
                        mm_group(bank(m), [(wdn[:, k, m * 128:(m + 1) * 128], hT[:, k, :]) for k in range(8)],
                                 reads=kdn + K("hT", range(8)), writes=[BK(m)])
                    mm_group(bank(2)[0:64, :], [(wdn[:, k, 256:320], hT[:, k, :]) for k in range(8)],
                             reads=kdn + K("hT", range(8)), writes=[BK(2)])
                    norm_T([bank(0), bank(1)], [[BK(0)], [BK(1)]], 128, [G_LAT, G_LAT + 1],
                           [cnT[:, 0, :], cnT[:, 1, :]], [K("cnT", [0]), K("cnT", [1])],
                           [sqb[:, 0, :], sqb[:, 1, :]], [K("sqb", [0]), K("sqb", [1])], rs[:], K("rs"), 3, T)
                    norm_T([bank(2)[0:64, :]], [[BK(2)]], 64, [G_KR], [kpf[:]], [K("kpf")],
                           [sqb[0:64, 2, :]], [K("sqb", [2])], rs[0:64, :], K("rs"), 3, T)
                    rope_T(kpf[:], K("kpf"), 64, tab[:, 0, :], tab[:, 1, :], K("tab"), t1[0:64, :], t2[0:64, :],
                           [("t1",), ("t2", 0), ("t2", 1)], kpo[:], K("kpo"))
                    P.dma("sp", kpeT_d[:, t * T:(t + 1) * T], kpo[:], reads=K("kpo"))
                    for h in range(H):
                        b = 4 + h % 2
                        mm_group(bank(b), [(wup[:, m, h * 256:h * 256 + 128], cnT[:, m, :]) for m in range(2)],
                                 reads=kup + K("cnT", [0, 1]), writes=[BK(b)])
                        norm_T([bank(b)], [[BK(b)]], 128, [G_KN], [kno[:, h, :]], [K("kno", [h])],
                               [sqb[:, 3 + h % 2, :]], [K("sqb", [3 + h % 2])], rs[:], K("rs"), 6, T)
                    P.dma("sp", knT_d[:, :, t * T:(t + 1) * T].rearrange("h p t -> p h t"), kno[:], reads=K("kno", range(8)))
                    psw = pstride(Wt)
                    for s in range(4):
                        for half in range(2):
                            b = half
                            pairs = []
                            for m in range(2):
                                rhs = bass.AP(Wt, 4 * 1024 + m * 2048 + half * 1024 + 128, [[psw, 128], [256, 4], [1, 128]])
                                pairs.append((cnT[:, m, s * 128:(s + 1) * 128], rhs))
                            mm_group(bank(b), pairs, reads=kup + K("cnT", [0, 1]), writes=[BK(b)])
                            P.op("act", lambda e, s=s, half=half, b=b: e.activation(out=vsb[:, s, half * 512:(half + 1) * 512], in_=bank(b), func=AF.Copy),
                                 reads=[BK(b)], writes=K("vsb", [s]))
                    for h in range(H):
                        P.dma("sp", vs_d[h, :, t * 4:(t + 1) * 4, :], vsb[:, :, h * 128:(h + 1) * 128], reads=K("vsb", range(4)))
                P.barrier()

        def mla_stage(layer):
            T = 512
            NTI = NT // T
            j_ = layer - N_SELF
            gbase = G_NORM + (layer * 3 + 1) * 8
            gq = G_QL + j_ * 5
            scale = (NOPE + ROPE) ** -0.5
            wdq, kdq = wload(mla_w_dq[j_], 0, 8, QL)
            wuq, kuq = wload(mla_w_uq[j_], 3, 3, 1536)
            wo, ko_ = wload(mla_w_o[j_], 8, 8, D)
            with contextlib.ExitStack() as st:
                sbt = lambda n, s, d=F32: st.enter_context(nc.sbuf_tensor(uniq(n), s, d))
                def aview(slot0, nslots):
                    return Wt[:, slot0:slot0 + nslots, :].rearrange("p s c -> p (s c)")
                xT = sbt("xT", [128, 8, T])
                rs = sbt("rs", [128, T])
                cqf = sbt("cqf", [128, 3, T])
                qpf = sbt("qpf", [64, T])
                t1 = sbt("t1", [64, T])
                t2 = sbt("t2", [64, T])
                tab = sbt("tab", [64, 2, T])
                hT = aview(16, 4).rearrange("p (c t) -> p c t", c=8)
                sqb = aview(20, 4).rearrange("p (c t) -> p c t", c=8)
                qnT = aview(24, 4).rearrange("p (c t) -> p c t", c=8)
                qpT = aview(28, 4).rearrange("p (c t) -> p c t", c=8)[0:64]
                oT = aview(32, 4).rearrange("p (c t) -> p c t", c=8)
                cqT = aview(36, 2)[:, 0:3 * T].rearrange("p (c t) -> p c t", c=3)
                pTall = aview(38, 2).rearrange("p (c t) -> p c t", c=4)
                pT = [pTall[:, i, :] for i in range(4)]
                knS = [aview(40 + 4 * i, 4) for i in range(2)]
                vS = [aview(48 + 4 * i, 4).rearrange("p (b d) -> p b d", d=128) for i in range(2)]
                kpS = aview(56, 4)[0:64]
                rl = sbt("rl", [128, T])
                for t in range(NTI):
                    nk = (t + 1) * T
                    nkb = nk // 128
                    if t == 0:
                        load_xT(xT, t, T, False, None)
                    P.dma("sp", tab[:], tabM[:, :, t * T:(t + 1) * T].rearrange("w p t -> p w t"), writes=K("tab"))
                    P.dma("sp", kpS[:, 0:nk], kpeT_d[:, 0:nk], writes=K("kpS"))
                    norm_T([xT[:, c, :] for c in range(8)], [K("xT", [c]) for c in range(8)], 128,
                           [gbase + c for c in range(8)],
                           [hT[:, c, :] for c in range(8)], [K("hT", [c]) for c in range(8)],
                           [sqb[:, c, :] for c in range(8)], [K("sqb", [c]) for c in range(8)],
                           rs[:], K("rs"), 6, T)
                    for m in range(3):
                        b = 6 + m % 2
                        mm_group(bank(b), [(wdq[:, k, m * 128:(m + 1) * 128], hT[:, k, :]) for k in range(8)],
                                 reads=kdq + K("hT", range(8)), writes=[BK(b)])
                        P.op("act", lambda e, m=m, b=b: e.activation(out=cqf[:, m, :], in_=bank(b), func=AF.Copy),
                             reads=[BK(b)], writes=K("cqf", [m]))
                    norm_T([cqf[:, m, :] for m in range(3)], [K("cqf", [m]) for m in range(3)], 128, [gq, gq + 1, gq + 2],
                           [cqT[:, m, :] for m in range(3)], [K("cqT", [m]) for m in range(3)],
                           [sqb[:, m, :] for m in range(3)], [K("sqb", [m]) for m in range(3)], rs[:], K("rs"), 6, T)
                    for h in range(H):
                        b = 6 + h % 2
                        mm_group(bank(b), [(wuq[:, m, h * 192:h * 192 + 128], cqT[:, m, :]) for m in range(3)],
                                 reads=kuq + K("cqT", range(3)), writes=[BK(b)])
                        norm_T([bank(b)], [[BK(b)]], 128, [gq + 3], [qnT[:, h, :]], [K("qnT", [h])],
                               [sqb[:, 3 + h % 2, :]], [K("sqb", [3 + h % 2])], rs[:], K("rs"), 5, T)
                        mm_group(bank(b)[0:64, :], [(wuq[:, m, h * 192 + 128:h * 192 + 192], cqT[:, m, :]) for m in range(3)],
                                 reads=kuq + K("cqT", range(3)), writes=[BK(b)])
                        norm_T([bank(b)[0:64, :]], [[BK(b)]], 64, [gq + 4], [qpf[:]], [K("qpf")],
                               [sqb[0:64, 5 + h % 2, :]], [K("sqb", [5 + h % 2])], rs[0:64, :], K("rs"), 5, T)
                        rope_T(qpf[:], K("qpf"), 64, tab[:, 0, :], tab[:, 1, :], K("tab"), t1[:], t2[:],
                               [("t1",), ("t2", 0), ("t2", 1)], qpT[:, h, :], K("qpT", [h]))
                    SB = [0, 1, 6]
                    items = [(h, jb) for h in range(H) for jb in range(nkb)]

                    def geom(jb):
                        d = jb - 4 * t
                        return d, (d * 128 if d > 0 else 0)

                    def emit_S(i):
                        h, jb = items[i]
                        kb = h % 2
                        if jb == 0:
                            P.dma("sp", knS[kb][:, 0:nk], knT_d[h, :, 0:nk], writes=K("knS", [kb]))
                            P.dma("sp", vS[kb][:, 0:nkb, :], vs_d[h, :, 0:nkb, :], writes=K("vS", [kb]))
                        d, c0 = geom(jb)
                        bs = SB[i % 3]
                        ks = slice(jb * 128, (jb + 1) * 128)
                        def fs(e):
                            e.matmul(bank(bs)[:, c0:T], knS[kb][:, ks], qnT[:, h, c0:T], start=True, stop=False)
                            return e.matmul(bank(bs)[:, c0:T], kpS[:, ks], qpT[:, h, c0:T], start=False, stop=True)
                        P.op("pe", fs, reads=K("knS", [kb]) + K("kpS") + K("qnT", [h]) + K("qpT", [h]), writes=[BK(bs)])

                    def emit_rest(i):
                        h, jb = items[i]
                        kb = h % 2
                        d, c0 = geom(jb)
                        bs = SB[i % 3]
                        pt = pT[i % 4]
                        pk = K("pT", [i % 4])
                        bO, bL = 2 + h % 2, 4 + h % 2
                        P.op("act", lambda e: e.activation(out=pt[:, c0:T], in_=bank(bs)[:, c0:T], func=AF.Exp, scale=scale),
                             reads=[BK(bs)], writes=pk)
                        if d >= 0:
                            P.op("pool", lambda e: e.tensor_tensor(out=pt[:, c0:c0 + 128], in0=pt[:, c0:c0 + 128], in1=maskb[:], op=ALU.mult),
                                 reads=pk + K("maskb"), writes=pk)
                        def fpv(e):
                            e.matmul(bank(bO)[:, c0:T], vS[kb][:, jb, :], pt[:, c0:T], start=(jb == 0), stop=(jb == nkb - 1))
                            return e.matmul(bank(bL)[:, c0:T], onesb[:], pt[:, c0:T], start=(jb == 0), stop=(jb == nkb - 1))
                        P.op("pe", fpv, reads=K("vS", [kb]) + pk + K("onesb"), writes=[BK(bO), BK(bL)])
                        if jb == nkb - 1:
                            P.op("dve", lambda e: e.reciprocal(out=rl[:], in_=bank(bL)), reads=[BK(bL)], writes=K("rl"))
                            P.op("dve", lambda e: e.tensor_tensor(out=oT[:, h, :], in0=bank(bO), in1=rl[:], op=ALU.mult),
                                 reads=[BK(bO)] + K("rl"), writes=K("oT", [h]))

                    DEPTH_S = 2
                    for i in range(min(DEPTH_S, len(items))):
                        emit_S(i)
                    for i in range(len(items)):
                        if i + DEPTH_S < len(items):
                            emit_S(i + DEPTH_S)
                        emit_rest(i)
                    for f in range(8):
                        bo = 6 + f % 2
                        fs_ = slice(f * 128, (f + 1) * 128)
                        mm_group(bank(bo), [(wo[:, hh, fs_], oT[:, hh, :]) for hh in range(H)],
                                 reads=ko_ + K("oT", range(8)), writes=[BK(bo)])
                        P.op("dve", lambda e, f=f, bo=bo: e.tensor_tensor(out=xT[:, f, :], in0=bank(bo), in1=xT[:, f, :], op=ALU.add),
                             reads=[BK(bo)] + K("xT", [f]), writes=K("xT", [f]))
                        store_chunk_prefetch(xT, f, t, T, NTI)
                P.barrier()

        stages = [("pro",)]
        for l in range(DEPTH):
            stages.append(("ffn", l, 0))
            stages.append(("ret", l) if l < N_SELF else ("mla", l))
            stages.append(("ffn", l, 1))
            if l == N_SELF - 1:
                stages.append(("kv",))
        if nstages is not None:
            stages = stages[:nstages]
        ffn_idx = [i for i, s in enumerate(stages) if s[0] == "ffn"]
        assert ffn_idx and ffn_idx[-1] == len(stages) - 1, "last stage must be an ffn stage"
        for i, s in enumerate(stages):
            if s[0] == "pro":
                prologue()
            elif s[0] == "ffn":
                ffn_stage(s[1], s[2], first=(i == ffn_idx[0]), last=(i == len(stages) - 1))
            elif s[0] == "ret":
                ret_stage(s[1])
            elif s[0] == "kv":
                kv_stage()
            elif s[0] == "mla":
                mla_stage(s[1])
        P.finish()
        nc._ninst = P.ninst
    return nc


def host_consts():
    c = np.zeros((128, C_NC), np.float32)
    c[:, C_ID:C_ID + 128] = np.eye(128, dtype=np.float32)
    p = np.arange(128)
    c[:, C_MASK:C_MASK + 128] = (p[:, None] <= p[None, :]).astype(np.float32)
    gam = 1.0 - 2.0 ** (-5.0 - np.arange(H, dtype=np.float64))
    c[:, C_KD:C_KD + 8] = (gam[None, :] ** (127 - p)[:, None]) * (RDK ** -0.5)
    c[:, C_EPSR:C_EPSR + 8] = EPS * gam[None, :] ** (2.0 * (127 - p)[:, None])
    c[:, C_INVR] = ROPE_BASE ** (-(2.0 * (p % 64)) / 128.0)
    c[:64, C_INVM] = ROPE_BASE ** (-(2.0 * (p[:64] % 32)) / 64.0)
    c[:, C_SGR] = np.where(p < 64, 1.0, -1.0)
    c[:64, C_SGM] = np.where(p[:64] < 32, 1.0, -1.0)
    return c


def host_gains(inp):
    g = np.zeros((128, G_NC), np.float32)

    def put(col, vec):
        vec = np.asarray(vec, np.float32).reshape(-1)
        n = vec.shape[0]
        if n >= 128:
            k = n // 128
            g[:, col:col + k] = vec.reshape(k, 128).T
        else:
            g[:n, col] = vec
    ng = np.asarray(inp["norm_g"])
    for l in range(DEPTH):
        for i in range(3):
            put(G_NORM + (l * 3 + i) * 8, ng[l, i])
    put(G_KVN, inp["kv_norm_g"])
    put(G_LAT, inp["kv_latent_norm_g"])
    put(G_KN, inp["k_nope_norm_g"])
    put(G_KR, inp["k_rope_norm_g"])
    for j in range(2):
        put(G_QL + j * 5, np.asarray(inp["mla_q_lora_norm_g"])[j])
        put(G_QL + j * 5 + 3, np.asarray(inp["mla_q_nope_norm_g"])[j])
        put(G_QL + j * 5 + 4, np.asarray(inp["mla_q_rope_norm_g"])[j])
    for l in range(N_SELF):
        put(G_GN + l * 16, np.asarray(inp["ret_gn_g"])[l].reshape(-1))
    return g


WKEYS = ["ffn_w_gate", "ffn_w_up", "ffn_w_down", "ret_w_in", "ret_w_o", "kv_w_down", "kv_w_up",
         "mla_w_dq", "mla_w_uq", "mla_w_o"]


def make_in_maps(inp, ncores, NT):
    consts = host_consts()
    gains = host_gains(inp)
    shared = {k: np.ascontiguousarray(np.asarray(inp[k], dtype=np.float32)) for k in WKEYS}
    x = np.asarray(inp["x"], dtype=np.float32)
    pos = np.asarray(inp["positions"]).astype(np.int32)
    maps = []
    for b in range(ncores):
        m = dict(shared)
        m["x"] = np.ascontiguousarray(x[b, :NT])
        m["pos"] = np.ascontiguousarray(pos[b:b + 1, :NT])
        m["consts"] = consts
        m["gains"] = gains
        maps.append(m)
    return maps


def kernel(**inputs):
    nc = build(SEQ)
    maps = make_in_maps(inputs, 8, SEQ)
    res = run_bass_kernel_spmd(nc, maps, core_ids=list(range(8)))
    return np.stack([np.asarray(r["out"], dtype=np.float32) for r in res.results], axis=0)
```

```python
import contextlib
import math
import numpy as np
import concourse.bass as bass
import concourse.mybir as mybir
from concourse.bass_utils import run_bass_kernel_spmd

F32 = mybir.dt.float32
BF16 = mybir.dt.bfloat16
I32 = mybir.dt.int32
AF = mybir.ActivationFunctionType
ALU = mybir.AluOpType
AX = mybir.AxisListType

D = 1024
DFF = 2816
NCH = DFF // 128
DEPTH = 4
N_SELF = 2
H = 8
RDK = 128
RDV = 256
RPROJ = 6144
NOPE = 128
ROPE = 64
QL = 384
KVL = 256
EPS = 1e-6
ROPE_BASE = 10000.0
SEQ = 4096
WSLOTS = 66

C_ID, C_MASK, C_KD, C_EPSR, C_INVR, C_INVM, C_SGR, C_SGM, C_NC = 0, 128, 256, 264, 272, 273, 274, 275, 276
G_NORM = 0
G_KVN = 96
G_LAT = 104
G_KN = 106
G_KR = 107
G_QL = 108
G_GN = 118
G_NC = 150


class Prog:
    NDMA = 8

    def __init__(self, nc, stack):
        self.nc = nc
        self.eng = {"pe": nc.tensor, "act": nc.scalar, "dve": nc.vector, "pool": nc.gpsimd, "sp": nc.sync}
        self.sem, self.cnt = {}, {}
        self.seen = {e: {} for e in self.eng}
        self.lastw, self.readers, self.dq = {}, {}, {}
        self.ninst = 0
        for e in self.eng:
            self.sem[e] = stack.enter_context(nc.semaphore(f"s_{e}"))
            self.cnt[e] = 0
        for q in ("sp", "pool"):
            sems = [stack.enter_context(nc.semaphore(f"d_{q}{i}")) for i in range(self.NDMA)]
            self.dq[q] = {"sems": sems, "cnt": [0] * self.NDMA, "i": 0}

    def _deps(self, e, reads, writes):
        toks = []
        for k in reads:
            w = self.lastw.get(k)
            if w is not None:
                toks.append(w)
        for k in writes:
            w = self.lastw.get(k)
            if w is not None:
                toks.append(w)
            toks.extend(self.readers.get(k, {}).values())
        best = {}
        for (sem, val, src) in toks:
            if src == "pe" and e == "pe":
                continue
            key = id(sem)
            if key not in best or best[key][1] < val:
                best[key] = (sem, val)
        out = []
        seen = self.seen[e]
        for key, (sem, val) in best.items():
            if seen.get(key, 0) >= val:
                continue
            seen[key] = val
            out.append((sem, val))
        return out

    def _commit(self, tok, reads, writes):
        sid = id(tok[0])
        for k in reads:
            self.readers.setdefault(k, {})[sid] = tok
        for k in writes:
            self.lastw[k] = tok
            self.readers[k] = {}

    def op(self, e, fn, reads=(), writes=()):
        eng = self.eng[e]
        for (sem, val) in self._deps(e, reads, writes):
            eng.wait_ge(sem, val)
            self.ninst += 1
        ins = fn(eng)
        self.cnt[e] += 1
        ins.then_inc(self.sem[e], 1)
        tok = (self.sem[e], self.cnt[e], e)
        self._commit(tok, reads, writes)
        self.ninst += 1
        return tok

    def dma(self, q, out, in_, reads=(), writes=()):
        eng = self.eng[q]
        d = self.dq[q]
        i = d["i"]
        d["i"] = (i + 1) % self.NDMA
        sem = d["sems"][i]
        waits = self._deps(q, reads, writes)
        prev = d["cnt"][i]
        if prev > 0 and self.seen[q].get(id(sem), 0) < prev:
            self.seen[q][id(sem)] = prev
            waits = [w for w in waits if w[0] is not sem] + [(sem, prev)]
        for (s, v) in waits:
            eng.wait_ge(s, v)
            self.ninst += 1
        eng.dma_start(out=out, in_=in_).then_inc(sem, 16)
        d["cnt"][i] = prev + 16
        tok = (sem, prev + 16, "dma")
        self._commit(tok, reads, writes)
        self.ninst += 1
        return tok

    def barrier(self, engines=("pe", "act", "dve", "pool", "sp"), dma_queues=("sp",)):
        targets = [(self.sem[e], self.cnt[e]) for e in engines if self.cnt[e] > 0]
        for q in dma_queues:
            d = self.dq[q]
            targets += [(s, c) for s, c in zip(d["sems"], d["cnt"]) if c > 0]
        for e in engines:
            for (s, v) in targets:
                if s is self.sem[e]:
                    continue
                if self.seen[e].get(id(s), 0) >= v:
                    continue
                self.seen[e][id(s)] = v
                self.eng[e].wait_ge(s, v)
                self.ninst += 1

    def finish(self):
        for q in ("sp", "pool"):
            d = self.dq[q]
            for s, c in zip(d["sems"], d["cnt"]):
                if c > 0:
                    self.nc.sync.wait_ge(s, c)


def K(name, idxs=None):
    if idxs is None:
        return [name]
    return [(name, i) for i in idxs]


def pstride(t):
    return t[:].ap[0][0]


_UNIQ = [0]


def uniq(n):
    _UNIQ[0] += 1
    return f"{n}_{_UNIQ[0]}"


def build(NT=SEQ, nstages=None, first_layer_only=False):
    nc = bass.Bass("TRN2", target_bir_lowering=False)
    NB = NT // 128

    def din(name, shape, dt=F32):
        return nc.dram_tensor(name, list(shape), dt, kind="ExternalInput").ap()

    x_in = din("x", [NT, D])
    pos_in = din("pos", [1, NT], I32)
    consts_in = din("consts", [128, C_NC])
    gains_in = din("gains", [128, G_NC])
    w_gate = din("ffn_w_gate", [DEPTH, 2, D, DFF])
    w_up = din("ffn_w_up", [DEPTH, 2, D, DFF])
    w_down = din("ffn_w_down", [DEPTH, 2, DFF, D])
    ret_w_in = din("ret_w_in", [N_SELF, D, RPROJ])
    ret_w_o = din("ret_w_o", [N_SELF, H * RDV, D])
    kv_w_down = din("kv_w_down", [D, KVL + ROPE])
    kv_w_up = din("kv_w_up", [KVL, H * (NOPE + 128)])
    mla_w_dq = din("mla_w_dq", [2, D, QL])
    mla_w_uq = din("mla_w_uq", [2, QL, H * (NOPE + ROPE)])
    mla_w_o = din("mla_w_o", [2, H * 128, D])
    out_d = nc.dram_tensor("out", [NT, D], F32, kind="ExternalOutput").ap()

    def dscr(name, shape, dt):
        return nc.dram_tensor(name, list(shape), dt, kind="Internal").ap()

    xs = dscr("xs", [8, 128, NT], F32)
    tabR = dscr("tabR", [2, 128, NT], F32)
    tabM = dscr("tabM", [2, 64, NT], F32)
    knT_d = dscr("knT", [H, 128, NT], BF16)
    kpeT_d = dscr("kpeT", [64, NT], BF16)
    vs_d = dscr("vs", [H, 128, NB, 128], BF16)

    with contextlib.ExitStack() as top:
        P = Prog(nc, top)
        Wt = top.enter_context(nc.sbuf_tensor("W", [128, WSLOTS, 1024], BF16))
        consts = top.enter_context(nc.sbuf_tensor("consts_sb", [128, C_NC], F32))
        gains = top.enter_context(nc.sbuf_tensor("gains_sb", [128, G_NC], F32))
        identb = top.enter_context(nc.sbuf_tensor("identb", [128, 128], BF16))
        onesb = top.enter_context(nc.sbuf_tensor("onesb", [128, 128], BF16))
        maskb = top.enter_context(nc.sbuf_tensor("maskb", [128, 128], BF16))
        PS = top.enter_context(nc.psum_tensor("PS", [128, 8, 512], F32))
        ident = consts[:, C_ID:C_ID + 128]

        def bank(i):
            return PS[:, i, :]

        def bankb(i):
            return PS[:, i, :].bitcast(BF16)

        def BK(i):
            return ("PS", i)

        P.dma("sp", consts[:], consts_in[:, :], writes=K("consts"))
        P.dma("sp", gains[:], gains_in[:, :], writes=K("gains"))
        P.op("dve", lambda e: e.tensor_copy(out=identb[:], in_=consts[:, C_ID:C_ID + 128]), reads=K("consts"), writes=K("identb"))
        P.op("dve", lambda e: e.tensor_copy(out=maskb[:], in_=consts[:, C_MASK:C_MASK + 128]), reads=K("consts"), writes=K("maskb"))
        P.op("pool", lambda e: e.memset(onesb[:], 1.0), writes=K("onesb"))
        epsc = top.enter_context(nc.sbuf_tensor("epsc", [128, 1], F32))
        P.op("pool", lambda e: e.memset(epsc[:], EPS), writes=K("epsc"))

        def wview(slot0, kch, ncols):
            tot = kch * ncols
            assert tot % 1024 == 0 or True
            nsl = (tot + 1023) // 1024
            assert slot0 + nsl <= WSLOTS
            ps_ = pstride(Wt)
            ap = bass.AP(Wt, slot0 * 1024, [[ps_, 128], [ncols, kch], [1, ncols]])
            return ap, list(range(slot0, slot0 + nsl))

        def wload(src2d, slot0, kch, ncols):
            view, slots = wview(slot0, kch, ncols)
            for k in range(kch):
                s_lo = slot0 + (k * ncols) // 1024
                s_hi = slot0 + ((k + 1) * ncols - 1) // 1024
                P.dma("pool", view[:, k, :], src2d[k * 128:(k + 1) * 128, :],
                      writes=K("W", range(s_lo, s_hi + 1)))
            return view, K("W", slots)

        def mm_group(outap, pairs, reads, writes, first=True, last=True):
            def fn(e):
                ins = None
                n = len(pairs)
                for i, (l, r) in enumerate(pairs):
                    ins = e.matmul(outap, l, r, start=(first and i == 0), stop=(last and i == n - 1))
                return ins
            return P.op("pe", fn, reads=reads, writes=writes)

        def norm_T(srcs, src_keys, npart, gcols, outs, out_keys, sq, sq_keys, rs, rs_key, ssb, Tn, sq_eng="act"):
            n = len(srcs)
            for i in range(n):
                if sq_eng == "act":
                    P.op("act", lambda e, i=i: e.activation(out=sq[i], in_=srcs[i], func=AF.Square),
                         reads=src_keys[i], writes=sq_keys[i])
                else:
                    P.op(sq_eng, lambda e, i=i: e.tensor_tensor(out=sq[i], in0=srcs[i], in1=srcs[i], op=ALU.mult),
                         reads=src_keys[i], writes=sq_keys[i])
            mm_group(bank(ssb)[0:npart, 0:Tn], [(onesb[0:npart, 0:npart], sq[i]) for i in range(n)],
                     reads=K("onesb") + [k for ks in sq_keys for k in ks], writes=[BK(ssb)])
            P.op("act", lambda e: e.activation(out=rs, in_=bank(ssb)[0:npart, 0:Tn], func=AF.Ln,
                                               scale=1.0 / (n * npart), bias=epsc[0:npart, 0:1]),
                 reads=[BK(ssb)] + K("epsc"), writes=rs_key)
            P.op("act", lambda e: e.activation(out=rs, in_=rs, func=AF.Exp, scale=-0.5), reads=rs_key, writes=rs_key)
            for i in range(n):
                P.op("dve", lambda e, i=i: e.scalar_tensor_tensor(
                    out=outs[i], in0=srcs[i], scalar=gains[0:npart, gcols[i]:gcols[i] + 1], in1=rs,
                    op0=ALU.mult, op1=ALU.mult),
                    reads=src_keys[i] + rs_key + K("gains"), writes=out_keys[i])

        def rope_T(src, src_key, npart, cosT, sinT, tab_key, t1, t2, tkeys, out, out_key, eng2="pool"):
            hf = npart // 2
            P.op("dve", lambda e: e.tensor_tensor(out=t1, in0=src, in1=cosT, op=ALU.mult),
                 reads=src_key + tab_key, writes=[tkeys[0]])
            P.op(eng2, lambda e: e.tensor_tensor(out=t2[0:hf], in0=src[hf:npart], in1=sinT[hf:npart], op=ALU.mult),
                 reads=src_key + tab_key, writes=[tkeys[1]])
            P.op(eng2, lambda e: e.tensor_tensor(out=t2[hf:npart], in0=src[0:hf], in1=sinT[0:hf], op=ALU.mult),
                 reads=src_key + tab_key, writes=[tkeys[2]])
            P.op("dve", lambda e: e.tensor_tensor(out=out, in0=t1, in1=t2[0:npart], op=ALU.add),
                 reads=list(tkeys), writes=out_key)

        def prologue():
            CH = min(1024, NT)
            with contextlib.ExitStack() as st:
                sbt = lambda n, s, d=F32: st.enter_context(nc.sbuf_tensor(uniq(n), s, d))
                pi = sbt("pi", [128, CH], I32)
                pf = sbt("pf", [128, CH])
                y = sbt("py", [128, CH])
                ki = sbt("pki", [128, CH], I32)
                kf = sbt("pkf", [128, CH])
                tb = sbt("ptb", [128, 2, CH])
                c1 = 6.28125
                c2 = 2.0 * math.pi - c1
                for c0 in range(0, NT, CH):
                    pos_b = bass.AP(pos_in.tensor, c0, [[0, 128], [1, CH]])
                    P.dma("sp", pi[:], pos_b, writes=K("pi"))
                    P.op("dve", lambda e: e.tensor_copy(out=pf[:], in_=pi[:]), reads=K("pi"), writes=K("pf"))
                    for (npart, invc, sgc, dst) in ((128, C_INVR, C_SGR, tabR), (64, C_INVM, C_SGM, tabM)):
                        for which in (0, 1):
                            shift = math.pi / 2 if which == 0 else 0.0
                            P.op("dve", lambda e: e.tensor_scalar(out=y[0:npart], in0=pf[0:npart], scalar1=consts[0:npart, invc:invc + 1],
                                                                  scalar2=shift, op0=ALU.mult, op1=ALU.add),
                                 reads=K("pf") + K("consts"), writes=K("py"))
                            P.op("dve", lambda e: e.tensor_scalar(out=ki[0:npart], in0=y[0:npart], scalar1=1.0 / (2 * math.pi), scalar2=None, op0=ALU.mult),
                                 reads=K("py"), writes=K("pki"))
                            P.op("dve", lambda e: e.tensor_copy(out=kf[0:npart], in_=ki[0:npart]), reads=K("pki"), writes=K("pkf"))
                            P.op("dve", lambda e: e.scalar_tensor_tensor(out=y[0:npart], in0=kf[0:npart], scalar=-c1, in1=y[0:npart], op0=ALU.mult, op1=ALU.add),
                                 reads=K("pkf") + K("py"), writes=K("py"))
                            P.op("dve", lambda e: e.scalar_tensor_tensor(out=y[0:npart], in0=kf[0:npart], scalar=-c2, in1=y[0:npart], op0=ALU.mult, op1=ALU.add),
                                 reads=K("pkf") + K("py"), writes=K("py"))
                            P.op("dve", lambda e: e.tensor_scalar(out=y[0:npart], in0=y[0:npart], scalar1=-math.pi, scalar2=math.pi, op0=ALU.max, op1=ALU.min),
                                 reads=K("py"), writes=K("py"))
                            if which == 0:
                                P.op("act", lambda e: e.activation(out=tb[0:npart, 0, :], in_=y[0:npart], func=AF.Sin),
                                     reads=K("py"), writes=K("ptb", [0]))
                            else:
                                P.op("act", lambda e: e.activation(out=tb[0:npart, 1, :], in_=y[0:npart], func=AF.Sin,
                                                                   scale=consts[0:npart, sgc:sgc + 1]),
                                     reads=K("py") + K("consts"), writes=K("ptb", [1]))
                        P.dma("sp", dst[:, :, c0:c0 + CH].rearrange("w p t -> p w t"), tb[0:npart, :, :], reads=K("ptb", [0, 1]))
                P.barrier()

        def load_xT(xT, t, T, first, stg):
            if not first:
                P.dma("sp", xT[:], xs[:, :, t * T:(t + 1) * T].rearrange("c p t -> p c t"), writes=K("xT", range(8)))
                return
            for s in range(T // 128):
                sg = stg[s % 2]
                P.dma("sp", sg[:], x_in[t * T + s * 128: t * T + (s + 1) * 128, :], writes=K("stg", [s % 2]))
                for hb in range(2):
                    b = 6 + hb
                    def fn(e, hb=hb, sg=sg, b=b):
                        ins = None
                        for c in range(4):
                            ins = e.transpose(out=bank(b)[:, c * 128:(c + 1) * 128], in_=sg[:, (hb * 4 + c) * 128:(hb * 4 + c + 1) * 128], identity=ident)
                        return ins
                    P.op("pe", fn, reads=K("stg", [s % 2]) + K("consts"), writes=[BK(b)])
                    P.op("act" if hb == 0 else "dve",
                         (lambda e, hb=hb, b=b, s=s: e.activation(out=xT[:, hb * 4:(hb + 1) * 4, s * 128:(s + 1) * 128],
                                                                  in_=bank(b).rearrange("p (c j) -> p c j", c=4), func=AF.Copy)) if hb == 0 else
                         (lambda e, hb=hb, b=b, s=s: e.tensor_copy(out=xT[:, hb * 4:(hb + 1) * 4, s * 128:(s + 1) * 128],
                                                                   in_=bank(b).rearrange("p (c j) -> p c j", c=4))),
                         reads=[BK(b)], writes=K("xT", range(hb * 4, hb * 4 + 4)))

        def store_xT(xT, t, T, last, stg):
            if not last:
                P.dma("sp", xs[:, :, t * T:(t + 1) * T].rearrange("c p t -> p c t"), xT[:], reads=K("xT", range(8)))
                return
            for s in range(T // 128):
                sg = stg[s % 2]
                for hb in range(2):
                    b = 6 + hb
                    def fn(e, hb=hb, b=b, s=s):
                        ins = None
                        for c in range(4):
                            ins = e.transpose(out=bank(b)[:, c * 128:(c + 1) * 128], in_=xT[:, hb * 4 + c, s * 128:(s + 1) * 128], identity=ident)
                        return ins
                    P.op("pe", fn, reads=K("xT", range(hb * 4, hb * 4 + 4)) + K("consts"), writes=[BK(b)])
                    if hb == 0:
                        P.op("act", lambda e, b=b, sg=sg: e.activation(out=sg[:, 0:512], in_=bank(b), func=AF.Copy),
                             reads=[BK(b)], writes=[("stg", s % 2, 0)] + K("stg", [s % 2]))
                    else:
                        P.op("dve", lambda e, b=b, sg=sg: e.tensor_copy(out=sg[:, 512:1024], in_=bank(b)),
                             reads=[BK(b)], writes=[("stg", s % 2, 1)])
                P.dma("sp", out_d[t * T + s * 128: t * T + (s + 1) * 128, :], sg[:],
                      reads=K("stg", [s % 2]) + [("stg", s % 2, 0), ("stg", s % 2, 1)])

        def store_chunk_prefetch(xT, f, t, T, NTI):
            P.dma("sp", xs[f, :, t * T:(t + 1) * T], xT[:, f, :], reads=K("xT", [f]))
            if t + 1 < NTI:
                P.dma("sp", xT[:, f, :], xs[f, :, (t + 1) * T:(t + 2) * T], writes=K("xT", [f]))

        def ffn_stage(layer, which, first, last):
            T = 512
            NTI = NT // T
            gbase = G_NORM + (layer * 3 + (0 if which == 0 else 2)) * 8
            wg, _ = wview(0, 8, DFF)
            wu, _ = wview(22, 8, DFF)
            wd, _ = wview(44, NCH, D)
            CG = [(0, 6), (6, 12), (12, 17), (17, 22)]
            cgrp = {c: gi for gi, (a, b) in enumerate(CG) for c in range(a, b)}
            for gi, (a, b) in enumerate(CG):
                for (wv, src, nm) in ((wg, w_gate, "Wg"), (wu, w_up, "Wu")):
                    for k in range(8):
                        P.dma("pool", wv[:, k, a * 128:b * 128], src[layer, which][k * 128:(k + 1) * 128, a * 128:b * 128],
                              writes=[(nm, gi, k)])
            for c in range(NCH):
                P.dma("pool", wd[:, c, :], w_down[layer, which][c * 128:(c + 1) * 128, :], writes=[("Wd", c)])
            kd = [("Wd", c) for c in range(NCH)]
            with contextlib.ExitStack() as st:
                sbt = lambda n, s, d=F32: st.enter_context(nc.sbuf_tensor(uniq(n), s, d))
                xT = sbt("xT", [128, 8, T])
                hT = sbt("hT", [128, 8, T], BF16)
                aT = sbt("aT", [128, NCH, T], BF16)
                sg = [sbt(f"sg{i}", [128, T]) for i in range(2)]
                rs = sbt("rs", [128, T])
                stg = [sbt(f"stg{i}", [128, D]) for i in range(2)] if (first or last) else None
                for t in range(NTI):
                    if first or t == 0:
                        load_xT(xT, t, T, first, stg)
                    norm_T([xT[:, c, :] for c in range(8)], [K("xT", [c]) for c in range(8)], 128,
                           [gbase + c for c in range(8)],
                           [hT[:, c, :] for c in range(8)], [K("hT", [c]) for c in range(8)],
                           [aT[:, c, :] for c in range(8)], [K("aT", [c]) for c in range(8)],
                           rs[:], K("rs"), 6, T)
                    for c in range(NCH):
                        bg, bu = c % 2, 2 + c % 2
                        cs = slice(c * 128, (c + 1) * 128)
                        mm_group(bank(bg), [(wg[:, k, cs], hT[:, k, :]) for k in range(8)],
                                 reads=[("Wg", cgrp[c], k) for k in range(8)] + K("hT", range(8)), writes=[BK(bg)])
                        mm_group(bank(bu), [(wu[:, k, cs], hT[:, k, :]) for k in range(8)],
                                 reads=[("Wu", cgrp[c], k) for k in range(8)] + K("hT", range(8)), writes=[BK(bu)])
                        P.op("act", lambda e, c=c, bg=bg: e.activation(out=sg[c % 2][:], in_=bank(bg), func=AF.Silu),
                             reads=[BK(bg)], writes=K("sg", [c % 2]))
                        P.op("dve", lambda e, c=c, bu=bu: e.tensor_tensor(out=aT[:, c, :], in0=bank(bu), in1=sg[c % 2][:], op=ALU.mult),
                             reads=[BK(bu)] + K("sg", [c % 2]), writes=K("aT", [c]))
                    for f in range(8):
                        bo = 4 + f % 2
                        fs = slice(f * 128, (f + 1) * 128)
                        mm_group(bank(bo), [(wd[:, c, fs], aT[:, c, :]) for c in range(NCH)],
                                 reads=kd + K("aT", range(NCH)), writes=[BK(bo)])
                        P.op("dve", lambda e, f=f, bo=bo: e.scalar_tensor_tensor(out=xT[:, f, :], in0=bank(bo), scalar=0.5, in1=xT[:, f, :],
                                                                                 op0=ALU.mult, op1=ALU.add),
                             reads=[BK(bo)] + K("xT", [f]), writes=K("xT", [f]))
                        if not last:
                            if first:
                                P.dma("sp", xs[f, :, t * T:(t + 1) * T], xT[:, f, :], reads=K("xT", [f]))
                            else:
                                store_chunk_prefetch(xT, f, t, T, NTI)
                    if last:
                        store_xT(xT, t, T, True, stg)
                        if t + 1 < NTI and not first:
                            load_xT(xT, t + 1, T, False, None)
                P.barrier()

        def ret_stage(layer):
            T = 256
            NTI = NT // T
            gbase = G_NORM + (layer * 3 + 1) * 8
            gam = [1.0 - 2.0 ** (-5.0 - h) for h in range(H)]
            gC = [g ** 128 for g in gam]
            wi, ki_ = wload(ret_w_in[layer], 0, 8, RPROJ)
            wo, ko_ = wload(ret_w_o[layer], 48, 16, D)
            for cc in range(16):
                P.op("dve", lambda e, cc=cc: e.tensor_scalar(out=wo[:, cc, :], in0=wo[:, cc, :],
                                                             scalar1=gains[:, G_GN + layer * 16 + cc:G_GN + layer * 16 + cc + 1],
                                                             scalar2=None, op0=ALU.mult),
                     reads=K("W", [48 + cc]) + K("gains"), writes=K("W", [48 + cc]))
            with contextlib.ExitStack() as st:
                sbt = lambda n, s, d=F32: st.enter_context(nc.sbuf_tensor(uniq(n), s, d))
                xT = sbt("xT", [128, 8, T])
                hT = sbt("hT", [128, 8, T], BF16)
                yT = sbt("yT", [128, 16, T], BF16)
                rs = sbt("rs", [128, T])
                qT = sbt("qT", [128, 8, T], BF16)
                kT = sbt("kT", [128, 8, T], BF16)
                qf = [sbt(f"qf{i}", [128, T]) for i in range(2)]
                t1s = sbt("t1", [128, T])
                t1 = [t1s, t1s]
                t2 = [sbt(f"t2{i}", [128, T]) for i in range(2)]
                tab = sbt("tab", [128, 2, T])
                vd = sbt("vd", [128, H * RDV], BF16)
                sil = sbt("sil", [128, 512])
                gs = sbt("gs", [128, H * RDV], BF16)
                ktok = sbt("ktok", [128, 8, 128], BF16)
                Ssb = sbt("Ssb", [128, 4, 128], BF16)
                state = sbt("state", [128, H, RDV])
                G = sbt("G", [128, H, RDV], BF16)
                sq = sbt("sq", [128, 1024])
                yn = sbt("yn", [128, 1024])
                y = sbt("y", [128, H * RDV], BF16)
                stt = sbt("stt", [128, 8, 4])
                P.op("pool", lambda e: e.memset(state[:], 0.0), writes=K("state", range(8)))
                P.op("pool", lambda e: e.memset(G[:], 0.0), writes=K("G", range(8)))
                ps_c = pstride(consts)
                for t in range(NTI):
                    if t == 0:
                        load_xT(xT, t, T, False, None)
                    P.dma("sp", tab[:], tabR[:, :, t * T:(t + 1) * T].rearrange("w p t -> p w t"), writes=K("tab"))
                    norm_T([xT[:, c, :] for c in range(8)], [K("xT", [c]) for c in range(8)], 128,
                           [gbase + c for c in range(8)],
                           [hT[:, c, :] for c in range(8)], [K("hT", [c]) for c in range(8)],
                           [yT[:, c, :] for c in range(8)], [K("yT", [c]) for c in range(8)],
                           rs[:], K("rs"), 4, T)
                    i = 0
                    for h in range(H):
                        for (dst, dkey, col0) in ((qT, "qT", 0), (kT, "kT", 1024)):
                            b = i % 2
                            i += 1
                            cs = slice(col0 + h * 128, col0 + (h + 1) * 128)
                            mm_group(bank(b)[:, 0:T], [(wi[:, k, cs], hT[:, k, :]) for k in range(8)],
                                     reads=ki_ + K("hT", range(8)), writes=[BK(b)])
                            P.op("act", lambda e, b=b: e.activation(out=qf[b][:], in_=bank(b)[:, 0:T], func=AF.Copy),
                                 reads=[BK(b)], writes=K("qf", [b]))
                            rope_T(qf[b][:], K("qf", [b]), 128, tab[:, 0, :], tab[:, 1, :], K("tab"), t1[b][:], t2[b][:],
                                   [("t1",), ("t2", b, 0), ("t2", b, 1)], dst[:, h, :], K(dkey, [h]))
                    for n in range(T // 128):
                        tk = slice(n * 128, (n + 1) * 128)
                        for grp in range(8):
                            b = 2 + grp % 2
                            col = 2048 + grp * 512
                            mm_group(bank(b), [(hT[:, k, tk], wi[:, k, col:col + 512]) for k in range(8)],
                                     reads=ki_ + K("hT", range(8)), writes=[BK(b)])
                            if grp < 4:
                                kdb = bass.AP(consts, C_KD + grp * 2, [[ps_c, 128], [1, 2], [0, RDV]])
                                P.op("dve", lambda e, b=b, grp=grp, kdb=kdb: e.tensor_tensor(
                                    out=vd[:, grp * 512:(grp + 1) * 512].rearrange("p (a v) -> p a v", a=2),
                                    in0=bank(b).rearrange("p (a v) -> p a v", a=2), in1=kdb, op=ALU.mult),
                                    reads=[BK(b)] + K("consts"), writes=K("vd", [grp]))
                            else:
                                g4 = grp - 4
                                P.op("act", lambda e, b=b, g4=g4: e.activation(out=gs[:, g4 * 512:(g4 + 1) * 512], in_=bank(b), func=AF.Silu),
                                     reads=[BK(b)], writes=K("gs", [g4]))
                        def fnk(e, tk=tk):
                            ins = None
                            for h in range(H):
                                ins = e.transpose(out=bankb(4)[:, h * 128:(h + 1) * 128], in_=kT[:, h, tk], identity=identb[:])
                            return ins
                        P.op("pe", fnk, reads=K("kT", range(8)) + K("identb"), writes=[BK(4)])
                        P.op("act", lambda e: e.activation(out=ktok[:].rearrange("p h d -> p (h d)"), in_=bankb(4), func=AF.Copy),
                             reads=[BK(4)], writes=K("ktok"))
                        for hg in range(2):
                            hs = list(range(hg * 4, hg * 4 + 4))
                            def fns(e, hs=hs, tk=tk):
                                ins = None
                                for j, h in enumerate(hs):
                                    ins = e.matmul(bank(5)[:, j * 128:(j + 1) * 128], kT[:, h, tk], qT[:, h, tk], start=True, stop=True)
                                return ins
                            P.op("pe", fns, reads=K("kT", hs) + K("qT", hs), writes=[BK(5)])
                            mkb = bass.AP(maskb, 0, [[pstride(maskb), 128], [0, 4], [1, 128]])
                            P.op("dve", lambda e, mkb=mkb: e.tensor_tensor(out=Ssb[:], in0=bank(5).rearrange("p (a i) -> p a i", a=4), in1=mkb, op=ALU.mult),
                                 reads=[BK(5)] + K("maskb"), writes=K("Ssb"))
                            def fno(e, hs=hs, tk=tk):
                                ins = None
                                for j, h in enumerate(hs):
                                    o = PS[:, 6 + j // 2, (j % 2) * 256:(j % 2 + 1) * 256]
                                    e.matmul(o, Ssb[:, j, :], vd[:, h * 256:(h + 1) * 256], start=True, stop=False)
                                    ins = e.matmul(o, qT[:, h, tk], G[:, h, :], start=False, stop=True)
                                return ins
                            P.op("pe", fno, reads=K("Ssb") + K("vd", [hg * 2, hg * 2 + 1]) + K("qT", hs) + K("G", hs), writes=[BK(6), BK(7)])
                            def fnu(e, hs=hs):
                                ins = None
                                for j, h in enumerate(hs):
                                    o = PS[:, 2 + j // 2, (j % 2) * 256:(j % 2 + 1) * 256]
                                    ins = e.matmul(o, ktok[:, h, :], vd[:, h * 256:(h + 1) * 256], start=True, stop=True)
                                return ins
                            P.op("pe", fnu, reads=K("ktok") + K("vd", [hg * 2, hg * 2 + 1]), writes=[BK(2), BK(3)])
                            for j, h in enumerate(hs):
                                o = PS[:, 2 + j // 2, (j % 2) * 256:(j % 2 + 1) * 256]
                                P.op("dve", lambda e, h=h, o=o: e.scalar_tensor_tensor(out=state[:, h, :], in0=state[:, h, :], scalar=gC[h], in1=o,
                                                                                       op0=ALU.mult, op1=ALU.add),
                                     reads=[BK(2 + j // 2)] + K("state", [h]), writes=K("state", [h]))
                                P.op("act", lambda e, h=h: e.activation(out=G[:, h, :], in_=state[:, h, :], func=AF.Copy, scale=gC[h]),
                                     reads=K("state", [h]), writes=K("G", [h]))
                            o4 = PS[:, 6:8, :].rearrange("p b (a v) -> p (b a) v", a=2)
                            oflat = PS[:, 6:8, :].rearrange("p b c -> p (b c)")
                            P.op("act", lambda e: e.activation(out=sq[:], in_=oflat, func=AF.Square), reads=[BK(6), BK(7)], writes=K("sq"))
                            P.op("dve", lambda e: e.tensor_reduce(out=stt[:, 0, :], in_=o4, axis=AX.X, op=ALU.add), reads=[BK(6), BK(7)], writes=K("stt", [0]))
                            P.op("dve", lambda e: e.tensor_reduce(out=stt[:, 1, :], in_=sq[:].rearrange("p (a v) -> p a v", a=4), axis=AX.X, op=ALU.add),
                                 reads=K("sq"), writes=K("stt", [1]))
                            P.op("dve", lambda e: e.tensor_scalar(out=stt[:, 2, :], in0=stt[:, 0, :], scalar1=1.0 / RDV, scalar2=None, op0=ALU.mult),
                                 reads=K("stt", [0]), writes=K("stt", [2]))
                            P.op("dve", lambda e: e.tensor_tensor(out=stt[:, 3, :], in0=stt[:, 2, :], in1=stt[:, 2, :], op=ALU.mult),
                                 reads=K("stt", [2]), writes=K("stt", [3]))
                            P.op("dve", lambda e: e.scalar_tensor_tensor(out=stt[:, 3, :], in0=stt[:, 1, :], scalar=1.0 / RDV, in1=stt[:, 3, :],
                                                                         op0=ALU.mult, op1=ALU.subtract),
                                 reads=K("stt", [1, 3]), writes=K("stt", [3]))
                            P.op("dve", lambda e, hg=hg: e.tensor_tensor(out=stt[:, 3, :], in0=stt[:, 3, :], in1=consts[:, C_EPSR + hg * 4:C_EPSR + hg * 4 + 4], op=ALU.add),
                                 reads=K("stt", [3]) + K("consts"), writes=K("stt", [3]))
                            P.op("act", lambda e: e.activation(out=stt[:, 4, :], in_=stt[:, 3, :], func=AF.Sqrt), reads=K("stt", [3]), writes=K("stt", [4]))
                            P.op("dve", lambda e: e.reciprocal(out=stt[:, 4, :], in_=stt[:, 4, :]), reads=K("stt", [4]), writes=K("stt", [4]))
                            P.op("dve", lambda e: e.scalar_tensor_tensor(out=stt[:, 5, :], in0=stt[:, 2, :], scalar=-1.0, in1=stt[:, 4, :],
                                                                         op0=ALU.mult, op1=ALU.mult),
                                 reads=K("stt", [2, 4]), writes=K("stt", [5]))
                            for j, h in enumerate(hs):
                                o = PS[:, 6 + j // 2, (j % 2) * 256:(j % 2 + 1) * 256]
                                P.op("act", lambda e, j=j, o=o: e.activation(out=yn[:, j * 256:(j + 1) * 256], in_=o, func=AF.Identity,
                                                                             scale=stt[:, 4, j:j + 1], bias=stt[:, 5, j:j + 1]),
                                     reads=[BK(6 + j // 2)] + K("stt", [4, 5]), writes=[("yn", j)])
                            P.op("pool", lambda e, hg=hg: e.tensor_tensor(out=y[:, hg * 1024:(hg + 1) * 1024], in0=yn[:], in1=gs[:, hg * 1024:(hg + 1) * 1024], op=ALU.mult),
                                 reads=[("yn", j) for j in range(4)] + K("gs", [hg * 2, hg * 2 + 1]), writes=K("y", [hg]))
                        for hb in range(2):
                            def fny(e, hb=hb):
                                ins = None
                                for c in range(8):
                                    cc = hb * 8 + c
                                    ins = e.transpose(out=bankb(hb)[:, c * 128:(c + 1) * 128], in_=y[:, cc * 128:(cc + 1) * 128], identity=identb[:])
                                return ins
                            P.op("pe", fny, reads=K("y", [hb]) + K("identb"), writes=[BK(hb)])
                            P.op("act" if hb == 0 else "dve",
                                 (lambda e, hb=hb, tk=tk: e.activation(out=yT[:, hb * 8:(hb + 1) * 8, tk], in_=bankb(hb).rearrange("p (c j) -> p c j", c=8), func=AF.Copy)) if hb == 0 else
                                 (lambda e, hb=hb, tk=tk: e.tensor_copy(out=yT[:, hb * 8:(hb + 1) * 8, tk], in_=bankb(hb).rearrange("p (c j) -> p c j", c=8))),
                                 reads=[BK(hb)], writes=K("yT", range(hb * 8, hb * 8 + 8)))
                    for f in range(8):
                        bo = 2 + f % 2
                        fs = slice(f * 128, (f + 1) * 128)
                        mm_group(bank(bo)[:, 0:T], [(wo[:, c, fs], yT[:, c, :]) for c in range(16)],
                                 reads=ko_ + K("yT", range(16)), writes=[BK(bo)])
                        P.op("dve", lambda e, f=f, bo=bo: e.tensor_tensor(out=xT[:, f, :], in0=bank(bo)[:, 0:T], in1=xT[:, f, :], op=ALU.add),
                             reads=[BK(bo)] + K("xT", [f]), writes=K("xT", [f]))
                        store_chunk_prefetch(xT, f, t, T, NTI)
                P.barrier()

        def kv_stage():
            T = 512
            NTI = NT // T
            wdn, kdn = wload(kv_w_down, 0, 8, KVL + ROPE)
            wup, kup = wload(kv_w_up, 4, 2, 2048)
            with contextlib.ExitStack() as st:
                sbt = lambda n, s, d=F32: st.enter_context(nc.sbuf_tensor(uniq(n), s, d))
                xT = sbt("xT", [128, 8, T])
                hT = sbt("hT", [128, 8, T], BF16)
                sqb = sbt("sqb", [128, 8, T], BF16)
                rs = sbt("rs", [128, T])
                cnT = sbt("cnT", [128, 2, T], BF16)
                kpf = sbt("kpf", [64, T])
                t1 = sbt("t1", [128, T])
                t2 = sbt("t2", [128, T])
                tab = sbt("tab", [64, 2, T])
                kpo = sbt("kpo", [64, T], BF16)
                kno = sbt("kno", [128, H, T], BF16)
                vsb = sbt("vsb", [128, 4, 1024], BF16)
                for t in range(NTI):
                    load_xT(xT, t, T, False, None)
                    P.dma("sp", tab[:], tabM[:, :, t * T:(t + 1) * T].rearrange("w p t -> p w t"), writes=K("tab"))
                    norm_T([xT[:, c, :] for c in range(8)], [K("xT", [c]) for c in range(8)], 128,
                           [G_KVN + c for c in range(8)],
                           [hT[:, c, :] for c in range(8)], [K("hT", [c]) for c in range(8)],
                           [sqb[:, c, :] for c in range(8)], [K("sqb", [c]) for c in range(8)],
                           rs[:], K("rs"), 7, T)
                    for m in range(2):
                        mm_group(bank(m), [(wdn[:, k, m * 128:(m + 1) * 128], hT[:, k, :]) for k in range(8)],
                                 reads=kdn + K("hT", range(8)), writes=[BK(m)])
                    mm_group(bank(2)[0:64, :], [(wdn[:, k, 256:320], hT[:, k, :]) for k in range(8)],
                             reads=kdn + K("hT", range(8)), writes=[BK(2)])
                    norm_T([bank(0), bank(1)], [[BK(0)], [BK(1)]], 128, [G_LAT, G_LAT + 1],
                           [cnT[:, 0, :], cnT[:, 1, :]], [K("cnT", [0]), K("cnT", [1])],
                           [sqb[:, 0, :], sqb[:, 1, :]], [K("sqb", [0]), K("sqb", [1])], rs[:], K("rs"), 3, T)
                    norm_T([bank(2)[0:64, :]], [[BK(2)]], 64, [G_KR], [kpf[:]], [K("kpf")],
                           [sqb[0:64, 2, :]], [K("sqb", [2])], rs[0:64, :], K("rs"), 3, T)
                    rope_T(kpf[:], K("kpf"), 64, tab[:, 0, :], tab[:, 1, :], K("tab"), t1[0:64, :], t2[0:64, :],
                           [("t1",), ("t2", 0), ("t2", 1)], kpo[:], K("kpo"))
                    P.dma("sp", kpeT_d[:, t * T:(t + 1) * T], kpo[:], reads=K("kpo"))
                    for h in range(H):
                        b = 4 + h % 2
                        mm_group(bank(b), [(wup[:, m, h * 256:h * 256 + 128], cnT[:, m, :]) for m in range(2)],
                                 reads=kup + K("cnT", [0, 1]), writes=[BK(b)])
                        norm_T([bank(b)], [[BK(b)]], 128, [G_KN], [kno[:, h, :]], [K("kno", [h])],
                               [sqb[:, 3 + h % 2, :]], [K("sqb", [3 + h % 2])], rs[:], K("rs"), 6, T)
                    P.dma("sp", knT_d[:, :, t * T:(t + 1) * T].rearrange("h p t -> p h t"), kno[:], reads=K("kno", range(8)))
                    psw = pstride(Wt)
                    for s in range(4):
                        for half in range(2):
                            b = half
                            pairs = []
                            for m in range(2):
                                rhs = bass.AP(Wt, 4 * 1024 + m * 2048 + half * 1024 + 128, [[psw, 128], [256, 4], [1, 128]])
                                pairs.append((cnT[:, m, s * 128:(s + 1) * 128], rhs))
                            mm_group(bank(b), pairs, reads=kup + K("cnT", [0, 1]), writes=[BK(b)])
                            P.op("act", lambda e, s=s, half=half, b=b: e.activation(out=vsb[:, s, half * 512:(half + 1) * 512], in_=bank(b), func=AF.Copy),
                                 reads=[BK(b)], writes=K("vsb", [s]))
                    for h in range(H):
                        P.dma("sp", vs_d[h, :, t * 4:(t + 1) * 4, :], vsb[:, :, h * 128:(h + 1) * 128], reads=K("vsb", range(4)))
                P.barrier()

        def mla_stage(layer):
            T = 512
            NTI = NT // T
            j_ = layer - N_SELF
            gbase = G_NORM + (layer * 3 + 1) * 8
            gq = G_QL + j_ * 5
            scale = (NOPE + ROPE) ** -0.5
            wdq, kdq = wload(mla_w_dq[j_], 0, 8, QL)
            wuq, kuq = wload(mla_w_uq[j_], 3, 3, 1536)
            wo, ko_ = wload(mla_w_o[j_], 8, 8, D)
            with contextlib.ExitStack() as st:
                sbt = lambda n, s, d=F32: st.enter_context(nc.sbuf_tensor(uniq(n), s, d))
                def aview(slot0, nslots):
                    return Wt[:, slot0:slot0 + nslots, :].rearrange("p s c -> p (s c)")
                xT = sbt("xT", [128, 8, T])
                rs = sbt("rs", [128, T])
                cqf = sbt("cqf", [128, 3, T])
                qnf = [sbt(f"qnf{i}", [128, T]) for i in range(2)]
                qpf = [sbt(f"qpf{i}", [64, T]) for i in range(2)]
                rsn = [sbt(f"rsn{i}", [128, T]) for i in range(2)]
                rsp = [sbt(f"rsp{i}", [64, T]) for i in range(2)]
                t1 = [sbt(f"t1{i}", [64, T]) for i in range(2)]
                t2 = [sbt(f"t2{i}", [64, T]) for i in range(2)]
                tab = sbt("tab", [64, 2, T])
                hT = aview(16, 4).rearrange("p (c t) -> p c t", c=8)
                sqb = aview(20, 4).rearrange("p (c t) -> p c t", c=8)
                qnT = aview(24, 4).rearrange("p (c t) -> p c t", c=8)
                qpT = aview(28, 4).rearrange("p (c t) -> p c t", c=8)
                oT = aview(32, 4).rearrange("p (c t) -> p c t", c=8)
                cqT = aview(36, 2)[:, 0:3 * T].rearrange("p (c t) -> p c t", c=3)
                pTall = aview(38, 2).rearrange("p (c t) -> p c t", c=4)
                pT = [pTall[:, i, :] for i in range(4)]
                knS = [aview(40 + 4 * i, 4) for i in range(2)]
                vS = [aview(48 + 4 * i, 4).rearrange("p (b d) -> p b d", d=128) for i in range(2)]
                kpS = aview(56, 4)
                P.op("pool", lambda e: e.memset(kpS[64:128, :], 0.0), writes=K("kpS"))
                P.op("pool", lambda e: e.memset(qpT[64:128, :, :], 0.0), writes=K("qpT", range(8)))
                rl = sbt("rl", [128, T])
                for t in range(NTI):
                    nk = (t + 1) * T
                    nkb = nk // 128
                    if t == 0:
                        load_xT(xT, t, T, False, None)
                    P.dma("sp", tab[:], tabM[:, :, t * T:(t + 1) * T].rearrange("w p t -> p w t"), writes=K("tab"))
                    P.dma("sp", kpS[0:64, 0:nk], kpeT_d[:, 0:nk], writes=K("kpS"))
                    norm_T([xT[:, c, :] for c in range(8)], [K("xT", [c]) for c in range(8)], 128,
                           [gbase + c for c in range(8)],
                           [hT[:, c, :] for c in range(8)], [K("hT", [c]) for c in range(8)],
                           [sqb[:, c, :] for c in range(8)], [K("sqb", [c]) for c in range(8)],
                           rs[:], K("rs"), 6, T)
                    for m in range(3):
                        b = 6 + m % 2
                        mm_group(bank(b), [(wdq[:, k, m * 128:(m + 1) * 128], hT[:, k, :]) for k in range(8)],
                                 reads=kdq + K("hT", range(8)), writes=[BK(b)])
                        P.op("act", lambda e, m=m, b=b: e.activation(out=cqf[:, m, :], in_=bank(b), func=AF.Copy),
                             reads=[BK(b)], writes=K("cqf", [m]))
                    norm_T([cqf[:, m, :] for m in range(3)], [K("cqf", [m]) for m in range(3)], 128, [gq, gq + 1, gq + 2],
                           [cqT[:, m, :] for m in range(3)], [K("cqT", [m]) for m in range(3)],
                           [sqb[:, m, :] for m in range(3)], [K("sqb", [m]) for m in range(3)], rs[:], K("rs"), 6, T)
                    for h in range(H):
                        r = h % 2
                        mm_group(bank(7), [(wuq[:, m, h * 192:h * 192 + 128], cqT[:, m, :]) for m in range(3)],
                                 reads=kuq + K("cqT", range(3)), writes=[BK(7)])
                        P.op("dve", lambda e, r=r: e.tensor_copy(out=qnf[r][:], in_=bank(7)), reads=[BK(7)], writes=K("qnf", [r]))
                        mm_group(bank(6)[0:64, :], [(wuq[:, m, h * 192 + 128:h * 192 + 192], cqT[:, m, :]) for m in range(3)],
                                 reads=kuq + K("cqT", range(3)), writes=[BK(6)])
                        P.op("act", lambda e, r=r: e.activation(out=qpf[r][:], in_=bank(6)[0:64, :], func=AF.Copy), reads=[BK(6)], writes=K("qpf", [r]))
                        norm_T([qnf[r][:]], [K("qnf", [r])], 128, [gq + 3], [qnT[:, h, :]], [K("qnT", [h])],
                               [sqb[:, 3 + r, :]], [K("sqb", [3 + r])], rsn[r][:], K("rsn", [r]), 5, T, sq_eng="pool")
                        norm_T([qpf[r][:]], [K("qpf", [r])], 64, [gq + 4], [qpf[r][:]], [K("qpf", [r])],
                               [sqb[0:64, 5 + r, :]], [K("sqb", [5 + r])], rsp[r][:], K("rsp", [r]), 4, T, sq_eng="pool")
                        rope_T(qpf[r][:], K("qpf", [r]), 64, tab[:, 0, :], tab[:, 1, :], K("tab"), t1[r][:], t2[r][:],
                               [("t1", r), ("t2", r, 0), ("t2", r, 1)], qpT[0:64, h, :], K("qpT", [h]))
                    SB = [0, 1, 6]
                    items = [(h, jb) for h in range(H) for jb in range(nkb)]

                    def geom(jb):
                        d = jb - 4 * t
                        return d, (d * 128 if d > 0 else 0)

                    def emit_S(i):
                        h, jb = items[i]
                        kb = h % 2
                        if jb == 0:
                            P.dma("sp", knS[kb][:, 0:nk], knT_d[h, :, 0:nk], writes=K("knS", [kb]))
                            P.dma("sp", vS[kb][:, 0:nkb, :], vs_d[h, :, 0:nkb, :], writes=K("vS", [kb]))
                        d, c0 = geom(jb)
                        bs = SB[i % 3]
                        ks = slice(jb * 128, (jb + 1) * 128)
                        def fs(e):
                            e.matmul(bank(bs)[:, c0:T], knS[kb][:, ks], qnT[:, h, c0:T], start=True, stop=False)
                            return e.matmul(bank(bs)[:, c0:T], kpS[:, ks], qpT[:, h, c0:T], start=False, stop=True)
                        P.op("pe", fs, reads=K("knS", [kb]) + K("kpS") + K("qnT", [h]) + K("qpT", [h]), writes=[BK(bs)])

                    def emit_rest(i):
                        h, jb = items[i]
                        kb = h % 2
                        d, c0 = geom(jb)
                        bs = SB[i % 3]
                        pt = pT[i % 4]
                        pk = K("pT", [i % 4])
                        bO, bL = 2 + h % 2, 4 + h % 2
                        P.op("act", lambda e: e.activation(out=pt[:, c0:T], in_=bank(bs)[:, c0:T], func=AF.Exp, scale=scale),
                             reads=[BK(bs)], writes=pk)
                        if d >= 0:
                            P.op("pool", lambda e: e.tensor_tensor(out=pt[:, c0:c0 + 128], in0=pt[:, c0:c0 + 128], in1=maskb[:], op=ALU.mult),
                                 reads=pk + K("maskb"), writes=pk)
                        def fpv(e):
                            e.matmul(bank(bO)[:, c0:T], vS[kb][:, jb, :], pt[:, c0:T], start=(jb == 0), stop=(jb == nkb - 1))
                            return e.matmul(bank(bL)[:, c0:T], onesb[:], pt[:, c0:T], start=(jb == 0), stop=(jb == nkb - 1))
                        P.op("pe", fpv, reads=K("vS", [kb]) + pk + K("onesb"), writes=[BK(bO), BK(bL)])
                        if jb == nkb - 1:
                            P.op("dve", lambda e: e.reciprocal(out=rl[:], in_=bank(bL)), reads=[BK(bL)], writes=K("rl"))
                            P.op("dve", lambda e: e.tensor_tensor(out=oT[:, h, :], in0=bank(bO), in1=rl[:], op=ALU.mult),
                                 reads=[BK(bO)] + K("rl"), writes=K("oT", [h]))

                    DEPTH_S = 2
                    for i in range(min(DEPTH_S, len(items))):
                        emit_S(i)
                    for i in range(len(items)):
                        if i + DEPTH_S < len(items):
                            emit_S(i + DEPTH_S)
                        emit_rest(i)
                    for f in range(8):
                        bo = 6 + f % 2
                        fs_ = slice(f * 128, (f + 1) * 128)
                        mm_group(bank(bo), [(wo[:, hh, fs_], oT[:, hh, :]) for hh in range(H)],
                                 reads=ko_ + K("oT", range(8)), writes=[BK(bo)])
                        P.op("dve", lambda e, f=f, bo=bo: e.tensor_tensor(out=xT[:, f, :], in0=bank(bo), in1=xT[:, f, :], op=ALU.add),
                             reads=[BK(bo)] + K("xT", [f]), writes=K("xT", [f]))
                        store_chunk_prefetch(xT, f, t, T, NTI)
                P.barrier()

        stages = [("pro",)]
        for l in range(DEPTH):
            stages.append(("ffn", l, 0))
            stages.append(("ret", l) if l < N_SELF else ("mla", l))
            stages.append(("ffn", l, 1))
            if l == N_SELF - 1:
                stages.append(("kv",))
        if nstages is not None:
            stages = stages[:nstages]
        ffn_idx = [i for i, s in enumerate(stages) if s[0] == "ffn"]
        assert ffn_idx and ffn_idx[-1] == len(stages) - 1, "last stage must be an ffn stage"
        for i, s in enumerate(stages):
            if s[0] == "pro":
                prologue()
            elif s[0] == "ffn":
                ffn_stage(s[1], s[2], first=(i == ffn_idx[0]), last=(i == len(stages) - 1))
            elif s[0] == "ret":
                ret_stage(s[1])
            elif s[0] == "kv":
                kv_stage()
            elif s[0] == "mla":
                mla_stage(s[1])
        P.finish()
        nc._ninst = P.ninst
    return nc


def host_consts():
    c = np.zeros((128, C_NC), np.float32)
    c[:, C_ID:C_ID + 128] = np.eye(128, dtype=np.float32)
    p = np.arange(128)
    c[:, C_MASK:C_MASK + 128] = (p[:, None] <= p[None, :]).astype(np.float32)
    gam = 1.0 - 2.0 ** (-5.0 - np.arange(H, dtype=np.float64))
    c[:, C_KD:C_KD + 8] = (gam[None, :] ** (127 - p)[:, None]) * (RDK ** -0.5)
    c[:, C_EPSR:C_EPSR + 8] = EPS * gam[None, :] ** (2.0 * (127 - p)[:, None])
    c[:, C_INVR] = ROPE_BASE ** (-(2.0 * (p % 64)) / 128.0)
    c[:64, C_INVM] = ROPE_BASE ** (-(2.0 * (p[:64] % 32)) / 64.0)
    c[:, C_SGR] = np.where(p < 64, 1.0, -1.0)
    c[:64, C_SGM] = np.where(p[:64] < 32, 1.0, -1.0)
    return c


def host_gains(inp):
    g = np.zeros((128, G_NC), np.float32)

    def put(col, vec):
        vec = np.asarray(vec, np.float32).reshape(-1)
        n = vec.shape[0]
        if n >= 128:
            k = n // 128
            g[:, col:col + k] = vec.reshape(k, 128).T
        else:
            g[:n, col] = vec
    ng = np.asarray(inp["norm_g"])
    for l in range(DEPTH):
        for i in range(3):
            put(G_NORM + (l * 3 + i) * 8, ng[l, i])
    put(G_KVN, inp["kv_norm_g"])
    put(G_LAT, inp["kv_latent_norm_g"])
    put(G_KN, inp["k_nope_norm_g"])
    put(G_KR, inp["k_rope_norm_g"])
    for j in range(2):
        put(G_QL + j * 5, np.asarray(inp["mla_q_lora_norm_g"])[j])
        put(G_QL + j * 5 + 3, np.asarray(inp["mla_q_nope_norm_g"])[j])
        put(G_QL + j * 5 + 4, np.asarray(inp["mla_q_rope_norm_g"])[j])
    for l in range(N_SELF):
        put(G_GN + l * 16, np.asarray(inp["ret_gn_g"])[l].reshape(-1))
    return g


WKEYS = ["ffn_w_gate", "ffn_w_up", "ffn_w_down", "ret_w_in", "ret_w_o", "kv_w_down", "kv_w_up",
         "mla_w_dq", "mla_w_uq", "mla_w_o"]


def make_in_maps(inp, ncores, NT):
    consts = host_consts()
    gains = host_gains(inp)
    shared = {k: np.ascontiguousarray(np.asarray(inp[k], dtype=np.float32)) for k in WKEYS}
    x = np.asarray(inp["x"], dtype=np.float32)
    pos = np.asarray(inp["positions"]).astype(np.int32)
    maps = []
    for b in range(ncores):
        m = dict(shared)
        m["x"] = np.ascontiguousarray(x[b, :NT])
        m["pos"] = np.ascontiguousarray(pos[b:b + 1, :NT])
        m["consts"] = consts
        m["gains"] = gains
        maps.append(m)
    return maps


def kernel(**inputs):
    nc = build(SEQ)
    maps = make_in_maps(inputs, 8, SEQ)
    res = run_bass_kernel_spmd(nc, maps, core_ids=list(range(8)))
    return np.stack([np.asarray(r["out"], dtype=np.float32) for r in res.results], axis=0)
```

```python
import contextlib
import math
import numpy as np
import concourse.bass as bass
import concourse.mybir as mybir
from concourse.bass_utils import run_bass_kernel_spmd

F32 = mybir.dt.float32
BF16 = mybir.dt.bfloat16
I32 = mybir.dt.int32
AF = mybir.ActivationFunctionType
ALU = mybir.AluOpType
AX = mybir.AxisListType

D = 1024
DFF = 2816
NCH = DFF // 128
DEPTH = 4
N_SELF = 2
H = 8
RDK = 128
RDV = 256
RPROJ = 6144
NOPE = 128
ROPE = 64
QL = 384
KVL = 256
EPS = 1e-6
ROPE_BASE = 10000.0
SEQ = 4096
WSLOTS = 66

C_ID, C_MASK, C_KD, C_EPSR, C_INVR, C_INVM, C_SGR, C_SGM, C_NC = 0, 128, 256, 264, 272, 273, 274, 275, 276
G_NORM = 0
G_KVN = 96
G_LAT = 104
G_KN = 106
G_KR = 107
G_QL = 108
G_GN = 118
G_NC = 150


class Prog:
    NDMA = 8

    def __init__(self, nc, stack):
        self.nc = nc
        self.eng = {"pe": nc.tensor, "act": nc.scalar, "dve": nc.vector, "pool": nc.gpsimd, "sp": nc.sync}
        self.sem, self.cnt = {}, {}
        self.seen = {e: {} for e in self.eng}
        self.lastw, self.readers, self.dq = {}, {}, {}
        self.ninst = 0
        for e in self.eng:
            self.sem[e] = stack.enter_context(nc.semaphore(f"s_{e}"))
            self.cnt[e] = 0
        for q in ("sp", "pool"):
            sems = [stack.enter_context(nc.semaphore(f"d_{q}{i}")) for i in range(self.NDMA)]
            self.dq[q] = {"sems": sems, "cnt": [0] * self.NDMA, "i": 0}

    def _deps(self, e, reads, writes):
        toks = []
        for k in reads:
            w = self.lastw.get(k)
            if w is not None:
                toks.append(w)
        for k in writes:
            w = self.lastw.get(k)
            if w is not None:
                toks.append(w)
            toks.extend(self.readers.get(k, {}).values())
        best = {}
        for (sem, val, src) in toks:
            if src == "pe" and e == "pe":
                continue
            key = id(sem)
            if key not in best or best[key][1] < val:
                best[key] = (sem, val)
        out = []
        seen = self.seen[e]
        for key, (sem, val) in best.items():
            if seen.get(key, 0) >= val:
                continue
            seen[key] = val
            out.append((sem, val))
        return out

    def _commit(self, tok, reads, writes):
        sid = id(tok[0])
        for k in reads:
            self.readers.setdefault(k, {})[sid] = tok
        for k in writes:
            self.lastw[k] = tok
            self.readers[k] = {}

    def op(self, e, fn, reads=(), writes=()):
        eng = self.eng[e]
        for (sem, val) in self._deps(e, reads, writes):
            eng.wait_ge(sem, val)
            self.ninst += 1
        ins = fn(eng)
        self.cnt[e] += 1
        ins.then_inc(self.sem[e], 1)
        tok = (self.sem[e], self.cnt[e], e)
        self._commit(tok, reads, writes)
        self.ninst += 1
        return tok

    def dma(self, q, out, in_, reads=(), writes=()):
        eng = self.eng[q]
        d = self.dq[q]
        i = d["i"]
        d["i"] = (i + 1) % self.NDMA
        sem = d["sems"][i]
        waits = self._deps(q, reads, writes)
        prev = d["cnt"][i]
        if prev > 0 and self.seen[q].get(id(sem), 0) < prev:
            self.seen[q][id(sem)] = prev
            waits = [w for w in waits if w[0] is not sem] + [(sem, prev)]
        for (s, v) in waits:
            eng.wait_ge(s, v)
            self.ninst += 1
        eng.dma_start(out=out, in_=in_).then_inc(sem, 16)
        d["cnt"][i] = prev + 16
        tok = (sem, prev + 16, "dma")
        self._commit(tok, reads, writes)
        self.ninst += 1
        return tok

    def barrier(self, engines=("pe", "act", "dve", "pool", "sp"), dma_queues=("sp",)):
        targets = [(self.sem[e], self.cnt[e]) for e in engines if self.cnt[e] > 0]
        for q in dma_queues:
            d = self.dq[q]
            targets += [(s, c) for s, c in zip(d["sems"], d["cnt"]) if c > 0]
        for e in engines:
            for (s, v) in targets:
                if s is self.sem[e]:
                    continue
                if self.seen[e].get(id(s), 0) >= v:
                    continue
                self.seen[e][id(s)] = v
                self.eng[e].wait_ge(s, v)
                self.ninst += 1

    def finish(self):
        for q in ("sp", "pool"):
            d = self.dq[q]
            for s, c in zip(d["sems"], d["cnt"]):
                if c > 0:
                    self.nc.sync.wait_ge(s, c)


def K(name, idxs=None):
    if idxs is None:
        return [name]
    return [(name, i) for i in idxs]


def pstride(t):
    return t[:].ap[0][0]


_UNIQ = [0]


def uniq(n):
    _UNIQ[0] += 1
    return f"{n}_{_UNIQ[0]}"


def build(NT=SEQ, nstages=None, first_layer_only=False):
    nc = bass.Bass("TRN2", target_bir_lowering=False)
    NB = NT // 128

    def din(name, shape, dt=F32):
        return nc.dram_tensor(name, list(shape), dt, kind="ExternalInput").ap()

    x_in = din("x", [NT, D])
    pos_in = din("pos", [1, NT], I32)
    consts_in = din("consts", [128, C_NC])
    gains_in = din("gains", [128, G_NC])
    w_gate = din("ffn_w_gate", [DEPTH, 2, D, DFF])
    w_up = din("ffn_w_up", [DEPTH, 2, D, DFF])
    w_down = din("ffn_w_down", [DEPTH, 2, DFF, D])
    ret_w_in = din("ret_w_in", [N_SELF, D, RPROJ])
    ret_w_o = din("ret_w_o", [N_SELF, H * RDV, D])
    kv_w_down = din("kv_w_down", [D, KVL + ROPE])
    kv_w_up = din("kv_w_up", [KVL, H * (NOPE + 128)])
    mla_w_dq = din("mla_w_dq", [2, D, QL])
    mla_w_uq = din("mla_w_uq", [2, QL, H * (NOPE + ROPE)])
    mla_w_o = din("mla_w_o", [2, H * 128, D])
    out_d = nc.dram_tensor("out", [NT, D], F32, kind="ExternalOutput").ap()

    def dscr(name, shape, dt):
        return nc.dram_tensor(name, list(shape), dt, kind="Internal").ap()

    xs = dscr("xs", [8, 128, NT], F32)
    tabR = dscr("tabR", [2, 128, NT], F32)
    tabM = dscr("tabM", [2, 64, NT], F32)
    knT_d = dscr("knT", [H, 128, NT], BF16)
    kpeT_d = dscr("kpeT", [64, NT], BF16)
    vs_d = dscr("vs", [H, 128, NB, 128], BF16)

    with contextlib.ExitStack() as top:
        P = Prog(nc, top)
        Wt = top.enter_context(nc.sbuf_tensor("W", [128, WSLOTS, 1024], BF16))
        consts = top.enter_context(nc.sbuf_tensor("consts_sb", [128, C_NC], F32))
        gains = top.enter_context(nc.sbuf_tensor("gains_sb", [128, G_NC], F32))
        identb = top.enter_context(nc.sbuf_tensor("identb", [128, 128], BF16))
        onesb = top.enter_context(nc.sbuf_tensor("onesb", [128, 128], BF16))
        maskb = top.enter_context(nc.sbuf_tensor("maskb", [128, 128], BF16))
        PS = top.enter_context(nc.psum_tensor("PS", [128, 8, 512], F32))
        ident = consts[:, C_ID:C_ID + 128]

        def bank(i):
            return PS[:, i, :]

        def bankb(i):
            return PS[:, i, :].bitcast(BF16)

        def BK(i):
            return ("PS", i)

        P.dma("sp", consts[:], consts_in[:, :], writes=K("consts"))
        P.dma("sp", gains[:], gains_in[:, :], writes=K("gains"))
        P.op("dve", lambda e: e.tensor_copy(out=identb[:], in_=consts[:, C_ID:C_ID + 128]), reads=K("consts"), writes=K("identb"))
        P.op("dve", lambda e: e.tensor_copy(out=maskb[:], in_=consts[:, C_MASK:C_MASK + 128]), reads=K("consts"), writes=K("maskb"))
        P.op("pool", lambda e: e.memset(onesb[:], 1.0), writes=K("onesb"))
        epsc = top.enter_context(nc.sbuf_tensor("epsc", [128, 1], F32))
        P.op("pool", lambda e: e.memset(epsc[:], EPS), writes=K("epsc"))

        def wview(slot0, kch, ncols):
            tot = kch * ncols
            assert tot % 1024 == 0 or True
            nsl = (tot + 1023) // 1024
            assert slot0 + nsl <= WSLOTS
            ps_ = pstride(Wt)
            ap = bass.AP(Wt, slot0 * 1024, [[ps_, 128], [ncols, kch], [1, ncols]])
            return ap, list(range(slot0, slot0 + nsl))

        def wload(src2d, slot0, kch, ncols):
            view, slots = wview(slot0, kch, ncols)
            for k in range(kch):
                s_lo = slot0 + (k * ncols) // 1024
                s_hi = slot0 + ((k + 1) * ncols - 1) // 1024
                P.dma("pool", view[:, k, :], src2d[k * 128:(k + 1) * 128, :],
                      writes=K("W", range(s_lo, s_hi + 1)))
            return view, K("W", slots)

        def mm_group(outap, pairs, reads, writes, first=True, last=True):
            def fn(e):
                ins = None
                n = len(pairs)
                for i, (l, r) in enumerate(pairs):
                    ins = e.matmul(outap, l, r, start=(first and i == 0), stop=(last and i == n - 1))
                return ins
            return P.op("pe", fn, reads=reads, writes=writes)

        def norm_T(srcs, src_keys, npart, gcols, outs, out_keys, sq, sq_keys, rs, rs_key, ssb, Tn, sq_eng="act"):
            n = len(srcs)
            for i in range(n):
                if sq_eng == "act":
                    P.op("act", lambda e, i=i: e.activation(out=sq[i], in_=srcs[i], func=AF.Square),
                         reads=src_keys[i], writes=sq_keys[i])
                else:
                    P.op(sq_eng, lambda e, i=i: e.tensor_tensor(out=sq[i], in0=srcs[i], in1=srcs[i], op=ALU.mult),
                         reads=src_keys[i], writes=sq_keys[i])
            mm_group(bank(ssb)[0:npart, 0:Tn], [(onesb[0:npart, 0:npart], sq[i]) for i in range(n)],
                     reads=K("onesb") + [k for ks in sq_keys for k in ks], writes=[BK(ssb)])
            P.op("act", lambda e: e.activation(out=rs, in_=bank(ssb)[0:npart, 0:Tn], func=AF.Ln,
                                               scale=1.0 / (n * npart), bias=epsc[0:npart, 0:1]),
                 reads=[BK(ssb)] + K("epsc"), writes=rs_key)
            P.op("act", lambda e: e.activation(out=rs, in_=rs, func=AF.Exp, scale=-0.5), reads=rs_key, writes=rs_key)
            for i in range(n):
                P.op("dve", lambda e, i=i: e.scalar_tensor_tensor(
                    out=outs[i], in0=srcs[i], scalar=gains[0:npart, gcols[i]:gcols[i] + 1], in1=rs,
                    op0=ALU.mult, op1=ALU.mult),
                    reads=src_keys[i] + rs_key + K("gains"), writes=out_keys[i])

        def rope_T(src, src_key, npart, cosT, sinT, tab_key, t1, t2, tkeys, out, out_key, eng2="pool"):
            hf = npart // 2
            P.op("dve", lambda e: e.tensor_tensor(out=t1, in0=src, in1=cosT, op=ALU.mult),
                 reads=src_key + tab_key, writes=[tkeys[0]])
            P.op(eng2, lambda e: e.tensor_tensor(out=t2[0:hf], in0=src[hf:npart], in1=sinT[hf:npart], op=ALU.mult),
                 reads=src_key + tab_key, writes=[tkeys[1]])
            P.op(eng2, lambda e: e.tensor_tensor(out=t2[hf:npart], in0=src[0:hf], in1=sinT[0:hf], op=ALU.mult),
                 reads=src_key + tab_key, writes=[tkeys[2]])
            P.op("dve", lambda e: e.tensor_tensor(out=out, in0=t1, in1=t2[0:npart], op=ALU.add),
                 reads=list(tkeys), writes=out_key)

        def prologue():
            CH = min(1024, NT)
            with contextlib.ExitStack() as st:
                sbt = lambda n, s, d=F32: st.enter_context(nc.sbuf_tensor(uniq(n), s, d))
                pi = sbt("pi", [128, CH], I32)
                pf = sbt("pf", [128, CH])
                y = sbt("py", [128, CH])
                ki = sbt("pki", [128, CH], I32)
                kf = sbt("pkf", [128, CH])
                tb = sbt("ptb", [128, 2, CH])
                c1 = 6.28125
                c2 = 2.0 * math.pi - c1
                for c0 in range(0, NT, CH):
                    pos_b = bass.AP(pos_in.tensor, c0, [[0, 128], [1, CH]])
                    P.dma("sp", pi[:], pos_b, writes=K("pi"))
                    P.op("dve", lambda e: e.tensor_copy(out=pf[:], in_=pi[:]), reads=K("pi"), writes=K("pf"))
                    for (npart, invc, sgc, dst) in ((128, C_INVR, C_SGR, tabR), (64, C_INVM, C_SGM, tabM)):
                        for which in (0, 1):
                            shift = math.pi / 2 if which == 0 else 0.0
                            P.op("dve", lambda e: e.tensor_scalar(out=y[0:npart], in0=pf[0:npart], scalar1=consts[0:npart, invc:invc + 1],
                                                                  scalar2=shift, op0=ALU.mult, op1=ALU.add),
                                 reads=K("pf") + K("consts"), writes=K("py"))
                            P.op("dve", lambda e: e.tensor_scalar(out=ki[0:npart], in0=y[0:npart], scalar1=1.0 / (2 * math.pi), scalar2=None, op0=ALU.mult),
                                 reads=K("py"), writes=K("pki"))
                            P.op("dve", lambda e: e.tensor_copy(out=kf[0:npart], in_=ki[0:npart]), reads=K("pki"), writes=K("pkf"))
                            P.op("dve", lambda e: e.scalar_tensor_tensor(out=y[0:npart], in0=kf[0:npart], scalar=-c1, in1=y[0:npart], op0=ALU.mult, op1=ALU.add),
                                 reads=K("pkf") + K("py"), writes=K("py"))
                            P.op("dve", lambda e: e.scalar_tensor_tensor(out=y[0:npart], in0=kf[0:npart], scalar=-c2, in1=y[0:npart], op0=ALU.mult, op1=ALU.add),
                                 reads=K("pkf") + K("py"), writes=K("py"))
                            P.op("dve", lambda e: e.tensor_scalar(out=y[0:npart], in0=y[0:npart], scalar1=-math.pi, scalar2=math.pi, op0=ALU.max, op1=ALU.min),
                                 reads=K("py"), writes=K("py"))
                            if which == 0:
                                P.op("act", lambda e: e.activation(out=tb[0:npart, 0, :], in_=y[0:npart], func=AF.Sin),
                                     reads=K("py"), writes=K("ptb", [0]))
                            else:
                                P.op("act", lambda e: e.activation(out=tb[0:npart, 1, :], in_=y[0:npart], func=AF.Sin,
                                                                   scale=consts[0:npart, sgc:sgc + 1]),
                                     reads=K("py") + K("consts"), writes=K("ptb", [1]))
                        P.dma("sp", dst[:, :, c0:c0 + CH].rearrange("w p t -> p w t"), tb[0:npart, :, :], reads=K("ptb", [0, 1]))
                P.barrier()

        def load_xT(xT, t, T, first, stg):
            if not first:
                P.dma("sp", xT[:], xs[:, :, t * T:(t + 1) * T].rearrange("c p t -> p c t"), writes=K("xT", range(8)))
                return
            for s in range(T // 128):
                sg = stg[s % 2]
                P.dma("sp", sg[:], x_in[t * T + s * 128: t * T + (s + 1) * 128, :], writes=K("stg", [s % 2]))
                for hb in range(2):
                    b = 6 + hb
                    def fn(e, hb=hb, sg=sg, b=b):
                        ins = None
                        for c in range(4):
                            ins = e.transpose(out=bank(b)[:, c * 128:(c + 1) * 128], in_=sg[:, (hb * 4 + c) * 128:(hb * 4 + c + 1) * 128], identity=ident)
                        return ins
                    P.op("pe", fn, reads=K("stg", [s % 2]) + K("consts"), writes=[BK(b)])
                    P.op("act" if hb == 0 else "dve",
                         (lambda e, hb=hb, b=b, s=s: e.activation(out=xT[:, hb * 4:(hb + 1) * 4, s * 128:(s + 1) * 128],
                                                                  in_=bank(b).rearrange("p (c j) -> p c j", c=4), func=AF.Copy)) if hb == 0 else
                         (lambda e, hb=hb, b=b, s=s: e.tensor_copy(out=xT[:, hb * 4:(hb + 1) * 4, s * 128:(s + 1) * 128],
                                                                   in_=bank(b).rearrange("p (c j) -> p c j", c=4))),
                         reads=[BK(b)], writes=K("xT", range(hb * 4, hb * 4 + 4)))

        def store_xT(xT, t, T, last, stg):
            if not last:
                P.dma("sp", xs[:, :, t * T:(t + 1) * T].rearrange("c p t -> p c t"), xT[:], reads=K("xT", range(8)))
                return
            for s in range(T // 128):
                sg = stg[s % 2]
                for hb in range(2):
                    b = 6 + hb
                    def fn(e, hb=hb, b=b, s=s):
                        ins = None
                        for c in range(4):
                            ins = e.transpose(out=bank(b)[:, c * 128:(c + 1) * 128], in_=xT[:, hb * 4 + c, s * 128:(s + 1) * 128], identity=ident)
                        return ins
                    P.op("pe", fn, reads=K("xT", range(hb * 4, hb * 4 + 4)) + K("consts"), writes=[BK(b)])
                    if hb == 0:
                        P.op("act", lambda e, b=b, sg=sg: e.activation(out=sg[:, 0:512], in_=bank(b), func=AF.Copy),
                             reads=[BK(b)], writes=[("stg", s % 2, 0)] + K("stg", [s % 2]))
                    else:
                        P.op("dve", lambda e, b=b, sg=sg: e.tensor_copy(out=sg[:, 512:1024], in_=bank(b)),
                             reads=[BK(b)], writes=[("stg", s % 2, 1)])
                P.dma("sp", out_d[t * T + s * 128: t * T + (s + 1) * 128, :], sg[:],
                      reads=K("stg", [s % 2]) + [("stg", s % 2, 0), ("stg", s % 2, 1)])

        def store_chunk_prefetch(xT, f, t, T, NTI):
            P.dma("sp", xs[f, :, t * T:(t + 1) * T], xT[:, f, :], reads=K("xT", [f]))
            if t + 1 < NTI:
                P.dma("sp", xT[:, f, :], xs[f, :, (t + 1) * T:(t + 2) * T], writes=K("xT", [f]))

        def ffn_stage(layer, which, first, last):
            T = 512
            NTI = NT // T
            gbase = G_NORM + (layer * 3 + (0 if which == 0 else 2)) * 8
            wg, _ = wview(0, 8, DFF)
            wu, _ = wview(22, 8, DFF)
            wd, _ = wview(44, NCH, D)
            CG = [(0, 6), (6, 12), (12, 17), (17, 22)]
            cgrp = {c: gi for gi, (a, b) in enumerate(CG) for c in range(a, b)}
            for gi, (a, b) in enumerate(CG):
                for (wv, src, nm) in ((wg, w_gate, "Wg"), (wu, w_up, "Wu")):
                    for k in range(8):
                        P.dma("pool", wv[:, k, a * 128:b * 128], src[layer, which][k * 128:(k + 1) * 128, a * 128:b * 128],
                              writes=[(nm, gi, k)])
            for c in range(NCH):
                P.dma("pool", wd[:, c, :], w_down[layer, which][c * 128:(c + 1) * 128, :], writes=[("Wd", c)])
            kd = [("Wd", c) for c in range(NCH)]
            with contextlib.ExitStack() as st:
                sbt = lambda n, s, d=F32: st.enter_context(nc.sbuf_tensor(uniq(n), s, d))
                xT = sbt("xT", [128, 8, T])
                hT = sbt("hT", [128, 8, T], BF16)
                aT = sbt("aT", [128, NCH, T], BF16)
                sg = [sbt(f"sg{i}", [128, T]) for i in range(2)]
                rs = sbt("rs", [128, T])
                stg = [sbt(f"stg{i}", [128, D]) for i in range(2)] if (first or last) else None
                for t in range(NTI):
                    if first or t == 0:
                        load_xT(xT, t, T, first, stg)
                    norm_T([xT[:, c, :] for c in range(8)], [K("xT", [c]) for c in range(8)], 128,
                           [gbase + c for c in range(8)],
                           [hT[:, c, :] for c in range(8)], [K("hT", [c]) for c in range(8)],
                           [aT[:, c, :] for c in range(8)], [K("aT", [c]) for c in range(8)],
                           rs[:], K("rs"), 6, T)
                    for c in range(NCH):
                        bg, bu = c % 2, 2 + c % 2
                        cs = slice(c * 128, (c + 1) * 128)
                        mm_group(bank(bg), [(wg[:, k, cs], hT[:, k, :]) for k in range(8)],
                                 reads=[("Wg", cgrp[c], k) for k in range(8)] + K("hT", range(8)), writes=[BK(bg)])
                        mm_group(bank(bu), [(wu[:, k, cs], hT[:, k, :]) for k in range(8)],
                                 reads=[("Wu", cgrp[c], k) for k in range(8)] + K("hT", range(8)), writes=[BK(bu)])
                        P.op("act", lambda e, c=c, bg=bg: e.activation(out=sg[c % 2][:], in_=bank(bg), func=AF.Silu),
                             reads=[BK(bg)], writes=K("sg", [c % 2]))
                        P.op("dve", lambda e, c=c, bu=bu: e.tensor_tensor(out=aT[:, c, :], in0=bank(bu), in1=sg[c % 2][:], op=ALU.mult),
                             reads=[BK(bu)] + K("sg", [c % 2]), writes=K("aT", [c]))
                    for f in range(8):
                        bo = 4 + f % 2
                        fs = slice(f * 128, (f + 1) * 128)
                        mm_group(bank(bo), [(wd[:, c, fs], aT[:, c, :]) for c in range(NCH)],
                                 reads=kd + K("aT", range(NCH)), writes=[BK(bo)])
                        P.op("dve", lambda e, f=f, bo=bo: e.scalar_tensor_tensor(out=xT[:, f, :], in0=bank(bo), scalar=0.5, in1=xT[:, f, :],
                                                                                 op0=ALU.mult, op1=ALU.add),
                             reads=[BK(bo)] + K("xT", [f]), writes=K("xT", [f]))
                        if not last:
                            if first:
                                P.dma("sp", xs[f, :, t * T:(t + 1) * T], xT[:, f, :], reads=K("xT", [f]))
                            else:
                                store_chunk_prefetch(xT, f, t, T, NTI)
                    if last:
                        store_xT(xT, t, T, True, stg)
                        if t + 1 < NTI and not first:
                            load_xT(xT, t + 1, T, False, None)
                P.barrier()

        def ret_stage(layer):
            T = 256
            NTI = NT // T
            gbase = G_NORM + (layer * 3 + 1) * 8
            gam = [1.0 - 2.0 ** (-5.0 - h) for h in range(H)]
            gC = [g ** 128 for g in gam]
            wi, ki_ = wload(ret_w_in[layer], 0, 8, RPROJ)
            wo, ko_ = wload(ret_w_o[layer], 48, 16, D)
            for cc in range(16):
                P.op("dve", lambda e, cc=cc: e.tensor_scalar(out=wo[:, cc, :], in0=wo[:, cc, :],
                                                             scalar1=gains[:, G_GN + layer * 16 + cc:G_GN + layer * 16 + cc + 1],
                                                             scalar2=None, op0=ALU.mult),
                     reads=K("W", [48 + cc]) + K("gains"), writes=K("W", [48 + cc]))
            with contextlib.ExitStack() as st:
                sbt = lambda n, s, d=F32: st.enter_context(nc.sbuf_tensor(uniq(n), s, d))
                xT = sbt("xT", [128, 8, T])
                hT = sbt("hT", [128, 8, T], BF16)
                yT = sbt("yT", [128, 16, T], BF16)
                rs = sbt("rs", [128, T])
                qT = sbt("qT", [128, 8, T], BF16)
                kT = sbt("kT", [128, 8, T], BF16)
                qf = [sbt(f"qf{i}", [128, T]) for i in range(2)]
                t1s = sbt("t1", [128, T])
                t1 = [t1s, t1s]
                t2 = [sbt(f"t2{i}", [128, T]) for i in range(2)]
                tab = sbt("tab", [128, 2, T])
                vd = sbt("vd", [128, H * RDV], BF16)
                gs = sbt("gs", [128, H * RDV], BF16)
                ktok = sbt("ktok", [128, 8, 128], BF16)
                Ssb = sbt("Ssb", [128, 4, 128], BF16)
                state = sbt("state", [128, H, RDV])
                G = sbt("G", [128, H, RDV], BF16)
                sq = sbt("sq", [128, 1024])
                yn = sbt("yn", [128, 1024])
                y = sbt("y", [128, H * RDV], BF16)
                stt = sbt("stt", [128, 8, 4])
                P.op("pool", lambda e: e.memset(state[:], 0.0), writes=K("state", range(8)))
                P.op("pool", lambda e: e.memset(G[:], 0.0), writes=K("G", range(8)))
                ps_c = pstride(consts)
                for t in range(NTI):
                    if t == 0:
                        load_xT(xT, t, T, False, None)
                    P.dma("sp", tab[:], tabR[:, :, t * T:(t + 1) * T].rearrange("w p t -> p w t"), writes=K("tab"))
                    norm_T([xT[:, c, :] for c in range(8)], [K("xT", [c]) for c in range(8)], 128,
                           [gbase + c for c in range(8)],
                           [hT[:, c, :] for c in range(8)], [K("hT", [c]) for c in range(8)],
                           [yT[:, c, :] for c in range(8)], [K("yT", [c]) for c in range(8)],
                           rs[:], K("rs"), 4, T)
                    i = 0
                    for h in range(H):
                        for (dst, dkey, col0) in ((qT, "qT", 0), (kT, "kT", 1024)):
                            b = i % 2
                            i += 1
                            cs = slice(col0 + h * 128, col0 + (h + 1) * 128)
                            mm_group(bank(b)[:, 0:T], [(wi[:, k, cs], hT[:, k, :]) for k in range(8)],
                                     reads=ki_ + K("hT", range(8)), writes=[BK(b)])
                            P.op("act", lambda e, b=b: e.activation(out=qf[b][:], in_=bank(b)[:, 0:T], func=AF.Copy),
                                 reads=[BK(b)], writes=K("qf", [b]))
                            rope_T(qf[b][:], K("qf", [b]), 128, tab[:, 0, :], tab[:, 1, :], K("tab"), t1[b][:], t2[b][:],
                                   [("t1",), ("t2", b, 0), ("t2", b, 1)], dst[:, h, :], K(dkey, [h]))
                    def vg(n, grps):
                        tk = slice(n * 128, (n + 1) * 128)
                        for grp in grps:
                            b = 2 + grp % 2
                            col = 2048 + grp * 512
                            mm_group(bank(b), [(hT[:, k, tk], wi[:, k, col:col + 512]) for k in range(8)],
                                     reads=ki_ + K("hT", range(8)), writes=[BK(b)])
                            if grp < 4:
                                kdb = bass.AP(consts, C_KD + grp * 2, [[ps_c, 128], [1, 2], [0, RDV]])
                                P.op("dve", lambda e, b=b, grp=grp, kdb=kdb: e.tensor_tensor(
                                    out=vd[:, grp * 512:(grp + 1) * 512].rearrange("p (a v) -> p a v", a=2),
                                    in0=bank(b).rearrange("p (a v) -> p a v", a=2), in1=kdb, op=ALU.mult),
                                    reads=[BK(b)] + K("consts"), writes=K("vd", [grp]))
                            else:
                                g4 = grp - 4
                                P.op("act", lambda e, b=b, g4=g4: e.activation(out=gs[:, g4 * 512:(g4 + 1) * 512], in_=bank(b), func=AF.Silu),
                                     reads=[BK(b)], writes=K("gs", [g4]))

                    def ktr(n):
                        tk = slice(n * 128, (n + 1) * 128)
                        def fnk(e):
                            ins = None
                            for h in range(H):
                                ins = e.transpose(out=bankb(4)[:, h * 128:(h + 1) * 128], in_=kT[:, h, tk], identity=identb[:])
                            return ins
                        P.op("pe", fnk, reads=K("kT", range(8)) + K("identb"), writes=[BK(4)])
                        P.op("act", lambda e: e.activation(out=ktok[:].rearrange("p h d -> p (h d)"), in_=bankb(4), func=AF.Copy),
                             reads=[BK(4)], writes=K("ktok"))

                    def SOU(n, hg):
                        tk = slice(n * 128, (n + 1) * 128)
                        ob = 6 if hg == 0 else 0
                        hs = list(range(hg * 4, hg * 4 + 4))
                        def fns(e):
                            ins = None
                            for j, h in enumerate(hs):
                                ins = e.matmul(bank(5)[:, j * 128:(j + 1) * 128], kT[:, h, tk], qT[:, h, tk], start=True, stop=True)
                            return ins
                        P.op("pe", fns, reads=K("kT", hs) + K("qT", hs), writes=[BK(5)])
                        mkb = bass.AP(maskb, 0, [[pstride(maskb), 128], [0, 4], [1, 128]])
                        P.op("dve", lambda e: e.tensor_tensor(out=Ssb[:], in0=bank(5).rearrange("p (a i) -> p a i", a=4), in1=mkb, op=ALU.mult),
                             reads=[BK(5)] + K("maskb"), writes=K("Ssb"))
                        def fno(e):
                            ins = None
                            for j, h in enumerate(hs):
                                o = PS[:, ob + j // 2, (j % 2) * 256:(j % 2 + 1) * 256]
                                e.matmul(o, Ssb[:, j, :], vd[:, h * 256:(h + 1) * 256], start=True, stop=False)
                                ins = e.matmul(o, qT[:, h, tk], G[:, h, :], start=False, stop=True)
                            return ins
                        P.op("pe", fno, reads=K("Ssb") + K("vd", [hg * 2, hg * 2 + 1]) + K("qT", hs) + K("G", hs), writes=[BK(ob), BK(ob + 1)])
                        def fnu(e):
                            ins = None
                            for j, h in enumerate(hs):
                                o = PS[:, 2 + j // 2, (j % 2) * 256:(j % 2 + 1) * 256]
                                ins = e.matmul(o, ktok[:, h, :], vd[:, h * 256:(h + 1) * 256], start=True, stop=True)
                            return ins
                        P.op("pe", fnu, reads=K("ktok") + K("vd", [hg * 2, hg * 2 + 1]), writes=[BK(2), BK(3)])
                        for j, h in enumerate(hs):
                            o = PS[:, 2 + j // 2, (j % 2) * 256:(j % 2 + 1) * 256]
                            P.op("dve", lambda e, h=h, o=o: e.scalar_tensor_tensor(out=state[:, h, :], in0=state[:, h, :], scalar=gC[h], in1=o,
                                                                                   op0=ALU.mult, op1=ALU.add),
                                 reads=[BK(2 + j // 2)] + K("state", [h]), writes=K("state", [h]))
                            P.op("act", lambda e, h=h: e.activation(out=G[:, h, :], in_=state[:, h, :], func=AF.Copy, scale=gC[h]),
                                 reads=K("state", [h]), writes=K("G", [h]))

                    def GN(n, hg):
                        ob = 6 if hg == 0 else 0
                        hs = list(range(hg * 4, hg * 4 + 4))
                        okeys = [BK(ob), BK(ob + 1)]
                        o4 = PS[:, ob:ob + 2, :].rearrange("p b (a v) -> p (b a) v", a=2)
                        oflat = PS[:, ob:ob + 2, :].rearrange("p b c -> p (b c)")
                        P.op("act", lambda e: e.activation(out=sq[:], in_=oflat, func=AF.Square), reads=okeys, writes=K("sq"))
                        P.op("dve", lambda e: e.tensor_reduce(out=stt[:, 0, :], in_=o4, axis=AX.X, op=ALU.add), reads=okeys, writes=K("stt", [0]))
                        P.op("dve", lambda e: e.tensor_reduce(out=stt[:, 1, :], in_=sq[:].rearrange("p (a v) -> p a v", a=4), axis=AX.X, op=ALU.add),
                             reads=K("sq"), writes=K("stt", [1]))
                        P.op("dve", lambda e: e.tensor_scalar(out=stt[:, 2, :], in0=stt[:, 0, :], scalar1=1.0 / RDV, scalar2=None, op0=ALU.mult),
                             reads=K("stt", [0]), writes=K("stt", [2]))
                        P.op("dve", lambda e: e.tensor_tensor(out=stt[:, 3, :], in0=stt[:, 2, :], in1=stt[:, 2, :], op=ALU.mult),
                             reads=K("stt", [2]), writes=K("stt", [3]))
                        P.op("dve", lambda e: e.scalar_tensor_tensor(out=stt[:, 3, :], in0=stt[:, 1, :], scalar=1.0 / RDV, in1=stt[:, 3, :],
                                                                     op0=ALU.mult, op1=ALU.subtract),
                             reads=K("stt", [1, 3]), writes=K("stt", [3]))
                        P.op("dve", lambda e: e.tensor_tensor(out=stt[:, 3, :], in0=stt[:, 3, :], in1=consts[:, C_EPSR + hg * 4:C_EPSR + hg * 4 + 4], op=ALU.add),
                             reads=K("stt", [3]) + K("consts"), writes=K("stt", [3]))
                        P.op("act", lambda e: e.activation(out=stt[:, 4, :], in_=stt[:, 3, :], func=AF.Sqrt), reads=K("stt", [3]), writes=K("stt", [4]))
                        P.op("dve", lambda e: e.reciprocal(out=stt[:, 4, :], in_=stt[:, 4, :]), reads=K("stt", [4]), writes=K("stt", [4]))
                        P.op("dve", lambda e: e.scalar_tensor_tensor(out=stt[:, 5, :], in0=stt[:, 2, :], scalar=-1.0, in1=stt[:, 4, :],
                                                                     op0=ALU.mult, op1=ALU.mult),
                             reads=K("stt", [2, 4]), writes=K("stt", [5]))
                        for j, h in enumerate(hs):
                            o = PS[:, ob + j // 2, (j % 2) * 256:(j % 2 + 1) * 256]
                            P.op("act", lambda e, j=j, o=o: e.activation(out=yn[:, j * 256:(j + 1) * 256], in_=o, func=AF.Identity,
                                                                         scale=stt[:, 4, j:j + 1], bias=stt[:, 5, j:j + 1]),
                                 reads=[BK(ob + j // 2)] + K("stt", [4, 5]), writes=[("yn", j)])
                        P.op("pool", lambda e: e.tensor_tensor(out=y[:, hg * 1024:(hg + 1) * 1024], in0=yn[:], in1=gs[:, hg * 1024:(hg + 1) * 1024], op=ALU.mult),
                             reads=[("yn", j) for j in range(4)] + K("gs", [hg * 2, hg * 2 + 1]), writes=K("y", [hg]))

                    def ytr(n):
                        tk = slice(n * 128, (n + 1) * 128)
                        for hb in range(2):
                            bb = 2 + hb
                            def fny(e, hb=hb, bb=bb):
                                ins = None
                                for c in range(8):
                                    cc = hb * 8 + c
                                    ins = e.transpose(out=bankb(bb)[:, c * 128:(c + 1) * 128], in_=y[:, cc * 128:(cc + 1) * 128], identity=identb[:])
                                return ins
                            P.op("pe", fny, reads=K("y", [hb]) + K("identb"), writes=[BK(bb)])
                            P.op("act" if hb == 0 else "dve",
                                 (lambda e, hb=hb, bb=bb: e.activation(out=yT[:, hb * 8:(hb + 1) * 8, tk], in_=bankb(bb).rearrange("p (c j) -> p c j", c=8), func=AF.Copy)) if hb == 0 else
                                 (lambda e, hb=hb, bb=bb: e.tensor_copy(out=yT[:, hb * 8:(hb + 1) * 8, tk], in_=bankb(bb).rearrange("p (c j) -> p c j", c=8))),
                                 reads=[BK(bb)], writes=K("yT", range(hb * 8, hb * 8 + 8)))

                    NCT = T // 128
                    vg(0, range(0, 4)); ktr(0); SOU(0, 0); SOU(0, 1); vg(0, range(4, 8))
                    for n in range(NCT):
                        if n + 1 < NCT:
                            vg(n + 1, range(0, 4))
                        GN(n, 0); GN(n, 1)
                        if n + 1 < NCT:
                            ktr(n + 1); SOU(n + 1, 0); SOU(n + 1, 1)
                        ytr(n)
                        if n + 1 < NCT:
                            vg(n + 1, range(4, 8))
                    for f in range(8):
                        bo = 2 + f % 2
                        fs = slice(f * 128, (f + 1) * 128)
                        mm_group(bank(bo)[:, 0:T], [(wo[:, c, fs], yT[:, c, :]) for c in range(16)],
                                 reads=ko_ + K("yT", range(16)), writes=[BK(bo)])
                        P.op("dve", lambda e, f=f, bo=bo: e.tensor_tensor(out=xT[:, f, :], in0=bank(bo)[:, 0:T], in1=xT[:, f, :], op=ALU.add),
                             reads=[BK(bo)] + K("xT", [f]), writes=K("xT", [f]))
                        store_chunk_prefetch(xT, f, t, T, NTI)
                P.barrier()

        def kv_stage():
            T = 512
            NTI = NT // T
            wdn, kdn = wload(kv_w_down, 0, 8, KVL + ROPE)
            wup, kup = wload(kv_w_up, 4, 2, 2048)
            with contextlib.ExitStack() as st:
                sbt = lambda n, s, d=F32: st.enter_context(nc.sbuf_tensor(uniq(n), s, d))
                xT = sbt("xT", [128, 8, T])
                hT = sbt("hT", [128, 8, T], BF16)
                sqb = sbt("sqb", [128, 8, T], BF16)
                rs = sbt("rs", [128, T])
                cnT = sbt("cnT", [128, 2, T], BF16)
                kpf = sbt("kpf", [64, T])
                t1 = sbt("t1", [128, T])
                t2 = sbt("t2", [128, T])
                tab = sbt("tab", [64, 2, T])
                kpo = sbt("kpo", [64, T], BF16)
                kno = sbt("kno", [128, H, T], BF16)
                vsb = sbt("vsb", [128, 4, 1024], BF16)
                for t in range(NTI):
                    load_xT(xT, t, T, False, None)
                    P.dma("sp", tab[:], tabM[:, :, t * T:(t + 1) * T].rearrange("w p t -> p w t"), writes=K("tab"))
                    norm_T([xT[:, c, :] for c in range(8)], [K("xT", [c]) for c in range(8)], 128,
                           [G_KVN + c for c in range(8)],
                           [hT[:, c, :] for c in range(8)], [K("hT", [c]) for c in range(8)],
                           [sqb[:, c, :] for c in range(8)], [K("sqb", [c]) for c in range(8)],
                           rs[:], K("rs"), 7, T)
                    for m in range(2):
                        mm_group(bank(m), [(wdn[:, k, m * 128:(m + 1) * 128], hT[:, k, :]) for k in range(8)],
                                 reads=kdn + K("hT", range(8)), writes=[BK(m)])
                    mm_group(bank(2)[0:64, :], [(wdn[:, k, 256:320], hT[:, k, :]) for k in range(8)],
                             reads=kdn + K("hT", range(8)), writes=[BK(2)])
                    norm_T([bank(0), bank(1)], [[BK(0)], [BK(1)]], 128, [G_LAT, G_LAT + 1],
                           [cnT[:, 0, :], cnT[:, 1, :]], [K("cnT", [0]), K("cnT", [1])],
                           [sqb[:, 0, :], sqb[:, 1, :]], [K("sqb", [0]), K("sqb", [1])], rs[:], K("rs"), 3, T)
                    norm_T([bank(2)[0:64, :]], [[BK(2)]], 64, [G_KR], [kpf[:]], [K("kpf")],
                           [sqb[0:64, 2, :]], [K("sqb", [2])], rs[0:64, :], K("rs"), 3, T)
                    rope_T(kpf[:], K("kpf"), 64, tab[:, 0, :], tab[:, 1, :], K("tab"), t1[0:64, :], t2[0:64, :],
                           [("t1",), ("t2", 0), ("t2", 1)], kpo[:], K("kpo"))
                    P.dma("sp", kpeT_d[:, t * T:(t + 1) * T], kpo[:], reads=K("kpo"))
                    for h in range(H):
                        b = 4 + h % 2
                        mm_group(bank(b), [(wup[:, m, h * 256:h * 256 + 128], cnT[:, m, :]) for m in range(2)],
                                 reads=kup + K("cnT", [0, 1]), writes=[BK(b)])
                        norm_T([bank(b)], [[BK(b)]], 128, [G_KN], [kno[:, h, :]], [K("kno", [h])],
                               [sqb[:, 3 + h % 2, :]], [K("sqb", [3 + h % 2])], rs[:], K("rs"), 6, T)
                    P.dma("sp", knT_d[:, :, t * T:(t + 1) * T].rearrange("h p t -> p h t"), kno[:], reads=K("kno", range(8)))
                    psw = pstride(Wt)
                    for s in range(4):
                        for half in range(2):
                            b = half
                            pairs = []
                            for m in range(2):
                                rhs = bass.AP(Wt, 4 * 1024 + m * 2048 + half * 1024 + 128, [[psw, 128], [256, 4], [1, 128]])
                                pairs.append((cnT[:, m, s * 128:(s + 1) * 128], rhs))
                            mm_group(bank(b), pairs, reads=kup + K("cnT", [0, 1]), writes=[BK(b)])
                            P.op("act", lambda e, s=s, half=half, b=b: e.activation(out=vsb[:, s, half * 512:(half + 1) * 512], in_=bank(b), func=AF.Copy),
                                 reads=[BK(b)], writes=K("vsb", [s]))
                    for h in range(H):
                        P.dma("sp", vs_d[h, :, t * 4:(t + 1) * 4, :], vsb[:, :, h * 128:(h + 1) * 128], reads=K("vsb", range(4)))
                P.barrier()

        def mla_stage(layer):
            T = 512
            NTI = NT // T
            j_ = layer - N_SELF
            gbase = G_NORM + (layer * 3 + 1) * 8
            gq = G_QL + j_ * 5
            scale = (NOPE + ROPE) ** -0.5
            wdq, kdq = wload(mla_w_dq[j_], 0, 8, QL)
            wuq, kuq = wload(mla_w_uq[j_], 3, 3, 1536)
            wo, ko_ = wload(mla_w_o[j_], 8, 8, D)
            with contextlib.ExitStack() as st:
                sbt = lambda n, s, d=F32: st.enter_context(nc.sbuf_tensor(uniq(n), s, d))
                def aview(slot0, nslots):
                    return Wt[:, slot0:slot0 + nslots, :].rearrange("p s c -> p (s c)")
                xT = sbt("xT", [128, 8, T])
                rs = sbt("rs", [128, T])
                cqf = sbt("cqf", [128, 3, T])
                qnf = [sbt(f"qnf{i}", [128, T]) for i in range(4)]
                qpf = [sbt(f"qpf{i}", [64, T]) for i in range(4)]
                rsn = [sbt(f"rsn{i}", [128, T]) for i in range(2)]
                rsp = [sbt(f"rsp{i}", [64, T]) for i in range(2)]
                t1 = [sbt(f"t1{i}", [64, T]) for i in range(2)]
                t2 = [sbt(f"t2{i}", [64, T]) for i in range(2)]
                tab = sbt("tab", [64, 2, T])
                hT = aview(16, 4).rearrange("p (c t) -> p c t", c=8)
                sqb = aview(20, 4).rearrange("p (c t) -> p c t", c=8)
                qnT = aview(24, 4).rearrange("p (c t) -> p c t", c=8)
                qpT = aview(28, 4).rearrange("p (c t) -> p c t", c=8)
                oT = aview(32, 4).rearrange("p (c t) -> p c t", c=8)
                cqT = aview(36, 2)[:, 0:3 * T].rearrange("p (c t) -> p c t", c=3)
                pTall = aview(38, 2).rearrange("p (c t) -> p c t", c=4)
                pT = [pTall[:, i, :] for i in range(4)]
                knS = [aview(40 + 4 * i, 4) for i in range(2)]
                vS = [aview(48 + 4 * i, 4).rearrange("p (b d) -> p b d", d=128) for i in range(2)]
                kpS = aview(56, 4)
                P.op("pool", lambda e: e.memset(kpS[64:128, :], 0.0), writes=K("kpS"))
                P.op("pool", lambda e: e.memset(qpT[64:128, :, :], 0.0), writes=K("qpT", range(8)))
                rl = sbt("rl", [128, T])
                for t in range(NTI):
                    nk = (t + 1) * T
                    nkb = nk // 128
                    if t == 0:
                        load_xT(xT, t, T, False, None)
                    P.dma("sp", tab[:], tabM[:, :, t * T:(t + 1) * T].rearrange("w p t -> p w t"), writes=K("tab"))
                    P.dma("sp", kpS[0:64, 0:nk], kpeT_d[:, 0:nk], writes=K("kpS"))
                    norm_T([xT[:, c, :] for c in range(8)], [K("xT", [c]) for c in range(8)], 128,
                           [gbase + c for c in range(8)],
                           [hT[:, c, :] for c in range(8)], [K("hT", [c]) for c in range(8)],
                           [sqb[:, c, :] for c in range(8)], [K("sqb", [c]) for c in range(8)],
                           rs[:], K("rs"), 6, T)
                    for m in range(3):
                        b = 6 + m % 2
                        mm_group(bank(b), [(wdq[:, k, m * 128:(m + 1) * 128], hT[:, k, :]) for k in range(8)],
                                 reads=kdq + K("hT", range(8)), writes=[BK(b)])
                        P.op("act", lambda e, m=m, b=b: e.activation(out=cqf[:, m, :], in_=bank(b), func=AF.Copy),
                             reads=[BK(b)], writes=K("cqf", [m]))
                    norm_T([cqf[:, m, :] for m in range(3)], [K("cqf", [m]) for m in range(3)], 128, [gq, gq + 1, gq + 2],
                           [cqT[:, m, :] for m in range(3)], [K("cqT", [m]) for m in range(3)],
                           [sqb[:, m, :] for m in range(3)], [K("sqb", [m]) for m in range(3)], rs[:], K("rs"), 6, T)
                    def qS0(h):
                        r4 = h % 4
                        mm_group(bank(7), [(wuq[:, m, h * 192:h * 192 + 128], cqT[:, m, :]) for m in range(3)],
                                 reads=kuq + K("cqT", range(3)), writes=[BK(7)])
                        P.op("dve", lambda e: e.tensor_copy(out=qnf[r4][:], in_=bank(7)), reads=[BK(7)], writes=K("qnf", [r4]))
                        mm_group(bank(6)[0:64, :], [(wuq[:, m, h * 192 + 128:h * 192 + 192], cqT[:, m, :]) for m in range(3)],
                                 reads=kuq + K("cqT", range(3)), writes=[BK(6)])
                        P.op("act", lambda e: e.activation(out=qpf[r4][:], in_=bank(6)[0:64, :], func=AF.Copy), reads=[BK(6)], writes=K("qpf", [r4]))

                    def qS1(h):
                        r4, r = h % 4, h % 2
                        P.op("pool", lambda e: e.tensor_tensor(out=sqb[:, 3 + r, :], in0=qnf[r4][:], in1=qnf[r4][:], op=ALU.mult),
                             reads=K("qnf", [r4]), writes=K("sqb", [3 + r]))
                        P.op("pool", lambda e: e.tensor_tensor(out=sqb[0:64, 5 + r, :], in0=qpf[r4][:], in1=qpf[r4][:], op=ALU.mult),
                             reads=K("qpf", [r4]), writes=K("sqb", [5 + r]))
                        mm_group(bank(5), [(onesb[:], sqb[:, 3 + r, :])], reads=K("onesb") + K("sqb", [3 + r]), writes=[BK(5)])
                        mm_group(bank(4)[0:64, :], [(onesb[0:64, 0:64], sqb[0:64, 5 + r, :])], reads=K("onesb") + K("sqb", [5 + r]), writes=[BK(4)])

                    def qS2(h):
                        r = h % 2
                        P.op("act", lambda e: e.activation(out=rsn[r][:], in_=bank(5), func=AF.Ln, scale=1.0 / 128, bias=epsc[:, 0:1]),
                             reads=[BK(5)] + K("epsc"), writes=K("rsn", [r]))
                        P.op("act", lambda e: e.activation(out=rsn[r][:], in_=rsn[r][:], func=AF.Exp, scale=-0.5), reads=K("rsn", [r]), writes=K("rsn", [r]))
                        P.op("act", lambda e: e.activation(out=rsp[r][:], in_=bank(4)[0:64, :], func=AF.Ln, scale=1.0 / 64, bias=epsc[0:64, 0:1]),
                             reads=[BK(4)] + K("epsc"), writes=K("rsp", [r]))
                        P.op("act", lambda e: e.activation(out=rsp[r][:], in_=rsp[r][:], func=AF.Exp, scale=-0.5), reads=K("rsp", [r]), writes=K("rsp", [r]))

                    def qS3(h):
                        r4, r = h % 4, h % 2
                        P.op("dve", lambda e: e.scalar_tensor_tensor(out=qnT[:, h, :], in0=qnf[r4][:], scalar=gains[:, gq + 3:gq + 4], in1=rsn[r][:],
                                                                     op0=ALU.mult, op1=ALU.mult),
                             reads=K("qnf", [r4]) + K("rsn", [r]) + K("gains"), writes=K("qnT", [h]))
                        P.op("dve", lambda e: e.scalar_tensor_tensor(out=qpf[r4][:], in0=qpf[r4][:], scalar=gains[0:64, gq + 4:gq + 5], in1=rsp[r][:],
                                                                     op0=ALU.mult, op1=ALU.mult),
                             reads=K("qpf", [r4]) + K("rsp", [r]) + K("gains"), writes=K("qpf", [r4]))

                    def qS4(h):
                        r4, r = h % 4, h % 2
                        rope_T(qpf[r4][:], K("qpf", [r4]), 64, tab[:, 0, :], tab[:, 1, :], K("tab"), t1[r][:], t2[r][:],
                               [("t1", r), ("t2", r, 0), ("t2", r, 1)], qpT[0:64, h, :], K("qpT", [h]))

                    qst = [qS0, qS1, qS2, qS3, qS4]
                    for step in range(H + len(qst) - 1):
                        for k in range(len(qst) - 1, -1, -1):
                            h = step - k
                            if 0 <= h < H:
                                qst[k](h)
                    SB = [0, 1, 6]
                    items = [(h, jb) for h in range(H) for jb in range(nkb)]

                    def geom(jb):
                        d = jb - 4 * t
                        return d, (d * 128 if d > 0 else 0)

                    def emit_S(i):
                        h, jb = items[i]
                        kb = h % 2
                        if jb == 0:
                            P.dma("sp", knS[kb][:, 0:nk], knT_d[h, :, 0:nk], writes=K("knS", [kb]))
                            P.dma("sp", vS[kb][:, 0:nkb, :], vs_d[h, :, 0:nkb, :], writes=K("vS", [kb]))
                        d, c0 = geom(jb)
                        bs = SB[i % 3]
                        ks = slice(jb * 128, (jb + 1) * 128)
                        def fs(e):
                            e.matmul(bank(bs)[:, c0:T], knS[kb][:, ks], qnT[:, h, c0:T], start=True, stop=False)
                            return e.matmul(bank(bs)[:, c0:T], kpS[:, ks], qpT[:, h, c0:T], start=False, stop=True)
                        P.op("pe", fs, reads=K("knS", [kb]) + K("kpS") + K("qnT", [h]) + K("qpT", [h]), writes=[BK(bs)])

                    def emit_rest(i):
                        h, jb = items[i]
                        kb = h % 2
                        d, c0 = geom(jb)
                        bs = SB[i % 3]
                        pt = pT[i % 4]
                        pk = K("pT", [i % 4])
                        bO, bL = 2 + h % 2, 4 + h % 2
                        P.op("act", lambda e: e.activation(out=pt[:, c0:T], in_=bank(bs)[:, c0:T], func=AF.Exp, scale=scale),
                             reads=[BK(bs)], writes=pk)
                        if d >= 0:
                            P.op("pool", lambda e: e.tensor_tensor(out=pt[:, c0:c0 + 128], in0=pt[:, c0:c0 + 128], in1=maskb[:], op=ALU.mult),
                                 reads=pk + K("maskb"), writes=pk)
                        def fpv(e):
                            e.matmul(bank(bO)[:, c0:T], vS[kb][:, jb, :], pt[:, c0:T], start=(jb == 0), stop=(jb == nkb - 1))
                            return e.matmul(bank(bL)[:, c0:T], onesb[:], pt[:, c0:T], start=(jb == 0), stop=(jb == nkb - 1))
                        P.op("pe", fpv, reads=K("vS", [kb]) + pk + K("onesb"), writes=[BK(bO), BK(bL)])
                        if jb == nkb - 1:
                            P.op("dve", lambda e: e.reciprocal(out=rl[:], in_=bank(bL)), reads=[BK(bL)], writes=K("rl"))
                            P.op("dve", lambda e: e.tensor_tensor(out=oT[:, h, :], in0=bank(bO), in1=rl[:], op=ALU.mult),
                                 reads=[BK(bO)] + K("rl"), writes=K("oT", [h]))

                    DEPTH_S = 2
                    for i in range(min(DEPTH_S, len(items))):
                        emit_S(i)
                    for i in range(len(items)):
                        if i + DEPTH_S < len(items):
                            emit_S(i + DEPTH_S)
                        emit_rest(i)
                    for f in range(8):
                        bo = 6 + f % 2
                        fs_ = slice(f * 128, (f + 1) * 128)
                        mm_group(bank(bo), [(wo[:, hh, fs_], oT[:, hh, :]) for hh in range(H)],
                                 reads=ko_ + K("oT", range(8)), writes=[BK(bo)])
                        P.op("dve", lambda e, f=f, bo=bo: e.tensor_tensor(out=xT[:, f, :], in0=bank(bo), in1=xT[:, f, :], op=ALU.add),
                             reads=[BK(bo)] + K("xT", [f]), writes=K("xT", [f]))
                        store_chunk_prefetch(xT, f, t, T, NTI)
                P.barrier()

        stages = [("pro",)]
        for l in range(DEPTH):
            stages.append(("ffn", l, 0))
            stages.append(("ret", l) if l < N_SELF else ("mla", l))
            stages.append(("ffn", l, 1))
            if l == N_SELF - 1:
                stages.append(("kv",))
        if nstages is not None:
            stages = stages[:nstages]
        ffn_idx = [i for i, s in enumerate(stages) if s[0] == "ffn"]
        assert ffn_idx and ffn_idx[-1] == len(stages) - 1, "last stage must be an ffn stage"
        for i, s in enumerate(stages):
            if s[0] == "pro":
                prologue()
            elif s[0] == "ffn":
                ffn_stage(s[1], s[2], first=(i == ffn_idx[0]), last=(i == len(stages) - 1))
            elif s[0] == "ret":
                ret_stage(s[1])
            elif s[0] == "kv":
                kv_stage()
            elif s[0] == "mla":
                mla_stage(s[1])
        P.finish()
        nc._ninst = P.ninst
    return nc


def host_consts():
    c = np.zeros((128, C_NC), np.float32)
    c[:, C_ID:C_ID + 128] = np.eye(128, dtype=np.float32)
    p = np.arange(128)
    c[:, C_MASK:C_MASK + 128] = (p[:, None] <= p[None, :]).astype(np.float32)
    gam = 1.0 - 2.0 ** (-5.0 - np.arange(H, dtype=np.float64))
    c[:, C_KD:C_KD + 8] = (gam[None, :] ** (127 - p)[:, None]) * (RDK ** -0.5)
    c[:, C_EPSR:C_EPSR + 8] = EPS * gam[None, :] ** (2.0 * (127 - p)[:, None])
    c[:, C_INVR] = ROPE_BASE ** (-(2.0 * (p % 64)) / 128.0)
    c[:64, C_INVM] = ROPE_BASE ** (-(2.0 * (p[:64] % 32)) / 64.0)
    c[:, C_SGR] = np.where(p < 64, 1.0, -1.0)
    c[:64, C_SGM] = np.where(p[:64] < 32, 1.0, -1.0)
    return c


def host_gains(inp):
    g = np.zeros((128, G_NC), np.float32)

    def put(col, vec):
        vec = np.asarray(vec, np.float32).reshape(-1)
        n = vec.shape[0]
        if n >= 128:
            k = n // 128
            g[:, col:col + k] = vec.reshape(k, 128).T
        else:
            g[:n, col] = vec
    ng = np.asarray(inp["norm_g"])
    for l in range(DEPTH):
        for i in range(3):
            put(G_NORM + (l * 3 + i) * 8, ng[l, i])
    put(G_KVN, inp["kv_norm_g"])
    put(G_LAT, inp["kv_latent_norm_g"])
    put(G_KN, inp["k_nope_norm_g"])
    put(G_KR, inp["k_rope_norm_g"])
    for j in range(2):
        put(G_QL + j * 5, np.asarray(inp["mla_q_lora_norm_g"])[j])
        put(G_QL + j * 5 + 3, np.asarray(inp["mla_q_nope_norm_g"])[j])
        put(G_QL + j * 5 + 4, np.asarray(inp["mla_q_rope_norm_g"])[j])
    for l in range(N_SELF):
        put(G_GN + l * 16, np.asarray(inp["ret_gn_g"])[l].reshape(-1))
    return g


WKEYS = ["ffn_w_gate", "ffn_w_up", "ffn_w_down", "ret_w_in", "ret_w_o", "kv_w_down", "kv_w_up",
         "mla_w_dq", "mla_w_uq", "mla_w_o"]


def make_in_maps(inp, ncores, NT):
    consts = host_consts()
    gains = host_gains(inp)
    shared = {k: np.ascontiguousarray(np.asarray(inp[k], dtype=np.float32)) for k in WKEYS}
    x = np.asarray(inp["x"], dtype=np.float32)
    pos = np.asarray(inp["positions"]).astype(np.int32)
    maps = []
    for b in range(ncores):
        m = dict(shared)
        m["x"] = np.ascontiguousarray(x[b, :NT])
        m["pos"] = np.ascontiguousarray(pos[b:b + 1, :NT])
        m["consts"] = consts
        m["gains"] = gains
        maps.append(m)
    return maps


def kernel(**inputs):
    nc = build(SEQ)
    maps = make_in_maps(inputs, 8, SEQ)
    res = run_bass_kernel_spmd(nc, maps, core_ids=list(range(8)))
    return np.stack([np.asarray(r["out"], dtype=np.float32) for r in res.results], axis=0)
```

```python
import contextlib
import math
import numpy as np
import concourse.bass as bass
import concourse.mybir as mybir
from concourse.bass_utils import run_bass_kernel_spmd

F32 = mybir.dt.float32
BF16 = mybir.dt.bfloat16
I32 = mybir.dt.int32
AF = mybir.ActivationFunctionType
ALU = mybir.AluOpType
AX = mybir.AxisListType

D = 1024
DFF = 2816
NCH = DFF // 128
DEPTH = 4
N_SELF = 2
H = 8
RDK = 128
RDV = 256
RPROJ = 6144
NOPE = 128
ROPE = 64
QL = 384
KVL = 256
EPS = 1e-6
ROPE_BASE = 10000.0
SEQ = 4096
WSLOTS = 66

C_ID, C_MASK, C_KD, C_EPSR, C_INVR, C_INVM, C_SGR, C_SGM, C_NC = 0, 128, 256, 264, 272, 273, 274, 275, 276
G_NORM = 0
G_KVN = 96
G_LAT = 104
G_KN = 106
G_KR = 107
G_QL = 108
G_GN = 118
G_NC = 150


class Prog:
    NDMA = 8

    def __init__(self, nc, stack):
        self.nc = nc
        self.eng = {"pe": nc.tensor, "act": nc.scalar, "dve": nc.vector, "pool": nc.gpsimd, "sp": nc.sync}
        self.sem, self.cnt = {}, {}
        self.seen = {e: {} for e in self.eng}
        self.lastw, self.readers, self.dq = {}, {}, {}
        self.ninst = 0
        for e in self.eng:
            self.sem[e] = stack.enter_context(nc.semaphore(f"s_{e}"))
            self.cnt[e] = 0
        for q in ("sp", "pool"):
            sems = [stack.enter_context(nc.semaphore(f"d_{q}{i}")) for i in range(self.NDMA)]
            self.dq[q] = {"sems": sems, "cnt": [0] * self.NDMA, "i": 0}

    def _deps(self, e, reads, writes):
        toks = []
        for k in reads:
            w = self.lastw.get(k)
            if w is not None:
                toks.append(w)
        for k in writes:
            w = self.lastw.get(k)
            if w is not None:
                toks.append(w)
            toks.extend(self.readers.get(k, {}).values())
        best = {}
        for (sem, val, src) in toks:
            if src == "pe" and e == "pe":
                continue
            key = id(sem)
            if key not in best or best[key][1] < val:
                best[key] = (sem, val)
        out = []
        seen = self.seen[e]
        for key, (sem, val) in best.items():
            if seen.get(key, 0) >= val:
                continue
            seen[key] = val
            out.append((sem, val))
        return out

    def _commit(self, tok, reads, writes):
        sid = id(tok[0])
        for k in reads:
            self.readers.setdefault(k, {})[sid] = tok
        for k in writes:
            self.lastw[k] = tok
            self.readers[k] = {}

    def op(self, e, fn, reads=(), writes=()):
        eng = self.eng[e]
        for (sem, val) in self._deps(e, reads, writes):
            eng.wait_ge(sem, val)
            self.ninst += 1
        ins = fn(eng)
        self.cnt[e] += 1
        ins.then_inc(self.sem[e], 1)
        tok = (self.sem[e], self.cnt[e], e)
        self._commit(tok, reads, writes)
        self.ninst += 1
        return tok

    def dma(self, q, out, in_, reads=(), writes=()):
        eng = self.eng[q]
        d = self.dq[q]
        i = d["i"]
        d["i"] = (i + 1) % self.NDMA
        sem = d["sems"][i]
        waits = self._deps(q, reads, writes)
        prev = d["cnt"][i]
        if prev > 0 and self.seen[q].get(id(sem), 0) < prev:
            self.seen[q][id(sem)] = prev
            waits = [w for w in waits if w[0] is not sem] + [(sem, prev)]
        for (s, v) in waits:
            eng.wait_ge(s, v)
            self.ninst += 1
        eng.dma_start(out=out, in_=in_).then_inc(sem, 16)
        d["cnt"][i] = prev + 16
        tok = (sem, prev + 16, "dma")
        self._commit(tok, reads, writes)
        self.ninst += 1
        return tok

    def barrier(self, engines=("pe", "act", "dve", "pool", "sp"), dma_queues=("sp",)):
        targets = [(self.sem[e], self.cnt[e]) for e in engines if self.cnt[e] > 0]
        for q in dma_queues:
            d = self.dq[q]
            targets += [(s, c) for s, c in zip(d["sems"], d["cnt"]) if c > 0]
        for e in engines:
            for (s, v) in targets:
                if s is self.sem[e]:
                    continue
                if self.seen[e].get(id(s), 0) >= v:
                    continue
                self.seen[e][id(s)] = v
                self.eng[e].wait_ge(s, v)
                self.ninst += 1

    def finish(self):
        for q in ("sp", "pool"):
            d = self.dq[q]
            for s, c in zip(d["sems"], d["cnt"]):
                if c > 0:
                    self.nc.sync.wait_ge(s, c)


def K(name, idxs=None):
    if idxs is None:
        return [name]
    return [(name, i) for i in idxs]


def pstride(t):
    return t[:].ap[0][0]


_UNIQ = [0]


def uniq(n):
    _UNIQ[0] += 1
    return f"{n}_{_UNIQ[0]}"


def build(NT=SEQ, nstages=None, first_layer_only=False):
    nc = bass.Bass("TRN2", target_bir_lowering=False)
    NB = NT // 128

    def din(name, shape, dt=F32):
        return nc.dram_tensor(name, list(shape), dt, kind="ExternalInput").ap()

    x_in = din("x", [NT, D])
    pos_in = din("pos", [1, NT], I32)
    consts_in = din("consts", [128, C_NC])
    gains_in = din("gains", [128, G_NC])
    w_gate = din("ffn_w_gate", [DEPTH, 2, D, DFF])
    w_up = din("ffn_w_up", [DEPTH, 2, D, DFF])
    w_down = din("ffn_w_down", [DEPTH, 2, DFF, D])
    ret_w_in = din("ret_w_in", [N_SELF, D, RPROJ])
    ret_w_o = din("ret_w_o", [N_SELF, H * RDV, D])
    kv_w_down = din("kv_w_down", [D, KVL + ROPE])
    kv_w_up = din("kv_w_up", [KVL, H * (NOPE + 128)])
    mla_w_dq = din("mla_w_dq", [2, D, QL])
    mla_w_uq = din("mla_w_uq", [2, QL, H * (NOPE + ROPE)])
    mla_w_o = din("mla_w_o", [2, H * 128, D])
    out_d = nc.dram_tensor("out", [NT, D], F32, kind="ExternalOutput").ap()

    def dscr(name, shape, dt):
        return nc.dram_tensor(name, list(shape), dt, kind="Internal").ap()

    xs = dscr("xs", [8, 128, NT], F32)
    tabR = dscr("tabR", [2, 128, NT], F32)
    tabM = dscr("tabM", [2, 64, NT], F32)
    knT_d = dscr("knT", [H, 128, NT], BF16)
    kpeT_d = dscr("kpeT", [64, NT], BF16)
    vs_d = dscr("vs", [H, 128, NB, 128], BF16)

    with contextlib.ExitStack() as top:
        P = Prog(nc, top)
        Wt = top.enter_context(nc.sbuf_tensor("W", [128, WSLOTS, 1024], BF16))
        consts = top.enter_context(nc.sbuf_tensor("consts_sb", [128, C_NC], F32))
        gains = top.enter_context(nc.sbuf_tensor("gains_sb", [128, G_NC], F32))
        identb = top.enter_context(nc.sbuf_tensor("identb", [128, 128], BF16))
        onesb = top.enter_context(nc.sbuf_tensor("onesb", [128, 128], BF16))
        maskb = top.enter_context(nc.sbuf_tensor("maskb", [128, 128], BF16))
        PS = top.enter_context(nc.psum_tensor("PS", [128, 8, 512], F32))
        ident = consts[:, C_ID:C_ID + 128]

        def bank(i):
            return PS[:, i, :]

        def bankb(i):
            return PS[:, i, :].bitcast(BF16)

        def BK(i):
            return ("PS", i)

        P.dma("sp", consts[:], consts_in[:, :], writes=K("consts"))
        P.dma("sp", gains[:], gains_in[:, :], writes=K("gains"))
        P.op("dve", lambda e: e.tensor_copy(out=identb[:], in_=consts[:, C_ID:C_ID + 128]), reads=K("consts"), writes=K("identb"))
        P.op("dve", lambda e: e.tensor_copy(out=maskb[:], in_=consts[:, C_MASK:C_MASK + 128]), reads=K("consts"), writes=K("maskb"))
        P.op("pool", lambda e: e.memset(onesb[:], 1.0), writes=K("onesb"))
        epsc = top.enter_context(nc.sbuf_tensor("epsc", [128, 1], F32))
        P.op("pool", lambda e: e.memset(epsc[:], EPS), writes=K("epsc"))

        def wview(slot0, kch, ncols):
            tot = kch * ncols
            assert tot % 1024 == 0 or True
            nsl = (tot + 1023) // 1024
            assert slot0 + nsl <= WSLOTS
            ps_ = pstride(Wt)
            ap = bass.AP(Wt, slot0 * 1024, [[ps_, 128], [ncols, kch], [1, ncols]])
            return ap, list(range(slot0, slot0 + nsl))

        def wload(src2d, slot0, kch, ncols):
            view, slots = wview(slot0, kch, ncols)
            for k in range(kch):
                s_lo = slot0 + (k * ncols) // 1024
                s_hi = slot0 + ((k + 1) * ncols - 1) // 1024
                P.dma("pool", view[:, k, :], src2d[k * 128:(k + 1) * 128, :],
                      writes=K("W", range(s_lo, s_hi + 1)))
            return view, K("W", slots)

        def mm_group(outap, pairs, reads, writes, first=True, last=True):
            def fn(e):
                ins = None
                n = len(pairs)
                for i, (l, r) in enumerate(pairs):
                    ins = e.matmul(outap, l, r, start=(first and i == 0), stop=(last and i == n - 1))
                return ins
            return P.op("pe", fn, reads=reads, writes=writes)

        def norm_T(srcs, src_keys, npart, gcols, outs, out_keys, sq, sq_keys, rs, rs_key, ssb, Tn, sq_eng="act"):
            n = len(srcs)
            for i in range(n):
                if sq_eng == "act":
                    P.op("act", lambda e, i=i: e.activation(out=sq[i], in_=srcs[i], func=AF.Square),
                         reads=src_keys[i], writes=sq_keys[i])
                else:
                    P.op(sq_eng, lambda e, i=i: e.tensor_tensor(out=sq[i], in0=srcs[i], in1=srcs[i], op=ALU.mult),
                         reads=src_keys[i], writes=sq_keys[i])
            mm_group(bank(ssb)[0:npart, 0:Tn], [(onesb[0:npart, 0:npart], sq[i]) for i in range(n)],
                     reads=K("onesb") + [k for ks in sq_keys for k in ks], writes=[BK(ssb)])
            P.op("act", lambda e: e.activation(out=rs, in_=bank(ssb)[0:npart, 0:Tn], func=AF.Ln,
                                               scale=1.0 / (n * npart), bias=epsc[0:npart, 0:1]),
                 reads=[BK(ssb)] + K("epsc"), writes=rs_key)
            P.op("act", lambda e: e.activation(out=rs, in_=rs, func=AF.Exp, scale=-0.5), reads=rs_key, writes=rs_key)
            for i in range(n):
                P.op("dve", lambda e, i=i: e.scalar_tensor_tensor(
                    out=outs[i], in0=srcs[i], scalar=gains[0:npart, gcols[i]:gcols[i] + 1], in1=rs,
                    op0=ALU.mult, op1=ALU.mult),
                    reads=src_keys[i] + rs_key + K("gains"), writes=out_keys[i])

        def rope_T(src, src_key, npart, cosT, sinT, tab_key, t1, t2, tkeys, out, out_key, eng2="pool"):
            hf = npart // 2
            P.op("dve", lambda e: e.tensor_tensor(out=t1, in0=src, in1=cosT, op=ALU.mult),
                 reads=src_key + tab_key, writes=[tkeys[0]])
            P.op(eng2, lambda e: e.tensor_tensor(out=t2[0:hf], in0=src[hf:npart], in1=sinT[hf:npart], op=ALU.mult),
                 reads=src_key + tab_key, writes=[tkeys[1]])
            P.op("dve", lambda e: e.tensor_tensor(out=t2[hf:npart], in0=src[0:hf], in1=sinT[0:hf], op=ALU.mult),
                 reads=src_key + tab_key, writes=[tkeys[2]])
            P.op("dve", lambda e: e.tensor_tensor(out=out, in0=t1, in1=t2[0:npart], op=ALU.add),
                 reads=list(tkeys), writes=out_key)

        def prologue():
            CH = min(1024, NT)
            with contextlib.ExitStack() as st:
                sbt = lambda n, s, d=F32: st.enter_context(nc.sbuf_tensor(uniq(n), s, d))
                pi = sbt("pi", [128, CH], I32)
                pf = sbt("pf", [128, CH])
                y = sbt("py", [128, CH])
                ki = sbt("pki", [128, CH], I32)
                kf = sbt("pkf", [128, CH])
                tb = sbt("ptb", [128, 2, CH])
                c1 = 6.28125
                c2 = 2.0 * math.pi - c1
                for c0 in range(0, NT, CH):
                    pos_b = bass.AP(pos_in.tensor, c0, [[0, 128], [1, CH]])
                    P.dma("sp", pi[:], pos_b, writes=K("pi"))
                    P.op("dve", lambda e: e.tensor_copy(out=pf[:], in_=pi[:]), reads=K("pi"), writes=K("pf"))
                    for (npart, invc, sgc, dst) in ((128, C_INVR, C_SGR, tabR), (64, C_INVM, C_SGM, tabM)):
                        for which in (0, 1):
                            shift = math.pi / 2 if which == 0 else 0.0
                            P.op("dve", lambda e: e.tensor_scalar(out=y[0:npart], in0=pf[0:npart], scalar1=consts[0:npart, invc:invc + 1],
                                                                  scalar2=shift, op0=ALU.mult, op1=ALU.add),
                                 reads=K("pf") + K("consts"), writes=K("py"))
                            P.op("dve", lambda e: e.tensor_scalar(out=ki[0:npart], in0=y[0:npart], scalar1=1.0 / (2 * math.pi), scalar2=None, op0=ALU.mult),
                                 reads=K("py"), writes=K("pki"))
                            P.op("dve", lambda e: e.tensor_copy(out=kf[0:npart], in_=ki[0:npart]), reads=K("pki"), writes=K("pkf"))
                            P.op("dve", lambda e: e.scalar_tensor_tensor(out=y[0:npart], in0=kf[0:npart], scalar=-c1, in1=y[0:npart], op0=ALU.mult, op1=ALU.add),
                                 reads=K("pkf") + K("py"), writes=K("py"))
                            P.op("dve", lambda e: e.scalar_tensor_tensor(out=y[0:npart], in0=kf[0:npart], scalar=-c2, in1=y[0:npart], op0=ALU.mult, op1=ALU.add),
                                 reads=K("pkf") + K("py"), writes=K("py"))
                            P.op("dve", lambda e: e.tensor_scalar(out=y[0:npart], in0=y[0:npart], scalar1=-math.pi, scalar2=math.pi, op0=ALU.max, op1=ALU.min),
                                 reads=K("py"), writes=K("py"))
                            if which == 0:
                                P.op("act", lambda e: e.activation(out=tb[0:npart, 0, :], in_=y[0:npart], func=AF.Sin),
                                     reads=K("py"), writes=K("ptb", [0]))
                            else:
                                P.op("act", lambda e: e.activation(out=tb[0:npart, 1, :], in_=y[0:npart], func=AF.Sin,
                                                                   scale=consts[0:npart, sgc:sgc + 1]),
                                     reads=K("py") + K("consts"), writes=K("ptb", [1]))
                        P.dma("sp", dst[:, :, c0:c0 + CH].rearrange("w p t -> p w t"), tb[0:npart, :, :], reads=K("ptb", [0, 1]))
                P.barrier()

        def load_xT(xT, t, T, first, stg):
            if not first:
                P.dma("sp", xT[:], xs[:, :, t * T:(t + 1) * T].rearrange("c p t -> p c t"), writes=K("xT", range(8)))
                return
            for s in range(T // 128):
                sg = stg[s % 2]
                P.dma("sp", sg[:], x_in[t * T + s * 128: t * T + (s + 1) * 128, :], writes=K("stg", [s % 2]))
                for hb in range(2):
                    b = 6 + hb
                    def fn(e, hb=hb, sg=sg, b=b):
                        ins = None
                        for c in range(4):
                            ins = e.transpose(out=bank(b)[:, c * 128:(c + 1) * 128], in_=sg[:, (hb * 4 + c) * 128:(hb * 4 + c + 1) * 128], identity=ident)
                        return ins
                    P.op("pe", fn, reads=K("stg", [s % 2]) + K("consts"), writes=[BK(b)])
                    P.op("act" if hb == 0 else "dve",
                         (lambda e, hb=hb, b=b, s=s: e.activation(out=xT[:, hb * 4:(hb + 1) * 4, s * 128:(s + 1) * 128],
                                                                  in_=bank(b).rearrange("p (c j) -> p c j", c=4), func=AF.Copy)) if hb == 0 else
                         (lambda e, hb=hb, b=b, s=s: e.tensor_copy(out=xT[:, hb * 4:(hb + 1) * 4, s * 128:(s + 1) * 128],
                                                                   in_=bank(b).rearrange("p (c j) -> p c j", c=4))),
                         reads=[BK(b)], writes=K("xT", range(hb * 4, hb * 4 + 4)))

        def store_xT(xT, t, T, last, stg):
            if not last:
                P.dma("sp", xs[:, :, t * T:(t + 1) * T].rearrange("c p t -> p c t"), xT[:], reads=K("xT", range(8)))
                return
            for s in range(T // 128):
                sg = stg[s % 2]
                for hb in range(2):
                    b = 6 + hb
                    def fn(e, hb=hb, b=b, s=s):
                        ins = None
                        for c in range(4):
                            ins = e.transpose(out=bank(b)[:, c * 128:(c + 1) * 128], in_=xT[:, hb * 4 + c, s * 128:(s + 1) * 128], identity=ident)
                        return ins
                    P.op("pe", fn, reads=K("xT", range(hb * 4, hb * 4 + 4)) + K("consts"), writes=[BK(b)])
                    if hb == 0:
                        P.op("act", lambda e, b=b, sg=sg: e.activation(out=sg[:, 0:512], in_=bank(b), func=AF.Copy),
                             reads=[BK(b)], writes=[("stg", s % 2, 0)] + K("stg", [s % 2]))
                    else:
                        P.op("dve", lambda e, b=b, sg=sg: e.tensor_copy(out=sg[:, 512:1024], in_=bank(b)),
                             reads=[BK(b)], writes=[("stg", s % 2, 1)])
                P.dma("sp", out_d[t * T + s * 128: t * T + (s + 1) * 128, :], sg[:],
                      reads=K("stg", [s % 2]) + [("stg", s % 2, 0), ("stg", s % 2, 1)])

        def store_chunk_prefetch(xT, f, t, T, NTI):
            P.dma("sp", xs[f, :, t * T:(t + 1) * T], xT[:, f, :], reads=K("xT", [f]))
            if t + 1 < NTI:
                P.dma("sp", xT[:, f, :], xs[f, :, (t + 1) * T:(t + 2) * T], writes=K("xT", [f]))

        def ffn_stage(layer, which, first, last):
            T = 512
            NTI = NT // T
            gbase = G_NORM + (layer * 3 + (0 if which == 0 else 2)) * 8
            wg, _ = wview(0, 8, DFF)
            wu, _ = wview(22, 8, DFF)
            wd, _ = wview(44, NCH, D)
            CG = [(0, 6), (6, 12), (12, 17), (17, 22)]
            cgrp = {c: gi for gi, (a, b) in enumerate(CG) for c in range(a, b)}
            for gi, (a, b) in enumerate(CG):
                for (wv, src, nm) in ((wg, w_gate, "Wg"), (wu, w_up, "Wu")):
                    for k in range(8):
                        P.dma("pool", wv[:, k, a * 128:b * 128], src[layer, which][k * 128:(k + 1) * 128, a * 128:b * 128],
                              writes=[(nm, gi, k)])
            for c in range(NCH):
                P.dma("pool", wd[:, c, :], w_down[layer, which][c * 128:(c + 1) * 128, :], writes=[("Wd", c)])
            kd = [("Wd", c) for c in range(NCH)]
            with contextlib.ExitStack() as st:
                sbt = lambda n, s, d=F32: st.enter_context(nc.sbuf_tensor(uniq(n), s, d))
                xT = sbt("xT", [128, 8, T])
                hT = sbt("hT", [128, 8, T], BF16)
                aT = sbt("aT", [128, NCH, T], BF16)
                sg = [sbt(f"sg{i}", [128, T]) for i in range(2)]
                rs = sbt("rs", [128, T])
                stg = [sbt(f"stg{i}", [128, D]) for i in range(2)] if (first or last) else None
                for t in range(NTI):
                    if first or t == 0:
                        load_xT(xT, t, T, first, stg)
                    norm_T([xT[:, c, :] for c in range(8)], [K("xT", [c]) for c in range(8)], 128,
                           [gbase + c for c in range(8)],
                           [hT[:, c, :] for c in range(8)], [K("hT", [c]) for c in range(8)],
                           [aT[:, c, :] for c in range(8)], [K("aT", [c]) for c in range(8)],
                           rs[:], K("rs"), 6, T)
                    for c in range(NCH):
                        bg, bu = c % 2, 2 + c % 2
                        cs = slice(c * 128, (c + 1) * 128)
                        if c == 0:
                            for k in range(8):
                                mm_group(bank(bg), [(wg[:, k, cs], hT[:, k, :])], reads=[("Wg", 0, k)] + K("hT", [k]), writes=[BK(bg)],
                                         first=(k == 0), last=(k == 7))
                            for k in range(8):
                                mm_group(bank(bu), [(wu[:, k, cs], hT[:, k, :])], reads=[("Wu", 0, k)] + K("hT", [k]), writes=[BK(bu)],
                                         first=(k == 0), last=(k == 7))
                        else:
                            mm_group(bank(bg), [(wg[:, k, cs], hT[:, k, :]) for k in range(8)],
                                     reads=[("Wg", cgrp[c], k) for k in range(8)] + K("hT", range(8)), writes=[BK(bg)])
                            mm_group(bank(bu), [(wu[:, k, cs], hT[:, k, :]) for k in range(8)],
                                     reads=[("Wu", cgrp[c], k) for k in range(8)] + K("hT", range(8)), writes=[BK(bu)])
                        P.op("act", lambda e, c=c, bg=bg: e.activation(out=sg[c % 2][:], in_=bank(bg), func=AF.Silu),
                             reads=[BK(bg)], writes=K("sg", [c % 2]))
                        P.op("dve", lambda e, c=c, bu=bu: e.tensor_tensor(out=aT[:, c, :], in0=bank(bu), in1=sg[c % 2][:], op=ALU.mult),
                             reads=[BK(bu)] + K("sg", [c % 2]), writes=K("aT", [c]))
                    for f in range(8):
                        bo = 4 + f % 2
                        fs = slice(f * 128, (f + 1) * 128)
                        mm_group(bank(bo), [(wd[:, c, fs], aT[:, c, :]) for c in range(NCH)],
                                 reads=kd + K("aT", range(NCH)), writes=[BK(bo)])
                        P.op("dve", lambda e, f=f, bo=bo: e.scalar_tensor_tensor(out=xT[:, f, :], in0=bank(bo), scalar=0.5, in1=xT[:, f, :],
                                                                                 op0=ALU.mult, op1=ALU.add),
                             reads=[BK(bo)] + K("xT", [f]), writes=K("xT", [f]))
                        if not last:
                            if first:
                                P.dma("sp", xs[f, :, t * T:(t + 1) * T], xT[:, f, :], reads=K("xT", [f]))
                            else:
                                store_chunk_prefetch(xT, f, t, T, NTI)
                    if last:
                        store_xT(xT, t, T, True, stg)
                        if t + 1 < NTI and not first:
                            load_xT(xT, t + 1, T, False, None)
                P.barrier()

        def ret_stage(layer):
            T = 256
            NTI = NT // T
            gbase = G_NORM + (layer * 3 + 1) * 8
            gam = [1.0 - 2.0 ** (-5.0 - h) for h in range(H)]
            gC = [g ** 128 for g in gam]
            wi, ki_ = wload(ret_w_in[layer], 0, 8, RPROJ)
            wo, ko_ = wload(ret_w_o[layer], 48, 16, D)
            for cc in range(16):
                P.op("dve", lambda e, cc=cc: e.tensor_scalar(out=wo[:, cc, :], in0=wo[:, cc, :],
                                                             scalar1=gains[:, G_GN + layer * 16 + cc:G_GN + layer * 16 + cc + 1],
                                                             scalar2=None, op0=ALU.mult),
                     reads=K("W", [48 + cc]) + K("gains"), writes=K("W", [48 + cc]))
            with contextlib.ExitStack() as st:
                sbt = lambda n, s, d=F32: st.enter_context(nc.sbuf_tensor(uniq(n), s, d))
                xT = sbt("xT", [128, 8, T])
                hT = sbt("hT", [128, 8, T], BF16)
                yT = sbt("yT", [128, 16, T], BF16)
                rs = sbt("rs", [128, T])
                qT = sbt("qT", [128, 8, T], BF16)
                kT = sbt("kT", [128, 8, T], BF16)
                qf = [sbt(f"qf{i}", [128, T]) for i in range(2)]
                t1s = sbt("t1", [128, T])
                t1 = [t1s, t1s]
                t2 = [sbt(f"t2{i}", [128, T]) for i in range(2)]
                tab = sbt("tab", [128, 2, T])
                vd = sbt("vd", [128, H * RDV], BF16)
                gs = sbt("gs", [128, H * RDV], BF16)
                ktok = sbt("ktok", [128, 8, 128], BF16)
                Ssb = sbt("Ssb", [128, 4, 128], BF16)
                state = sbt("state", [128, H, RDV])
                G = sbt("G", [128, H, RDV], BF16)
                sq = sbt("sq", [128, 1024])
                yn = sbt("yn", [128, 1024])
                y = sbt("y", [128, H * RDV], BF16)
                stt = sbt("stt", [128, 8, 4])
                P.op("pool", lambda e: e.memset(state[:], 0.0), writes=K("state", range(8)))
                P.op("pool", lambda e: e.memset(G[:], 0.0), writes=K("G", range(8)))
                ps_c = pstride(consts)
                for t in range(NTI):
                    if t == 0:
                        load_xT(xT, t, T, False, None)
                    P.dma("sp", tab[:], tabR[:, :, t * T:(t + 1) * T].rearrange("w p t -> p w t"), writes=K("tab"))
                    norm_T([xT[:, c, :] for c in range(8)], [K("xT", [c]) for c in range(8)], 128,
                           [gbase + c for c in range(8)],
                           [hT[:, c, :] for c in range(8)], [K("hT", [c]) for c in range(8)],
                           [yT[:, c, :] for c in range(8)], [K("yT", [c]) for c in range(8)],
                           rs[:], K("rs"), 4, T)
                    i = 0
                    for h in range(H):
                        for (dst, dkey, col0) in ((qT, "qT", 0), (kT, "kT", 1024)):
                            b = i % 2
                            i += 1
                            cs = slice(col0 + h * 128, col0 + (h + 1) * 128)
                            if i == 1:
                                for k in range(8):
                                    mm_group(bank(b)[:, 0:T], [(wi[:, k, cs], hT[:, k, :])], reads=ki_ + K("hT", [k]), writes=[BK(b)],
                                             first=(k == 0), last=(k == 7))
                            else:
                                mm_group(bank(b)[:, 0:T], [(wi[:, k, cs], hT[:, k, :]) for k in range(8)],
                                         reads=ki_ + K("hT", range(8)), writes=[BK(b)])
                            P.op("act", lambda e, b=b: e.activation(out=qf[b][:], in_=bank(b)[:, 0:T], func=AF.Copy),
                                 reads=[BK(b)], writes=K("qf", [b]))
                            rope_T(qf[b][:], K("qf", [b]), 128, tab[:, 0, :], tab[:, 1, :], K("tab"), t1[b][:], t2[b][:],
                                   [("t1",), ("t2", b, 0), ("t2", b, 1)], dst[:, h, :], K(dkey, [h]))
                    def vg(n, grps):
                        tk = slice(n * 128, (n + 1) * 128)
                        for grp in grps:
                            b = 2 + grp % 2
                            col = 2048 + grp * 512
                            mm_group(bank(b), [(hT[:, k, tk], wi[:, k, col:col + 512]) for k in range(8)],
                                     reads=ki_ + K("hT", range(8)), writes=[BK(b)])
                            if grp < 4:
                                kdb = bass.AP(consts, C_KD + grp * 2, [[ps_c, 128], [1, 2], [0, RDV]])
                                P.op("dve", lambda e, b=b, grp=grp, kdb=kdb: e.tensor_tensor(
                                    out=vd[:, grp * 512:(grp + 1) * 512].rearrange("p (a v) -> p a v", a=2),
                                    in0=bank(b).rearrange("p (a v) -> p a v", a=2), in1=kdb, op=ALU.mult),
                                    reads=[BK(b)] + K("consts"), writes=K("vd", [grp]))
                            else:
                                g4 = grp - 4
                                P.op("act", lambda e, b=b, g4=g4: e.activation(out=gs[:, g4 * 512:(g4 + 1) * 512], in_=bank(b), func=AF.Silu),
                                     reads=[BK(b)], writes=K("gs", [g4]))

                    def ktr(n):
                        tk = slice(n * 128, (n + 1) * 128)
                        def fnk(e):
                            ins = None
                            for h in range(H):
                                ins = e.transpose(out=bankb(4)[:, h * 128:(h + 1) * 128], in_=kT[:, h, tk], identity=identb[:])
                            return ins
                        P.op("pe", fnk, reads=K("kT", range(8)) + K("identb"), writes=[BK(4)])
                        P.op("act", lambda e: e.activation(out=ktok[:].rearrange("p h d -> p (h d)"), in_=bankb(4), func=AF.Copy),
                             reads=[BK(4)], writes=K("ktok"))

                    def SOU(n, hg):
                        tk = slice(n * 128, (n + 1) * 128)
                        ob = 6 if hg == 0 else 0
                        hs = list(range(hg * 4, hg * 4 + 4))
                        def fns(e):
                            ins = None
                            for j, h in enumerate(hs):
                                ins = e.matmul(bank(5)[:, j * 128:(j + 1) * 128], kT[:, h, tk], qT[:, h, tk], start=True, stop=True)
                            return ins
                        P.op("pe", fns, reads=K("kT", hs) + K("qT", hs), writes=[BK(5)])
                        mkb = bass.AP(maskb, 0, [[pstride(maskb), 128], [0, 4], [1, 128]])
                        P.op("dve", lambda e: e.tensor_tensor(out=Ssb[:], in0=bank(5).rearrange("p (a i) -> p a i", a=4), in1=mkb, op=ALU.mult),
                             reads=[BK(5)] + K("maskb"), writes=K("Ssb"))
                        def fno(e):
                            ins = None
                            for j, h in enumerate(hs):
                                o = PS[:, ob + j // 2, (j % 2) * 256:(j % 2 + 1) * 256]
                                e.matmul(o, Ssb[:, j, :], vd[:, h * 256:(h + 1) * 256], start=True, stop=False)
                                ins = e.matmul(o, qT[:, h, tk], G[:, h, :], start=False, stop=True)
                            return ins
                        P.op("pe", fno, reads=K("Ssb") + K("vd", [hg * 2, hg * 2 + 1]) + K("qT", hs) + K("G", hs), writes=[BK(ob), BK(ob + 1)])
                        def fnu(e):
                            ins = None
                            for j, h in enumerate(hs):
                                o = PS[:, 2 + j // 2, (j % 2) * 256:(j % 2 + 1) * 256]
                                ins = e.matmul(o, ktok[:, h, :], vd[:, h * 256:(h + 1) * 256], start=True, stop=True)
                            return ins
                        P.op("pe", fnu, reads=K("ktok") + K("vd", [hg * 2, hg * 2 + 1]), writes=[BK(2), BK(3)])
                        for j, h in enumerate(hs):
                            o = PS[:, 2 + j // 2, (j % 2) * 256:(j % 2 + 1) * 256]
                            P.op("dve", lambda e, h=h, o=o: e.scalar_tensor_tensor(out=state[:, h, :], in0=state[:, h, :], scalar=gC[h], in1=o,
                                                                                   op0=ALU.mult, op1=ALU.add),
                                 reads=[BK(2 + j // 2)] + K("state", [h]), writes=K("state", [h]))
                            P.op("act", lambda e, h=h: e.activation(out=G[:, h, :], in_=state[:, h, :], func=AF.Copy, scale=gC[h]),
                                 reads=K("state", [h]), writes=K("G", [h]))

                    def GN(n, hg):
                        ob = 6 if hg == 0 else 0
                        hs = list(range(hg * 4, hg * 4 + 4))
                        okeys = [BK(ob), BK(ob + 1)]
                        o4 = PS[:, ob:ob + 2, :].rearrange("p b (a v) -> p (b a) v", a=2)
                        oflat = PS[:, ob:ob + 2, :].rearrange("p b c -> p (b c)")
                        P.op("act", lambda e: e.activation(out=sq[:], in_=oflat, func=AF.Square), reads=okeys, writes=K("sq"))
                        P.op("dve", lambda e: e.tensor_reduce(out=stt[:, 0, :], in_=o4, axis=AX.X, op=ALU.add), reads=okeys, writes=K("stt", [0]))
                        P.op("dve", lambda e: e.tensor_reduce(out=stt[:, 1, :], in_=sq[:].rearrange("p (a v) -> p a v", a=4), axis=AX.X, op=ALU.add),
                             reads=K("sq"), writes=K("stt", [1]))
                        P.op("dve", lambda e: e.tensor_scalar(out=stt[:, 2, :], in0=stt[:, 0, :], scalar1=1.0 / RDV, scalar2=None, op0=ALU.mult),
                             reads=K("stt", [0]), writes=K("stt", [2]))
                        P.op("dve", lambda e: e.tensor_tensor(out=stt[:, 3, :], in0=stt[:, 2, :], in1=stt[:, 2, :], op=ALU.mult),
                             reads=K("stt", [2]), writes=K("stt", [3]))
                        P.op("dve", lambda e: e.scalar_tensor_tensor(out=stt[:, 3, :], in0=stt[:, 1, :], scalar=1.0 / RDV, in1=stt[:, 3, :],
                                                                     op0=ALU.mult, op1=ALU.subtract),
                             reads=K("stt", [1, 3]), writes=K("stt", [3]))
                        P.op("dve", lambda e: e.tensor_tensor(out=stt[:, 3, :], in0=stt[:, 3, :], in1=consts[:, C_EPSR + hg * 4:C_EPSR + hg * 4 + 4], op=ALU.add),
                             reads=K("stt", [3]) + K("consts"), writes=K("stt", [3]))
                        P.op("act", lambda e: e.activation(out=stt[:, 4, :], in_=stt[:, 3, :], func=AF.Sqrt), reads=K("stt", [3]), writes=K("stt", [4]))
                        P.op("dve", lambda e: e.reciprocal(out=stt[:, 4, :], in_=stt[:, 4, :]), reads=K("stt", [4]), writes=K("stt", [4]))
                        P.op("dve", lambda e: e.scalar_tensor_tensor(out=stt[:, 5, :], in0=stt[:, 2, :], scalar=-1.0, in1=stt[:, 4, :],
                                                                     op0=ALU.mult, op1=ALU.mult),
                             reads=K("stt", [2, 4]), writes=K("stt", [5]))
                        for j, h in enumerate(hs):
                            o = PS[:, ob + j // 2, (j % 2) * 256:(j % 2 + 1) * 256]
                            P.op("act", lambda e, j=j, o=o: e.activation(out=yn[:, j * 256:(j + 1) * 256], in_=o, func=AF.Identity,
                                                                         scale=stt[:, 4, j:j + 1], bias=stt[:, 5, j:j + 1]),
                                 reads=[BK(ob + j // 2)] + K("stt", [4, 5]), writes=[("yn", j)])
                        P.op("pool", lambda e: e.tensor_tensor(out=y[:, hg * 1024:(hg + 1) * 1024], in0=yn[:], in1=gs[:, hg * 1024:(hg + 1) * 1024], op=ALU.mult),
                             reads=[("yn", j) for j in range(4)] + K("gs", [hg * 2, hg * 2 + 1]), writes=K("y", [hg]))

                    def ytr(n):
                        tk = slice(n * 128, (n + 1) * 128)
                        for hb in range(2):
                            bb = 2 + hb
                            def fny(e, hb=hb, bb=bb):
                                ins = None
                                for c in range(8):
                                    cc = hb * 8 + c
                                    ins = e.transpose(out=bankb(bb)[:, c * 128:(c + 1) * 128], in_=y[:, cc * 128:(cc + 1) * 128], identity=identb[:])
                                return ins
                            P.op("pe", fny, reads=K("y", [hb]) + K("identb"), writes=[BK(bb)])
                            P.op("act" if hb == 0 else "dve",
                                 (lambda e, hb=hb, bb=bb: e.activation(out=yT[:, hb * 8:(hb + 1) * 8, tk], in_=bankb(bb).rearrange("p (c j) -> p c j", c=8), func=AF.Copy)) if hb == 0 else
                                 (lambda e, hb=hb, bb=bb: e.tensor_copy(out=yT[:, hb * 8:(hb + 1) * 8, tk], in_=bankb(bb).rearrange("p (c j) -> p c j", c=8))),
                                 reads=[BK(bb)], writes=K("yT", range(hb * 8, hb * 8 + 8)))

                    NCT = T // 128
                    vg(0, range(0, 4)); ktr(0); SOU(0, 0); SOU(0, 1); vg(0, range(4, 8))
                    for n in range(NCT):
                        if n + 1 < NCT:
                            vg(n + 1, range(0, 4))
                        GN(n, 0); GN(n, 1)
                        if n + 1 < NCT:
                            ktr(n + 1); SOU(n + 1, 0); SOU(n + 1, 1)
                        ytr(n)
                        if n + 1 < NCT:
                            vg(n + 1, range(4, 8))
                    for f in range(8):
                        bo = 2 + f % 2
                        fs = slice(f * 128, (f + 1) * 128)
                        mm_group(bank(bo)[:, 0:T], [(wo[:, c, fs], yT[:, c, :]) for c in range(16)],
                                 reads=ko_ + K("yT", range(16)), writes=[BK(bo)])
                        P.op("dve", lambda e, f=f, bo=bo: e.tensor_tensor(out=xT[:, f, :], in0=bank(bo)[:, 0:T], in1=xT[:, f, :], op=ALU.add),
                             reads=[BK(bo)] + K("xT", [f]), writes=K("xT", [f]))
                        store_chunk_prefetch(xT, f, t, T, NTI)
                P.barrier()

        def kv_stage():
            T = 512
            NTI = NT // T
            wdn, kdn = wload(kv_w_down, 0, 8, KVL + ROPE)
            wup, kup = wload(kv_w_up, 4, 2, 2048)
            with contextlib.ExitStack() as st:
                sbt = lambda n, s, d=F32: st.enter_context(nc.sbuf_tensor(uniq(n), s, d))
                xT = sbt("xT", [128, 8, T])
                hT = sbt("hT", [128, 8, T], BF16)
                sqb = sbt("sqb", [128, 8, T], BF16)
                rs = sbt("rs", [128, T])
                cnT = sbt("cnT", [128, 2, T], BF16)
                kpf = sbt("kpf", [64, T])
                t1 = sbt("t1", [128, T])
                t2 = sbt("t2", [128, T])
                tab = sbt("tab", [64, 2, T])
                kpo = sbt("kpo", [64, T], BF16)
                kno = sbt("kno", [128, H, T], BF16)
                vsb = sbt("vsb", [128, 4, 1024], BF16)
                for t in range(NTI):
                    load_xT(xT, t, T, False, None)
                    P.dma("sp", tab[:], tabM[:, :, t * T:(t + 1) * T].rearrange("w p t -> p w t"), writes=K("tab"))
                    norm_T([xT[:, c, :] for c in range(8)], [K("xT", [c]) for c in range(8)], 128,
                           [G_KVN + c for c in range(8)],
                           [hT[:, c, :] for c in range(8)], [K("hT", [c]) for c in range(8)],
                           [sqb[:, c, :] for c in range(8)], [K("sqb", [c]) for c in range(8)],
                           rs[:], K("rs"), 7, T)
                    for m in range(2):
                        mm_group(bank(m), [(wdn[:, k, m * 128:(m + 1) * 128], hT[:, k, :]) for k in range(8)],
                                 reads=kdn + K("hT", range(8)), writes=[BK(m)])
                    mm_group(bank(2)[0:64, :], [(wdn[:, k, 256:320], hT[:, k, :]) for k in range(8)],
                             reads=kdn + K("hT", range(8)), writes=[BK(2)])
                    norm_T([bank(0), bank(1)], [[BK(0)], [BK(1)]], 128, [G_LAT, G_LAT + 1],
                           [cnT[:, 0, :], cnT[:, 1, :]], [K("cnT", [0]), K("cnT", [1])],
                           [sqb[:, 0, :], sqb[:, 1, :]], [K("sqb", [0]), K("sqb", [1])], rs[:], K("rs"), 3, T)
                    norm_T([bank(2)[0:64, :]], [[BK(2)]], 64, [G_KR], [kpf[:]], [K("kpf")],
                           [sqb[0:64, 2, :]], [K("sqb", [2])], rs[0:64, :], K("rs"), 3, T)
                    rope_T(kpf[:], K("kpf"), 64, tab[:, 0, :], tab[:, 1, :], K("tab"), t1[0:64, :], t2[0:64, :],
                           [("t1",), ("t2", 0), ("t2", 1)], kpo[:], K("kpo"))
                    P.dma("sp", kpeT_d[:, t * T:(t + 1) * T], kpo[:], reads=K("kpo"))
                    for h in range(H):
                        b = 4 + h % 2
                        mm_group(bank(b), [(wup[:, m, h * 256:h * 256 + 128], cnT[:, m, :]) for m in range(2)],
                                 reads=kup + K("cnT", [0, 1]), writes=[BK(b)])
                        norm_T([bank(b)], [[BK(b)]], 128, [G_KN], [kno[:, h, :]], [K("kno", [h])],
                               [sqb[:, 3 + h % 2, :]], [K("sqb", [3 + h % 2])], rs[:], K("rs"), 6, T)
                    P.dma("sp", knT_d[:, :, t * T:(t + 1) * T].rearrange("h p t -> p h t"), kno[:], reads=K("kno", range(8)))
                    psw = pstride(Wt)
                    for s in range(4):
                        for half in range(2):
                            b = half
                            pairs = []
                            for m in range(2):
                                rhs = bass.AP(Wt, 4 * 1024 + m * 2048 + half * 1024 + 128, [[psw, 128], [256, 4], [1, 128]])
                                pairs.append((cnT[:, m, s * 128:(s + 1) * 128], rhs))
                            mm_group(bank(b), pairs, reads=kup + K("cnT", [0, 1]), writes=[BK(b)])
                            P.op("act", lambda e, s=s, half=half, b=b: e.activation(out=vsb[:, s, half * 512:(half + 1) * 512], in_=bank(b), func=AF.Copy),
                                 reads=[BK(b)], writes=K("vsb", [s]))
                    for h in range(H):
                        P.dma("sp", vs_d[h, :, t * 4:(t + 1) * 4, :], vsb[:, :, h * 128:(h + 1) * 128], reads=K("vsb", range(4)))
                P.barrier()

        def mla_stage(layer):
            T = 512
            NTI = NT // T
            j_ = layer - N_SELF
            gbase = G_NORM + (layer * 3 + 1) * 8
            gq = G_QL + j_ * 5
            scale = (NOPE + ROPE) ** -0.5
            wdq, kdq = wload(mla_w_dq[j_], 0, 8, QL)
            wuq, kuq = wload(mla_w_uq[j_], 3, 3, 1536)
            wo, ko_ = wload(mla_w_o[j_], 8, 8, D)
            with contextlib.ExitStack() as st:
                sbt = lambda n, s, d=F32: st.enter_context(nc.sbuf_tensor(uniq(n), s, d))
                def aview(slot0, nslots):
                    return Wt[:, slot0:slot0 + nslots, :].rearrange("p s c -> p (s c)")
                xT = sbt("xT", [128, 8, T])
                rs = sbt("rs", [128, T])
                cqf = sbt("cqf", [128, 3, T])
                qnf = [sbt(f"qnf{i}", [128, T]) for i in range(4)]
                qpf = [sbt(f"qpf{i}", [64, T]) for i in range(4)]
                rsn = [sbt(f"rsn{i}", [128, T]) for i in range(2)]
                rsp = [sbt(f"rsp{i}", [64, T]) for i in range(2)]
                t1 = [sbt(f"t1{i}", [64, T]) for i in range(2)]
                t2 = [sbt(f"t2{i}", [64, T]) for i in range(2)]
                tab = sbt("tab", [64, 2, T])
                hT = aview(16, 4).rearrange("p (c t) -> p c t", c=8)
                sqb = aview(20, 4).rearrange("p (c t) -> p c t", c=8)
                qnT = aview(24, 4).rearrange("p (c t) -> p c t", c=8)
                qpT = aview(28, 4).rearrange("p (c t) -> p c t", c=8)
                oT = aview(32, 4).rearrange("p (c t) -> p c t", c=8)
                cqT = aview(36, 2)[:, 0:3 * T].rearrange("p (c t) -> p c t", c=3)
                pTall = aview(38, 2).rearrange("p (c t) -> p c t", c=4)
                pT = [pTall[:, i, :] for i in range(4)]
                knS = [aview(40 + 4 * i, 4) for i in range(2)]
                vS = [aview(48 + 4 * i, 4).rearrange("p (b d) -> p b d", d=128) for i in range(2)]
                kpS = aview(56, 4)
                P.op("pool", lambda e: e.memset(kpS[64:128, :], 0.0), writes=K("kpS"))
                P.op("pool", lambda e: e.memset(qpT[64:128, :, :], 0.0), writes=K("qpT", range(8)))
                rl = sbt("rl", [128, T])
                for t in range(NTI):
                    nk = (t + 1) * T
                    nkb = nk // 128
                    if t == 0:
                        load_xT(xT, t, T, False, None)
                    P.dma("sp", tab[:], tabM[:, :, t * T:(t + 1) * T].rearrange("w p t -> p w t"), writes=K("tab"))
                    P.dma("sp", kpS[0:64, 0:nk], kpeT_d[:, 0:nk], writes=K("kpS"))
                    norm_T([xT[:, c, :] for c in range(8)], [K("xT", [c]) for c in range(8)], 128,
                           [gbase + c for c in range(8)],
                           [hT[:, c, :] for c in range(8)], [K("hT", [c]) for c in range(8)],
                           [sqb[:, c, :] for c in range(8)], [K("sqb", [c]) for c in range(8)],
                           rs[:], K("rs"), 6, T)
                    for m in range(3):
                        b = 6 + m % 2
                        if m == 0:
                            for k in range(8):
                                mm_group(bank(b), [(wdq[:, k, 0:128], hT[:, k, :])], reads=kdq + K("hT", [k]), writes=[BK(b)],
                                         first=(k == 0), last=(k == 7))
                        else:
                            mm_group(bank(b), [(wdq[:, k, m * 128:(m + 1) * 128], hT[:, k, :]) for k in range(8)],
                                     reads=kdq + K("hT", range(8)), writes=[BK(b)])
                        P.op("act", lambda e, m=m, b=b: e.activation(out=cqf[:, m, :], in_=bank(b), func=AF.Copy),
                             reads=[BK(b)], writes=K("cqf", [m]))
                    norm_T([cqf[:, m, :] for m in range(3)], [K("cqf", [m]) for m in range(3)], 128, [gq, gq + 1, gq + 2],
                           [cqT[:, m, :] for m in range(3)], [K("cqT", [m]) for m in range(3)],
                           [sqb[:, m, :] for m in range(3)], [K("sqb", [m]) for m in range(3)], rs[:], K("rs"), 6, T)
                    def qS0(h):
                        r4 = h % 4
                        mm_group(bank(7), [(wuq[:, m, h * 192:h * 192 + 128], cqT[:, m, :]) for m in range(3)],
                                 reads=kuq + K("cqT", range(3)), writes=[BK(7)])
                        P.op("dve", lambda e: e.tensor_copy(out=qnf[r4][:], in_=bank(7)), reads=[BK(7)], writes=K("qnf", [r4]))
                        mm_group(bank(6)[0:64, :], [(wuq[:, m, h * 192 + 128:h * 192 + 192], cqT[:, m, :]) for m in range(3)],
                                 reads=kuq + K("cqT", range(3)), writes=[BK(6)])
                        P.op("act", lambda e: e.activation(out=qpf[r4][:], in_=bank(6)[0:64, :], func=AF.Copy), reads=[BK(6)], writes=K("qpf", [r4]))

                    def qS1(h):
                        r4, r = h % 4, h % 2
                        P.op("pool", lambda e: e.tensor_tensor(out=sqb[:, 3 + r, :], in0=qnf[r4][:], in1=qnf[r4][:], op=ALU.mult),
                             reads=K("qnf", [r4]), writes=K("sqb", [3 + r]))
                        P.op("pool", lambda e: e.tensor_tensor(out=sqb[0:64, 5 + r, :], in0=qpf[r4][:], in1=qpf[r4][:], op=ALU.mult),
                             reads=K("qpf", [r4]), writes=K("sqb", [5 + r]))
                        mm_group(bank(5), [(onesb[:], sqb[:, 3 + r, :])], reads=K("onesb") + K("sqb", [3 + r]), writes=[BK(5)])
                        mm_group(bank(4)[0:64, :], [(onesb[0:64, 0:64], sqb[0:64, 5 + r, :])], reads=K("onesb") + K("sqb", [5 + r]), writes=[BK(4)])

                    def qS2(h):
                        r = h % 2
                        P.op("act", lambda e: e.activation(out=rsn[r][:], in_=bank(5), func=AF.Ln, scale=1.0 / 128, bias=epsc[:, 0:1]),
                             reads=[BK(5)] + K("epsc"), writes=K("rsn", [r]))
                        P.op("act", lambda e: e.activation(out=rsn[r][:], in_=rsn[r][:], func=AF.Exp, scale=-0.5), reads=K("rsn", [r]), writes=K("rsn", [r]))
                        P.op("act", lambda e: e.activation(out=rsp[r][:], in_=bank(4)[0:64, :], func=AF.Ln, scale=1.0 / 64, bias=epsc[0:64, 0:1]),
                             reads=[BK(4)] + K("epsc"), writes=K("rsp", [r]))
                        P.op("act", lambda e: e.activation(out=rsp[r][:], in_=rsp[r][:], func=AF.Exp, scale=-0.5), reads=K("rsp", [r]), writes=K("rsp", [r]))

                    def qS3(h):
                        r4, r = h % 4, h % 2
                        P.op("dve", lambda e: e.scalar_tensor_tensor(out=qnT[:, h, :], in0=qnf[r4][:], scalar=gains[:, gq + 3:gq + 4], in1=rsn[r][:],
                                                                     op0=ALU.mult, op1=ALU.mult),
                             reads=K("qnf", [r4]) + K("rsn", [r]) + K("gains"), writes=K("qnT", [h]))
                        P.op("dve", lambda e: e.scalar_tensor_tensor(out=qpf[r4][:], in0=qpf[r4][:], scalar=gains[0:64, gq + 4:gq + 5], in1=rsp[r][:],
                                                                     op0=ALU.mult, op1=ALU.mult),
                             reads=K("qpf", [r4]) + K("rsp", [r]) + K("gains"), writes=K("qpf", [r4]))

                    def qS4(h):
                        r4, r = h % 4, h % 2
                        rope_T(qpf[r4][:], K("qpf", [r4]), 64, tab[:, 0, :], tab[:, 1, :], K("tab"), t1[r][:], t2[r][:],
                               [("t1", r), ("t2", r, 0), ("t2", r, 1)], qpT[0:64, h, :], K("qpT", [h]))

                    qst = [qS0, qS1, qS2, qS3, qS4]
                    for step in range(H + len(qst) - 1):
                        for k in range(len(qst) - 1, -1, -1):
                            h = step - k
                            if 0 <= h < H:
                                qst[k](h)
                    SB = [0, 1, 6]
                    items = [(h, jb) for h in range(H) for jb in range(nkb)]

                    def geom(jb):
                        d = jb - 4 * t
                        return d, (d * 128 if d > 0 else 0)

                    def emit_S(i):
                        h, jb = items[i]
                        kb = h % 2
                        if jb == 0:
                            P.dma("sp", knS[kb][:, 0:nk], knT_d[h, :, 0:nk], writes=K("knS", [kb]))
                            P.dma("sp", vS[kb][:, 0:nkb, :], vs_d[h, :, 0:nkb, :], writes=K("vS", [kb]))
                        d, c0 = geom(jb)
                        bs = SB[i % 3]
                        ks = slice(jb * 128, (jb + 1) * 128)
                        def fs(e):
                            e.matmul(bank(bs)[:, c0:T], knS[kb][:, ks], qnT[:, h, c0:T], start=True, stop=False)
                            return e.matmul(bank(bs)[:, c0:T], kpS[:, ks], qpT[:, h, c0:T], start=False, stop=True)
                        P.op("pe", fs, reads=K("knS", [kb]) + K("kpS") + K("qnT", [h]) + K("qpT", [h]), writes=[BK(bs)])

                    def emit_rest(i):
                        h, jb = items[i]
                        kb = h % 2
                        d, c0 = geom(jb)
                        bs = SB[i % 3]
                        pt = pT[i % 4]
                        pk = K("pT", [i % 4])
                        bO, bL = 2 + h % 2, 4 + h % 2
                        P.op("act", lambda e: e.activation(out=pt[:, c0:T], in_=bank(bs)[:, c0:T], func=AF.Exp, scale=scale),
                             reads=[BK(bs)], writes=pk)
                        if d >= 0:
                            P.op("pool", lambda e: e.tensor_tensor(out=pt[:, c0:c0 + 128], in0=pt[:, c0:c0 + 128], in1=maskb[:], op=ALU.mult),
                                 reads=pk + K("maskb"), writes=pk)
                        def fpv(e):
                            e.matmul(bank(bO)[:, c0:T], vS[kb][:, jb, :], pt[:, c0:T], start=(jb == 0), stop=(jb == nkb - 1))
                            return e.matmul(bank(bL)[:, c0:T], onesb[:], pt[:, c0:T], start=(jb == 0), stop=(jb == nkb - 1))
                        P.op("pe", fpv, reads=K("vS", [kb]) + pk + K("onesb"), writes=[BK(bO), BK(bL)])
                        if jb == nkb - 1:
                            P.op("dve", lambda e: e.reciprocal(out=rl[:], in_=bank(bL)), reads=[BK(bL)], writes=K("rl"))
                            P.op("dve", lambda e: e.tensor_tensor(out=oT[:, h, :], in0=bank(bO), in1=rl[:], op=ALU.mult),
                                 reads=[BK(bO)] + K("rl"), writes=K("oT", [h]))

                    DEPTH_S = 2
                    for i in range(min(DEPTH_S, len(items))):
                        emit_S(i)
                    for i in range(len(items)):
                        if i + DEPTH_S < len(items):
                            emit_S(i + DEPTH_S)
                        emit_rest(i)
                    for f in range(8):
                        bo = 6 + f % 2
                        fs_ = slice(f * 128, (f + 1) * 128)
                        mm_group(bank(bo), [(wo[:, hh, fs_], oT[:, hh, :]) for hh in range(H)],
                                 reads=ko_ + K("oT", range(8)), writes=[BK(bo)])
                        P.op("dve", lambda e, f=f, bo=bo: e.tensor_tensor(out=xT[:, f, :], in0=bank(bo), in1=xT[:, f, :], op=ALU.add),
                             reads=[BK(bo)] + K("xT", [f]), writes=K("xT", [f]))
                        store_chunk_prefetch(xT, f, t, T, NTI)
                P.barrier()

        stages = [("pro",)]
        for l in range(DEPTH):
            stages.append(("ffn", l, 0))
            stages.append(("ret", l) if l < N_SELF else ("mla", l))
            stages.append(("ffn", l, 1))
            if l == N_SELF - 1:
                stages.append(("kv",))
        if nstages is not None:
            stages = stages[:nstages]
        ffn_idx = [i for i, s in enumerate(stages) if s[0] == "ffn"]
        assert ffn_idx and ffn_idx[-1] == len(stages) - 1, "last stage must be an ffn stage"
        for i, s in enumerate(stages):
            if s[0] == "pro":
                prologue()
            elif s[0] == "ffn":
                ffn_stage(s[1], s[2], first=(i == ffn_idx[0]), last=(i == len(stages) - 1))
            elif s[0] == "ret":
                ret_stage(s[1])
            elif s[0] == "kv":
                kv_stage()
            elif s[0] == "mla":
                mla_stage(s[1])
        P.finish()
        nc._ninst = P.ninst
    return nc


def host_consts():
    c = np.zeros((128, C_NC), np.float32)
    c[:, C_ID:C_ID + 128] = np.eye(128, dtype=np.float32)
    p = np.arange(128)
    c[:, C_MASK:C_MASK + 128] = (p[:, None] <= p[None, :]).astype(np.float32)
    gam = 1.0 - 2.0 ** (-5.0 - np.arange(H, dtype=np.float64))
    c[:, C_KD:C_KD + 8] = (gam[None, :] ** (127 - p)[:, None]) * (RDK ** -0.5)
    c[:, C_EPSR:C_EPSR + 8] = EPS * gam[None, :] ** (2.0 * (127 - p)[:, None])
    c[:, C_INVR] = ROPE_BASE ** (-(2.0 * (p % 64)) / 128.0)
    c[:64, C_INVM] = ROPE_BASE ** (-(2.0 * (p[:64] % 32)) / 64.0)
    c[:, C_SGR] = np.where(p < 64, 1.0, -1.0)
    c[:64, C_SGM] = np.where(p[:64] < 32, 1.0, -1.0)
    return c


def host_gains(inp):
    g = np.zeros((128, G_NC), np.float32)

    def put(col, vec):
        vec = np.asarray(vec, np.float32).reshape(-1)
        n = vec.shape[0]
        if n >= 128:
            k = n // 128
            g[:, col:col + k] = vec.reshape(k, 128).T
        else:
            g[:n, col] = vec
    ng = np.asarray(inp["norm_g"])
    for l in range(DEPTH):
        for i in range(3):
            put(G_NORM + (l * 3 + i) * 8, ng[l, i])
    put(G_KVN, inp["kv_norm_g"])
    put(G_LAT, inp["kv_latent_norm_g"])
    put(G_KN, inp["k_nope_norm_g"])
    put(G_KR, inp["k_rope_norm_g"])
    for j in range(2):
        put(G_QL + j * 5, np.asarray(inp["mla_q_lora_norm_g"])[j])
        put(G_QL + j * 5 + 3, np.asarray(inp["mla_q_nope_norm_g"])[j])
        put(G_QL + j * 5 + 4, np.asarray(inp["mla_q_rope_norm_g"])[j])
    for l in range(N_SELF):
        put(G_GN + l * 16, np.asarray(inp["ret_gn_g"])[l].reshape(-1))
    return g


WKEYS = ["ffn_w_gate", "ffn_w_up", "ffn_w_down", "ret_w_in", "ret_w_o", "kv_w_down", "kv_w_up",
         "mla_w_dq", "mla_w_uq", "mla_w_o"]


def make_in_maps(inp, ncores, NT):
    consts = host_consts()
    gains = host_gains(inp)
    shared = {k: np.ascontiguousarray(np.asarray(inp[k], dtype=np.float32)) for k in WKEYS}
    x = np.asarray(inp["x"], dtype=np.float32)
    pos = np.asarray(inp["positions"]).astype(np.int32)
    maps = []
    for b in range(ncores):
        m = dict(shared)
        m["x"] = np.ascontiguousarray(x[b, :NT])
        m["pos"] = np.ascontiguousarray(pos[b:b + 1, :NT])
        m["consts"] = consts
        m["gains"] = gains
        maps.append(m)
    return maps


def kernel(**inputs):
    nc = build(SEQ)
    maps = make_in_maps(inputs, 8, SEQ)
    res = run_bass_kernel_spmd(nc, maps, core_ids=list(range(8)))
    return np.stack([np.asarray(r["out"], dtype=np.float32) for r in res.results], axis=0)
```
